# Optimizing a Trainium2 kernel written in Bass

```python
import jax, jax.numpy as jnp
from jax import lax
import numpy as np

D_MODEL = 1024
BATCH = 8
SEQ = 2048
DEPTH = 4

CHUNK = 64
N_MEM = 256
MIX_WIDTH = D_MODEL
N_GROUPS = 4
GROUP_WIDTH = MIX_WIDTH // N_GROUPS
HEAD_DIM = 64
N_HEADS_A = GROUP_WIDTH // HEAD_DIM
N_HEADS_C = GROUP_WIDTH // HEAD_DIM
N_HEADS_D = GROUP_WIDTH // HEAD_DIM
CONV_WIDTH_A = 4
POOL_WINDOWS = (2, 4, 8, 16)
POOL_GROUP = GROUP_WIDTH // len(POOL_WINDOWS)
LORA_DECAY = 64
LORA_ICLR = 64
Q_BLOCK = 128
N_MEM_HEADS = 4
MEM_HEAD_DIM = D_MODEL // N_MEM_HEADS
NORM_EPS = 1e-6
GN_EPS = 64e-5
MLSTM_F_BIAS = (3.0, 6.0)
FOX_F_BIAS = (1.0, 4.0)

IN_LAYOUT = (
    ("a_q", GROUP_WIDTH), ("a_k", GROUP_WIDTH), ("a_v", GROUP_WIDTH),
    ("a_i", N_HEADS_A), ("a_f", N_HEADS_A), ("a_z", GROUP_WIDTH),
    ("b_x", GROUP_WIDTH), ("b_z", GROUP_WIDTH),
    ("c_r", GROUP_WIDTH), ("c_k", GROUP_WIDTH), ("c_v", GROUP_WIDTH),
    ("c_w", LORA_DECAY), ("c_a", LORA_ICLR), ("c_z", GROUP_WIDTH),
    ("d_q", GROUP_WIDTH), ("d_k", GROUP_WIDTH), ("d_v", GROUP_WIDTH),
    ("d_f", N_HEADS_D), ("d_z", GROUP_WIDTH),
)
IN_OFFSETS = {name: sum(s for _, s in IN_LAYOUT[:i]) for i, (name, _) in enumerate(IN_LAYOUT)}
IN_SPLITS = tuple(IN_OFFSETS[name] for name, _ in IN_LAYOUT[1:])
N_IN = sum(s for _, s in IN_LAYOUT)
MU_WIDTH = 3 * GROUP_WIDTH + LORA_DECAY + LORA_ICLR
MU_SPLITS = (GROUP_WIDTH, 2 * GROUP_WIDTH, 3 * GROUP_WIDTH, 3 * GROUP_WIDTH + LORA_DECAY)

kernel_name = "hybrid_parallel_group_streaming_encoder"


def rms_norm(x, g):
    x32 = x.astype(jnp.float32)
    y = x32 * lax.rsqrt(jnp.mean(x32 * x32, axis=-1, keepdims=True) + NORM_EPS)
    return (y * g).astype(x.dtype)


def split_heads(u, n_heads):
    b, s, _ = u.shape
    return u.reshape(b, s, n_heads, -1).transpose(0, 2, 1, 3)


def merge_heads(u):
    b, h, s, d = u.shape
    return u.transpose(0, 2, 1, 3).reshape(b, s, h * d)


def causal_depthwise_conv(u, w):
    c, k = u.shape[-1], w.shape[0]
    return lax.conv_general_dilated(
        u, w[:, None, :].astype(u.dtype), window_strides=(1,), padding=[(k - 1, 0)],
        dimension_numbers=("NWC", "WIO", "NWC"), feature_group_count=c)


def token_shift(u, mu):
    prev = jnp.pad(u, ((0, 0), (1, 0), (0, 0)))[:, :-1]
    return u + mu * (prev - u)


def head_rms_norm(y, g):
    y32 = y.astype(jnp.float32)
    y32 = y32 * lax.rsqrt(jnp.mean(y32 * y32, axis=-1, keepdims=True) + NORM_EPS)
    return merge_heads(y32) * g


def mlstm_chunkwise(q, k, v, i_pre, f_pre):
    f32 = jnp.float32
    b_, h_, s_, dh = q.shape
    nc, L = s_ // CHUNK, CHUNK
    qc = q.astype(f32).reshape(b_, h_, nc, L, dh)
    kc = (k.astype(f32) * dh ** -0.5).reshape(b_, h_, nc, L, dh)
    vc = v.astype(f32).reshape(b_, h_, nc, L, dh)
    ig = i_pre.astype(f32).reshape(b_, h_, nc, L)
    bcum = jnp.cumsum(jax.nn.log_sigmoid(f_pre.astype(f32)).reshape(b_, h_, nc, L), axis=-1)
    g = bcum[..., -1]
    w_loc = g[..., None] - bcum + ig
    m_loc = jnp.max(w_loc, axis=-1)
    e_loc = jnp.exp(w_loc - m_loc[..., None])
    c_loc = jnp.einsum("bhcs,bhcsd,bhcse->bhcde", e_loc, vc, kc)
    n_loc = jnp.einsum("bhcs,bhcse->bhce", e_loc, kc)

    def step(carry, xs):
        c_prev, n_prev, m_prev = carry
        g_c, m_l, c_l, n_l = xs
        m_new = jnp.maximum(g_c + m_prev, m_l)
        a = jnp.exp(g_c + m_prev - m_new)
        bb = jnp.exp(m_l - m_new)
        c_new = a[..., None, None] * c_prev + bb[..., None, None] * c_l
        n_new = a[..., None] * n_prev + bb[..., None] * n_l
        return (c_new, n_new, m_new), (c_prev, n_prev, m_prev)

    init = (jnp.zeros((b_, h_, dh, dh), f32), jnp.zeros((b_, h_, dh), f32), jnp.zeros((b_, h_), f32))
    xs = (jnp.moveaxis(g, 2, 0), jnp.moveaxis(m_loc, 2, 0), jnp.moveaxis(c_loc, 2, 0), jnp.moveaxis(n_loc, 2, 0))
    _, (c_in, n_in, m_in) = lax.scan(step, init, xs)
    c_in = jnp.moveaxis(c_in, 0, 2)
    n_in = jnp.moveaxis(n_in, 0, 2)
    m_in = jnp.moveaxis(m_in, 0, 2)
    causal = jnp.tril(jnp.ones((L, L), dtype=bool))
    dmat = jnp.where(causal, bcum[..., :, None] - bcum[..., None, :] + ig[..., None, :], -jnp.inf)
    m_inter = bcum + m_in[..., None]
    m_t = jnp.maximum(m_inter, jnp.max(dmat, axis=-1))
    s_qk = jnp.einsum("bhctd,bhcsd->bhcts", qc, kc) * jnp.exp(dmat - m_t[..., None])
    inter_w = jnp.exp(m_inter - m_t)
    num = jnp.einsum("bhcts,bhcsd->bhctd", s_qk, vc) + inter_w[..., None] * jnp.einsum("bhcde,bhcte->bhctd", c_in, qc)
    den = jnp.sum(s_qk, axis=-1) + inter_w * jnp.einsum("bhce,bhcte->bhct", n_in, qc)
    h = num / jnp.maximum(jnp.abs(den), jnp.exp(-m_t))[..., None]
    return h.reshape(b_, h_, s_, dh)


def multiscale_pool(u, w_pool, scale):
    u32 = u.astype(jnp.float32)
    b_, s_, _ = u.shape
    cs = jnp.cumsum(u32, axis=1)
    pos = jnp.arange(s_, dtype=jnp.float32)
    outs = []
    for gi, win in enumerate(POOL_WINDOWS):
        sl = slice(gi * POOL_GROUP, (gi + 1) * POOL_GROUP)
        cg = cs[..., sl]
        shifted = jnp.pad(cg, ((0, 0), (win, 0), (0, 0)))[:, :s_]
        count = jnp.minimum(pos + 1.0, float(win))
        outs.append((cg - shifted) / count[None, :, None] - u32[..., sl])
    pooled = jnp.stack(outs, axis=2)
    mixed = jnp.einsum("bsgc,gcd->bsgd", pooled, w_pool).reshape(b_, s_, GROUP_WIDTH)
    return mixed * scale


def rwkv7_time_mix(r, k, v, w_lo, a_lo, mu, w0, w2, a0, a2, key_k, key_a, bonus_u, gn_g, gn_b):
    f32 = jnp.float32
    mu_r, mu_k, mu_v, mu_w, mu_a = jnp.split(mu.astype(f32), MU_SPLITS)
    r = token_shift(r.astype(f32), mu_r)
    k = token_shift(k.astype(f32), mu_k)
    v = token_shift(v.astype(f32), mu_v)
    w_lo = token_shift(w_lo.astype(f32), mu_w)
    a_lo = token_shift(a_lo.astype(f32), mu_a)
    w_log = -jax.nn.softplus(-(w0 + jnp.tanh(w_lo) @ w2.astype(f32))) - 0.5
    decay = jnp.exp(-jnp.exp(w_log))
    a = jax.nn.sigmoid(a0 + a_lo @ a2.astype(f32))
    b_, s_, _ = r.shape
    hsplit = lambda u: u.reshape(b_, s_, N_HEADS_C, HEAD_DIM)
    kk = hsplit(k * key_k)
    kk = kk / jnp.maximum(jnp.sqrt(jnp.sum(kk * kk, axis=-1, keepdims=True)), 1e-12)
    k = k * (1.0 + (a - 1.0) * key_a)
    rh, kh, vh, wh, ah = hsplit(r), hsplit(k), hsplit(v), hsplit(decay), hsplit(a)

    def step(state, xs):
        r_t, k_t, v_t, w_t, kk_t, a_t = xs
        sa = jnp.einsum("bhvk,bhk->bhv", state, -kk_t)
        state = state * w_t[:, :, None, :] + sa[..., None] * (kk_t * a_t)[:, :, None, :] + v_t[..., None] * k_t[:, :, None, :]
        return state, jnp.einsum("bhvk,bhk->bhv", state, r_t)

    xs = tuple(u.transpose(1, 0, 2, 3) for u in (rh, kh, vh, wh, kk, ah))
    init = jnp.zeros((b_, N_HEADS_C, HEAD_DIM, HEAD_DIM), f32)
    _, y = lax.scan(step, init, xs)
    y = y.transpose(1, 0, 2, 3)
    mean = jnp.mean(y, axis=-1, keepdims=True)
    var = jnp.mean(jnp.square(y - mean), axis=-1, keepdims=True)
    y = ((y - mean) * lax.rsqrt(var + GN_EPS)).reshape(b_, s_, GROUP_WIDTH) * gn_g + gn_b
    bonus = jnp.sum(rh * kh * bonus_u.astype(f32).reshape(N_HEADS_C, HEAD_DIM), axis=-1, keepdims=True) * vh
    return y + bonus.reshape(b_, s_, GROUP_WIDTH)


def forgetting_attention(q, k, v, f_pre, qk_g):
    dh, s_len = q.shape[-1], q.shape[2]
    q = rms_norm(q, qk_g[0]) * (dh ** -0.5)
    k = rms_norm(k, qk_g[1])
    log_f_cum = jnp.cumsum(jax.nn.log_sigmoid(f_pre.astype(jnp.float32)), axis=-1)
    outs = []
    for blk in range(s_len // Q_BLOCK):
        q0, q1 = blk * Q_BLOCK, (blk + 1) * Q_BLOCK
        logits = jnp.einsum("bhtd,bhsd->bhts", q[:, :, q0:q1], k[:, :, :q1]).astype(jnp.float32)
        logits = logits + log_f_cum[:, :, q0:q1, None] - log_f_cum[:, :, None, :q1]
        causal = (q0 + jnp.arange(Q_BLOCK))[:, None] >= jnp.arange(q1)[None, :]
        p = jax.nn.softmax(jnp.where(causal, logits, -jnp.inf), axis=-1)
        outs.append(jnp.einsum("bhts,bhsd->bhtd", p.astype(v.dtype), v[:, :, :q1]))
    return jnp.concatenate(outs, axis=2)


def memory_cross_attention(h, mem_n, wq, wkv, wo):
    q = split_heads(h @ wq, N_MEM_HEADS)
    k, v = jnp.split(mem_n @ wkv, 2, axis=-1)
    k, v = split_heads(k, N_MEM_HEADS), split_heads(v, N_MEM_HEADS)
    logits = jnp.einsum("bhtd,bhmd->bhtm", q, k).astype(jnp.float32) * (MEM_HEAD_DIM ** -0.5)
    p = jax.nn.softmax(logits, axis=-1)
    o = jnp.einsum("bhtm,bhmd->bhtd", p.astype(v.dtype), v)
    return merge_heads(o) @ wo


def setup_inputs(seed: int = 0) -> dict:
    key = jax.random.key(seed)
    ks = jax.random.split(key, 32)
    f32 = jnp.float32
    nrm = lambda kk, shape, scale: scale * jax.random.normal(kk, shape, f32)
    L, D, GW = DEPTH, D_MODEL, GROUP_WIDTH
    b_in = nrm(ks[5], (L, N_IN), 0.02)
    fa, fd = IN_OFFSETS["a_f"], IN_OFFSETS["d_f"]
    b_in = b_in.at[:, fa:fa + N_HEADS_A].add(jnp.linspace(MLSTM_F_BIAS[0], MLSTM_F_BIAS[1], N_HEADS_A, dtype=f32))
    b_in = b_in.at[:, fd:fd + N_HEADS_D].add(jnp.linspace(FOX_F_BIAS[0], FOX_F_BIAS[1], N_HEADS_D, dtype=f32))
    return {
        "x": nrm(ks[0], (BATCH, SEQ, D), 1.0),
        "mem": nrm(ks[1], (BATCH, N_MEM, D), 1.0),
        "pre_norm_g": 1.0 + nrm(ks[2], (L, D), 0.05),
        "post_norm_g": 1.0 + nrm(ks[3], (L, D), 0.05),
        "w_in": nrm(ks[4], (L, D, N_IN), D ** -0.5),
        "b_in": b_in,
        "conv_a": nrm(ks[6], (L, CONV_WIDTH_A, 2 * GW), CONV_WIDTH_A ** -0.5),
        "norm_a_g": 1.0 + nrm(ks[7], (L, GW), 0.05),
        "pool_w": nrm(ks[8], (L, len(POOL_WINDOWS), POOL_GROUP, POOL_GROUP), POOL_GROUP ** -0.5),
        "pool_scale": 0.5 + nrm(ks[9], (L, GW), 0.05),
        "shift_mu_c": jax.random.uniform(ks[10], (L, MU_WIDTH), f32),
        "decay_w0": jax.random.uniform(ks[11], (L, GW), f32, -3.0, 1.0),
        "decay_w2": nrm(ks[12], (L, LORA_DECAY, GW), 0.1),
        "iclr_a0": nrm(ks[13], (L, GW), 0.1),
        "iclr_a2": nrm(ks[14], (L, LORA_ICLR, GW), 0.1),
        "key_k": 0.85 + nrm(ks[15], (L, GW), 0.05),
        "key_a": 1.0 + nrm(ks[16], (L, GW), 0.05),
        "bonus_u": nrm(ks[17], (L, GW), 0.1),
        "gn_c_g": 1.0 + nrm(ks[18], (L, GW), 0.05),
        "gn_c_b": nrm(ks[19], (L, GW), 0.02),
        "qk_norm_d": 1.0 + nrm(ks[20], (L, 2, HEAD_DIM), 0.05),
        "w_out": nrm(ks[21], (L, MIX_WIDTH, D), MIX_WIDTH ** -0.5),
        "mem_norm_g": 1.0 + nrm(ks[22], (D,), 0.05),
        "xattn_pre_g": 1.0 + nrm(ks[23], (L, D), 0.05),
        "xattn_post_g": 1.0 + nrm(ks[24], (L, D), 0.05),
        "xattn_wq": nrm(ks[25], (L, D, D), D ** -0.5),
        "xattn_wkv": nrm(ks[26], (L, D, 2 * D), D ** -0.5),
        "xattn_wo": nrm(ks[27], (L, D, D), D ** -0.5),
    }


def reference(x, mem, pre_norm_g, post_norm_g, w_in, b_in, conv_a, norm_a_g, pool_w, pool_scale,
              shift_mu_c, decay_w0, decay_w2, iclr_a0, iclr_a2, key_k, key_a, bonus_u, gn_c_g, gn_c_b,
              qk_norm_d, w_out, mem_norm_g, xattn_pre_g, xattn_post_g, xattn_wq, xattn_wkv, xattn_wo):
    mem_n = rms_norm(mem, mem_norm_g)
    for l in range(DEPTH):
        h = rms_norm(x, pre_norm_g[l])
        proj = h @ w_in[l] + b_in[l]
        (a_q, a_k, a_v, a_i, a_f, a_z, b_x, b_z, c_r, c_k, c_v, c_w, c_a, c_z,
         d_q, d_k, d_v, d_f, d_z) = jnp.split(proj, IN_SPLITS, axis=-1)
        a_qk = jax.nn.silu(causal_depthwise_conv(jnp.concatenate([a_q, a_k], axis=-1), conv_a[l]))
        a_q, a_k = jnp.split(a_qk, 2, axis=-1)
        h_a = mlstm_chunkwise(split_heads(a_q, N_HEADS_A), split_heads(a_k, N_HEADS_A),
                              split_heads(a_v, N_HEADS_A), a_i.transpose(0, 2, 1), a_f.transpose(0, 2, 1))
        y_a = head_rms_norm(h_a, norm_a_g[l]) * jax.nn.silu(a_z)
        y_b = multiscale_pool(b_x, pool_w[l], pool_scale[l]) * jax.nn.silu(b_z)
        y_c = rwkv7_time_mix(c_r, c_k, c_v, c_w, c_a, shift_mu_c[l], decay_w0[l], decay_w2[l],
                             iclr_a0[l], iclr_a2[l], key_k[l], key_a[l], bonus_u[l],
                             gn_c_g[l], gn_c_b[l]) * jax.nn.silu(c_z)
        h_d = forgetting_attention(split_heads(d_q, N_HEADS_D), split_heads(d_k, N_HEADS_D),
                                   split_heads(d_v, N_HEADS_D), d_f.transpose(0, 2, 1), qk_norm_d[l])
        y_d = merge_heads(h_d) * jax.nn.silu(d_z)
        y = jnp.concatenate([u.astype(x.dtype) for u in (y_a, y_b, y_c, y_d)], axis=-1) @ w_out[l]
        x = x + rms_norm(y, post_norm_g[l])
        hm = rms_norm(x, xattn_pre_g[l])
        x = x + rms_norm(memory_cross_attention(hm, mem_n, xattn_wq[l], xattn_wkv[l], xattn_wo[l]),
                         xattn_post_g[l])
    return x
```

```python
import contextlib
import os
import numpy as np
import concourse.bass as bass
import concourse.mybir as mybir
from concourse.bass_utils import run_bass_kernel_spmd

F32 = mybir.dt.float32
BF16 = mybir.dt.bfloat16
AF = mybir.ActivationFunctionType
ALU = mybir.AluOpType
AX = mybir.AxisListType

S = 2048
D = 1024
NL = 4
SEG = 512
NSEG = S // SEG
N_IN = 3724
OFF = dict(a_q=0, a_k=256, a_v=512, a_i=768, a_f=772, a_z=776, b_x=1032, b_z=1288, c_r=1544, c_k=1800,
           c_v=2056, c_w=2312, c_a=2376, c_z=2440, d_q=2696, d_k=2952, d_v=3208, d_f=3464, d_z=3468)
NEG = -30000.0
KSTOP = int(os.environ.get('KSTOP', '99'))
KD = int(os.environ.get('KD', '99'))


class Prog:
    NDMA = 12

    def __init__(self, nc):
        self.nc = nc
        self.ops = []
        self.state = {}
        self.children = {}
        self.clock = {e: {} for e in ("pe", "act", "dve", "pool", "sp")}
        self.evclock = {}
        self.count = {}
        self.dma_rr = {"sp": 0, "pool": 0, "act": 0}
        self.alias = set()

    def _conf(self, key):
        out = []
        for i in range(1, len(key) + 1):
            k = key[:i]
            if k in self.state:
                out.append(k)
        for k in self.children.get(key, ()):
            if k != key:
                out.append(k)
        return out

    def _reg(self, key):
        if key not in self.state:
            self.state[key] = [None, {}]
            for i in range(1, len(key) + 1):
                self.children.setdefault(key[:i], set()).add(key)

    def _k(self, key):
        key = key if isinstance(key, tuple) else (key,)
        if key[0] in self.alias:
            key = ("SCR",) + key
        return key

    def add(self, eng, fn, r=(), w=(), dma=False):
        r = [self._k(x) for x in r]
        w = [self._k(x) for x in w]
        deps = {}

        def need(ev):
            if ev is None:
                return
            s, v = ev
            if deps.get(s, 0) < v:
                deps[s] = v

        for k in r:
            self._reg(k)
            for c in self._conf(k):
                need(self.state[c][0])
        for k in w:
            self._reg(k)
            for c in self._conf(k):
                st = self.state[c]
                need(st[0])
                for s, v in st[1].items():
                    need((s, v))
        if dma:
            i = self.dma_rr[eng]
            self.dma_rr[eng] = (i + 1) % self.NDMA
            sem = "dma_%s_%d" % (eng, i)
            inc = 16
            if self.count.get(sem, 0) > 0:
                need((sem, self.count[sem]))
        else:
            sem = eng
            inc = 1
        val = self.count.get(sem, 0) + inc
        self.count[sem] = val
        ev = (sem, val)
        clk = self.clock[eng]
        waits = []
        for s, v in deps.items():
            if eng == "pe" and s == "pe":
                continue
            if clk.get(s, 0) >= v:
                continue
            waits.append((s, v))
        for s, v in waits:
            for s2, v2 in self.evclock[(s, v)].items():
                if clk.get(s2, 0) < v2:
                    clk[s2] = v2
        snap = dict(clk)
        snap[sem] = val
        self.evclock[ev] = snap
        for k in r:
            rd = self.state[k][1]
            if rd.get(sem, 0) < val:
                rd[sem] = val
        for k in w:
            self.state[k] = [ev, {}]
        self.ops.append((eng, fn, waits, sem, inc))
        return ev

    def pe(self, fn, r=(), w=()):
        return self.add("pe", fn, r, w)

    def act(self, fn, r=(), w=()):
        return self.add("act", fn, r, w)

    def dve(self, fn, r=(), w=()):
        return self.add("dve", fn, r, w)

    def pool(self, fn, r=(), w=()):
        return self.add("pool", fn, r, w)

    def dma(self, fn, r=(), w=(), q="sp"):
        return self.add(q, fn, r, w, dma=True)

    def finish(self, keys):
        self.add("sp", None, r=keys, w=())

    def emit(self, es):
        nc = self.nc
        names = sorted(self.count.keys())
        sems = {n: es.enter_context(nc.semaphore("s_" + n)) for n in names}
        block = es.enter_context(nc.Block())
        per = {e: [o for o in self.ops if o[0] == e] for e in self.clock}

        def run(engobj, lst):
            for (_, fn, waits, sem, inc) in lst:
                for s, v in waits:
                    engobj.wait_ge(sems[s], v)
                if fn is None:
                    continue
                ins = fn(engobj)
                ins.then_inc(sems[sem], inc)

        @block.tensor
        def _(e):
            run(e, per["pe"])

        @block.scalar
        def _(e):
            run(e, per["act"])

        @block.vector
        def _(e):
            run(e, per["dve"])

        @block.gpsimd
        def _(e):
            run(e, per["pool"])

        @block.sync
        def _(e):
            run(e, per["sp"])


NCOLP = 64
FM_OFFS = [0, 128, 256, 384, 1032, 1160, 1544, 1672, 1800, 1928, 2056, 2184, 2312]


def host_layout(inp):
    f = np.float32
    colp = np.zeros((NL, 128, NCOLP), f)
    rows = []
    wg = np.zeros((NL, D, 384), f)
    wsm = np.zeros((NL, 128, 256 + 256), f)
    brow = np.zeros((NL, 1, 2048), f)
    for l in range(NL):
        colp[l, :, 0:8] = inp["pre_norm_g"][l].reshape(8, 128).T
        colp[l, :, 8:16] = inp["xattn_pre_g"][l].reshape(8, 128).T
        b = inp["b_in"][l]
        for j, o in enumerate(FM_OFFS):
            colp[l, :, 16 + j] = b[o:o + 128]
        for h in range(4):
            colp[l, 32 * h, 29] = b[OFF["a_i"] + h]
            colp[l, 32 * h, 30] = b[OFF["a_f"] + h]
            colp[l, 32 * h, 31] = b[OFF["d_f"] + h]
        for j in range(4):
            for k in range(4):
                colp[l, :, 32 + 4 * j + k] = inp["conv_a"][l, k, 128 * j:128 * j + 128]
        colp[l, :, 48:55] = inp["shift_mu_c"][l].reshape(7, 128).T
        colp[l, :, 55:57] = inp["decay_w0"][l].reshape(2, 128).T
        colp[l, :, 57:59] = inp["iclr_a0"][l].reshape(2, 128).T
        colp[l, :, 59:61] = inp["key_k"][l].reshape(2, 128).T
        colp[l, :, 61:63] = inp["key_a"][l].reshape(2, 128).T
        colp[l, :, 63] = 0.0
        qk = inp["qk_norm_d"][l]
        rows.append(np.concatenate([
            inp["norm_a_g"][l], inp["pool_scale"][l], inp["bonus_u"][l], inp["gn_c_g"][l], inp["gn_c_b"][l],
            np.tile(qk[0], 4), np.tile(qk[1], 4)]).astype(f))
        w = inp["w_in"][l]
        for h in range(4):
            wg[l, :, 32 * h] = w[:, OFF["a_i"] + h]
            wg[l, :, 128 + 32 * h] = w[:, OFF["a_f"] + h]
            wg[l, :, 256 + 32 * h] = w[:, OFF["d_f"] + h]
        pw = inp["pool_w"][l]
        for j in range(2):
            for gl in range(2):
                wsm[l, 64 * gl:64 * gl + 64, 128 * j + 64 * gl:128 * j + 64 * gl + 64] = pw[2 * j + gl]
        wsm[l, 0:64, 256:512] = inp["decay_w2"][l]
        wsm[l, 64:128, 256:512] = inp["iclr_a2"][l]
        brow[l, 0, :] = np.concatenate([b[OFF["a_v"]:OFF["a_v"] + 256], b[OFF["a_z"]:OFF["a_z"] + 256],
                                        b[OFF["b_z"]:OFF["b_z"] + 256], b[OFF["c_z"]:OFF["c_z"] + 256],
                                        b[OFF["d_q"]:OFF["d_q"] + 512], b[OFF["d_v"]:OFF["d_v"] + 256],
                                        b[OFF["d_z"]:OFF["d_z"] + 256]])
    rowp = np.stack(rows)[:, None, :]
    colx = np.zeros((NL, 128, 2), f)
    for l in range(NL):
        colx[l] = inp["bonus_u"][l].reshape(2, 128).T
    c = {}
    c["ident"] = np.eye(128, dtype=f)
    s_i = np.arange(128)[:, None]
    t_i = np.arange(128)[None, :]
    c["maskT"] = (s_i <= t_i).astype(f)
    c["maskneg"] = np.where(s_i <= t_i, 0.0, NEG).astype(f)
    same = (s_i // 64) == (t_i // 64)
    c["mS"] = ((s_i < t_i) & same).astype(f)
    c["mI"] = ((s_i <= t_i) & same).astype(f)
    c["mSt"] = c["mS"].T.copy()
    c["ones"] = np.ones((128, 128), f)
    c["blk64"] = same.astype(f)
    hs = np.zeros((128, 128), f)
    hs[0:64, 0] = 1.0
    hs[64:128, 1] = 1.0
    c["hsel"] = hs
    sel = np.zeros((128, 4 * 128), f)
    for h in range(4):
        sel[32 * h, 128 * h + 64] = 1.0
    c["sel"] = sel
    selb = np.zeros((128, 256), f)
    for h in range(4):
        selb[32 * h, 64 * h:64 * h + 64] = 1.0
    c["selb"] = selb
    rs = np.ones((128, 512), f)
    rs[:, 0::64] = 0.0
    c["rs64"] = rs
    cnt = np.ones((128, 16), f)
    for t in range(16):
        cnt[:, t] = 1.0 / (t + 1.0)
    c["invc"] = cnt
    consts = np.concatenate([c[k] for k in CONST_KEYS], axis=1)
    constsb = np.concatenate([c[k] for k in CONSTB_KEYS], axis=1)
    return dict(colp=colp, rowp=rowp, wg=wg, wsm=wsm, brow=brow, colx=colx, consts=consts, constsb=constsb)


CONST_KEYS = ["ident", "maskT", "mS", "mI", "mSt", "sel", "selb", "invc"]
CONST_W = dict(ident=128, maskT=128, mS=128, mI=128, mSt=128, sel=512, selb=256, invc=16)
NCONST = sum(CONST_W[k] for k in CONST_KEYS)
CONSTB_KEYS = ["ident", "ones", "maskneg", "blk64", "hsel"]
NCONSTB = 128 * len(CONSTB_KEYS)


class Builder:
    def __init__(self, nc, nl=NL, dbg=(), mixers="ABCD", xattn=True):
        self.nc = nc
        self.nl = nl
        self.dbg = set(dbg)
        self.mixers = mixers
        self.xattn = xattn
        self.es = contextlib.ExitStack()
        self.P = Prog(nc)
        self.outs = []
        self.wq = []
        self.wissued = 0
        self.wused = 0
        self.wreleased = 0

    def sb(self, name, shape, dt=F32):
        nb = int(np.prod(shape[1:])) * (2 if dt == BF16 else 4)
        self.sbsizes = getattr(self, "sbsizes", {})
        self.sbsizes[name] = nb
        try:
            return self.es.enter_context(self.nc.sbuf_tensor("sb_" + name, shape, dt))
        except AssertionError:
            tot = 0
            for k, v in sorted(self.sbsizes.items(), key=lambda kv: -kv[1]):
                tot += v
                print("SBUF", k, v)
            print("SBUF total", tot)
            raise

    def ps(self, name, shape, dt=F32):
        return self.es.enter_context(self.nc.psum_tensor("ps_" + name, shape, dt))

    def scr_reset(self):
        self.o16 = 0
        self.o32 = 0
        self.P.dve(lambda e: e.memset(self.bar[:], 0.0), w=[("SCR",)])

    def scr(self, name, shape, dt=F32):
        n = int(np.prod(shape[1:]))
        if dt == BF16:
            ar, off = self.ar16, self.o16
            self.o16 += n + (n % 2)
            assert self.o16 <= self.N16, ("ar16 overflow", name, self.o16)
        else:
            ar, off = self.ar32, self.o32
            self.o32 += n
            assert self.o32 <= self.N32, ("ar32 overflow", name, self.o32)
        v = ar[0:shape[0], off:off + n]
        if len(shape) == 3:
            v = v.rearrange("p (a b) -> p a b", b=shape[2])
        elif len(shape) == 4:
            v = v.rearrange("p (a b c) -> p a b c", b=shape[2], c=shape[3])
        self.P.alias.add(name)
        return v

    def din(self, name, shape):
        return self.nc.dram_tensor(name, shape, F32, kind="ExternalInput").ap()

    def dump(self, name, ap, shape, key):
        if name not in self.dbg:
            return
        o = self.nc.dram_tensor("dbg_" + name, shape, ap.dtype, kind="ExternalOutput").ap()
        self.P.dma(lambda e: e.dma_start(out=o, in_=ap), r=[key], w=["dbg_" + name])
        self.outs.append("dbg_" + name)

    def wplan(self, tag, src, ncols):
        self.wq.append((tag, src, ncols))

    def _wissue(self):
        while self.wissued < len(self.wq) and self.wissued < self.wreleased + 3:
            i = self.wissued
            tag, src, n = self.wq[i]
            buf = self.WB[i % 3]
            srcv = src.rearrange("(dc p) n -> p dc n", p=128)
            self.P.dma(lambda e, buf=buf, srcv=srcv, n=n: e.dma_start(out=buf[:, :, 0:n], in_=srcv),
                       w=[("WB", i % 3)], q="pool")
            self.wissued += 1

    def wnext(self, tag):
        i = self.wused
        assert self.wq[i][0] == tag, (self.wq[i][0], tag)
        self._wissue()
        assert i < self.wissued, "weight group not issued (too many groups held)"
        self.wused += 1
        return self.WB[i % 3], ("WB", i % 3)

    def wrel(self, n=1):
        self.wreleased += n
        assert self.wreleased <= self.wused
        self._wissue()

    def build(self):
        nc, P = self.nc, self.P
        self.x_d = self.din("x", [S, D])
        self.mem_d = self.din("mem", [256, D])
        self.w_in = self.din("w_in", [self.nl, D, N_IN])
        self.w_out = self.din("w_out", [self.nl, D, D])
        self.wq_d = self.din("xattn_wq", [self.nl, D, D])
        self.wkv_d = self.din("xattn_wkv", [self.nl, D, 2 * D])
        self.wo_d = self.din("xattn_wo", [self.nl, D, D])
        self.post_g = self.din("post_norm_g", [self.nl, D])
        self.xpost_g = self.din("xattn_post_g", [self.nl, D])
        self.memg = self.din("mem_norm_g", [1, D])
        self.colp_d = self.din("colp", [self.nl, 128, NCOLP])
        self.colx_d = self.din("colx", [self.nl, 128, 2])
        self.rowp_d = self.din("rowp", [self.nl, 1, 1792])
        self.wg_d = self.din("wg", [self.nl, D, 384])
        self.wsm_d = self.din("wsm", [self.nl, 128, 512])
        self.brow_d = self.din("brow", [self.nl, 1, 2048])
        self.consts_d = self.din("consts", [128, NCONST])
        self.constsb_d = self.din("constsb", [128, NCONSTB])
        self.out_d = nc.dram_tensor("out", [S, D], F32, kind="ExternalOutput").ap()

        sb, ps = self.sb, self.ps
        self.X = sb("X", [128, 16, D])
        self.hT = sb("hT", [128, 8, SEG], BF16)
        self.yT = sb("yT", [128, 8, SEG], BF16)
        self.WB = [sb("WB%d" % i, [128, 8, 512], BF16) for i in range(3)]
        self.CF = sb("CF", [128, NCONST])
        self.cf = {}
        o = 0
        for k in CONST_KEYS:
            self.cf[k] = self.CF[:, o:o + CONST_W[k]]
            o += CONST_W[k]
        self.CB = sb("CB", [128, NCONSTB], BF16)
        self.identb = self.CB[:, 0:128]
        self.onesb = self.CB[:, 128:256]
        self.masknegb = self.CB[:, 256:384]
        self.blk64b = self.CB[:, 384:512]
        self.hselb = self.CB[:, 512:514]
        self.colp = sb("colp", [128, NCOLP])
        self.colx = sb("colx", [128, 2])
        self.growt = sb("growt", [128, D])
        self.brow = sb("brow", [1, 2048], BF16)
        self.wsm = sb("wsm", [128, 512], BF16)
        self.memT = sb("memT", [128, 8, 256], BF16)
        self.N16, self.N32 = 12416, 5440
        self.ar16 = sb("ar16", [128, self.N16], BF16)
        self.ar32 = sb("ar32", [128, self.N32])
        self.bar = sb("bar", [128, 1])
        self.o16 = self.o32 = 0
        self.xn = sb("xn", [128, D], BF16)
        self.col1 = sb("col1", [128, 8])
        self.epsc = sb("epsc", [128, 2])
        self.t32a = sb("t32a", [128, 512])
        self.t32b = sb("t32b", [128, 512])
        self.t32c = sb("t32c", [128, 512])
        self.pj = [ps("pj0", [128, 512]), ps("pj1", [128, 512])]
        self.tp = ps("tp", [128, 1024], BF16)
        self.tpf = ps("tpf", [128, 512])
        self.sc = [ps("sc0", [128, 512]), ps("sc1", [128, 512])]
        self.acc = ps("acc", [128, 512])
        self.stp = ps("stp", [128, 512])
        self.pji = 0

        self.plan_weights()
        self.setup()
        for l in range(self.nl):
            self.layer(l)
        for tt in range(16):
            P.dma(lambda e, tt=tt: e.dma_start(out=self.out_d[128 * tt:128 * tt + 128, :], in_=self.X[:, tt, :]),
                  r=[("X", tt)], w=[("out", tt)], q="sp")
        P.finish(["out"] + self.outs)
        P.emit(self.es)
        assert self.wused == len(self.wq) == self.wreleased, (self.wused, len(self.wq), self.wreleased)

    def plan_weights(self):
        for l in range(self.nl):
            wi = self.w_in[l]
            for sg in range(NSEG):
                if "A" in self.mixers:
                    self.wplan("A_fm", wi[:, 0:512], 512)
                    self.wplan("A_v", wi[:, OFF["a_v"]:OFF["a_v"] + 256], 256)
                    self.wplan("A_z", wi[:, OFF["a_z"]:OFF["a_z"] + 256], 256)
                    self.wplan("A_g", self.wg_d[l][:, 0:256], 256)
                if "B" in self.mixers:
                    self.wplan("B", wi[:, OFF["b_x"]:OFF["b_x"] + 512], 512)
                if "C" in self.mixers:
                    self.wplan("C_z", wi[:, OFF["c_z"]:OFF["c_z"] + 256], 256)
                    self.wplan("C_rk", wi[:, OFF["c_r"]:OFF["c_r"] + 512], 512)
                    self.wplan("C_vw", wi[:, OFF["c_v"]:OFF["c_v"] + 384], 384)
                if "D" in self.mixers:
                    self.wplan("D_qk", wi[:, OFF["d_q"]:OFF["d_q"] + 512], 512)
                    self.wplan("D_v", wi[:, OFF["d_v"]:OFF["d_v"] + 256], 256)
                    self.wplan("D_z", wi[:, OFF["d_z"]:OFF["d_z"] + 256], 256)
                    self.wplan("D_g", self.wg_d[l][:, 256:384], 128)
                self.wplan("O0", self.w_out[l][:, 0:512], 512)
                self.wplan("O1", self.w_out[l][:, 512:1024], 512)
            if self.xattn:
                for i in range(4):
                    self.wplan("KV%d" % i, self.wkv_d[l][:, 512 * i:512 * i + 512], 512)
                for sg in range(NSEG):
                    self.wplan("Q0", self.wq_d[l][:, 0:512], 512)
                    self.wplan("Q1", self.wq_d[l][:, 512:1024], 512)
                    self.wplan("XO0", self.wo_d[l][:, 0:512], 512)
                    self.wplan("XO1", self.wo_d[l][:, 512:1024], 512)

    def nextpj(self):
        i = self.pji
        self.pji ^= 1
        return self.pj[i], ("pj", i)

    def rstd_col(self, out, ss, n, eps, rkeys, wkey):
        P = self.P
        P.act(lambda e: e.activation(out=out, in_=ss, func=AF.Sqrt, scale=1.0 / n, bias=self.epsc[:, 0:1] if eps == 1e-6 else self.epsc[:, 1:2]),
              r=rkeys + ["epsc"], w=[wkey])
        P.dve(lambda e: e.reciprocal(out=out, in_=out), r=[wkey], w=[wkey])

    def transpose_to(self, src_ap, src_key, dst_ap, dst_key, n, evac="dve"):
        P = self.P
        for i in range(n):
            P.pe(lambda e, i=i: e.transpose(self.tp[:, 128 * i:128 * i + 128], src_ap(i), self.identb),
                 r=[src_key, "CB"], w=["tp"])
        if evac == "dve":
            P.dve(lambda e: e.tensor_copy(out=dst_ap, in_=self.tp[:, 0:128 * n]), r=["tp"], w=[dst_key])
        else:
            P.act(lambda e: e.activation(out=dst_ap, in_=self.tp[:, 0:128 * n], func=AF.Copy), r=["tp"], w=[dst_key])

    def setup(self):
        P = self.P
        P.dma(lambda e: e.dma_start(out=self.CF[:], in_=self.consts_d[:, :]), w=["CF"])
        for tt in range(16):
            P.dma(lambda e, tt=tt: e.dma_start(out=self.X[:, tt, :], in_=self.x_d[128 * tt:128 * tt + 128, :]),
                  w=[("X", tt)], q=("sp" if tt % 2 == 0 else "act"))
        c = self.cf
        P.dve(lambda e: e.memset(self.epsc[:, 0:1], 1e-6), w=["epsc"])
        P.dve(lambda e: e.memset(self.epsc[:, 1:2], 64e-5), w=["epsc"])
        P.dma(lambda e: e.dma_start(out=self.CB[:], in_=self.constsb_d[:, :]), w=["CB"], q="pool")
        P.dma(lambda e: e.dma_start(out=self.growt[:], in_=self.memg[0:1, :].partition_broadcast(128)), w=["growt"])
        for mt in range(2):
            xin = self.t32a
            P.dma(lambda e, mt=mt: e.dma_start(out=self.t32a[:], in_=self.mem_d[128 * mt:128 * mt + 128, 0:512]), w=["t32a"])
            P.dma(lambda e, mt=mt: e.dma_start(out=self.t32b[:], in_=self.mem_d[128 * mt:128 * mt + 128, 512:1024]), w=["t32b"])
            P.act(lambda e: e.activation(out=self.t32c[:], in_=self.t32a[:], func=AF.Square, accum_out=self.col1[:, 0:1]),
                  r=["t32a"], w=["t32c", "col1"])
            P.act(lambda e: e.activation(out=self.t32c[:], in_=self.t32b[:], func=AF.Square, accum_out=self.col1[:, 1:2]),
                  r=["t32b"], w=["t32c", "col1"])
            P.dve(lambda e: e.tensor_tensor(out=self.col1[:, 2:3], in0=self.col1[:, 0:1], in1=self.col1[:, 1:2], op=ALU.add),
                  r=["col1"], w=["col1"])
            self.rstd_col(self.col1[:, 3:4], self.col1[:, 2:3], D, 1e-6, ["col1"], "col1")
            P.dve(lambda e: e.scalar_tensor_tensor(out=self.xn[:, 0:512], in0=self.t32a[:], scalar=self.col1[:, 3:4],
                                                   in1=self.growt[:, 0:512], op0=ALU.mult, op1=ALU.mult),
                  r=["t32a", "col1", "growt"], w=["xn"])
            P.dve(lambda e: e.scalar_tensor_tensor(out=self.xn[:, 512:1024], in0=self.t32b[:], scalar=self.col1[:, 3:4],
                                                   in1=self.growt[:, 512:1024], op0=ALU.mult, op1=ALU.mult),
                  r=["t32b", "col1", "growt"], w=["xn"])
            self.transpose_to(lambda i: self.xn[:, 128 * i:128 * i + 128], "xn",
                              self.memT[:, :, 128 * mt:128 * mt + 128],
                              "memT", 8)
        self.dump("memT", self.memT[:], [128, 8, 256], "memT")
        if self.mixers != "ABCD":
            P.dve(lambda e: e.memset(self.yT[:], 0.0), w=["yT"])

    def layer(self, l):
        P = self.P
        P.dma(lambda e: e.dma_start(out=self.colp[:], in_=self.colp_d[l]), w=["colp"])
        P.dma(lambda e: e.dma_start(out=self.colx[:], in_=self.colx_d[l]), w=["colx"])
        P.dma(lambda e: e.dma_start(out=self.brow[:], in_=self.brow_d[l]), w=["brow"], q="pool")
        P.dma(lambda e: e.dma_start(out=self.wsm[:], in_=self.wsm_d[l]), w=["wsm"], q="pool")
        P.dma(lambda e: e.dma_start(out=self.growt[:], in_=self.post_g[l:l + 1, :].partition_broadcast(128)), w=["growt"])
        for sg in range(NSEG):
            self.norm_T(l, sg, 0)
            if l == 0 and sg == 0:
                self.dump("hT0", self.hT[:], [128, 8, SEG], "hT")
            if "A" in self.mixers:
                self.mixer_A(l, sg)
            if "B" in self.mixers:
                self.mixer_B(l, sg)
            if "C" in self.mixers:
                self.mixer_C(l, sg)
            if "D" in self.mixers:
                self.mixer_D(l, sg)
            if l == 0:
                self.dump("yT%d" % sg, self.yT[:], [128, 8, SEG], "yT")
            self.out_proj(sg, self.yT, "yT", "O0", "O1")
        if l == 0:
            self.dump("x1", self.X[:], [128, 16, D], "X")
        if self.xattn:
            P.dma(lambda e: e.dma_start(out=self.growt[:], in_=self.xpost_g[l:l + 1, :].partition_broadcast(128)), w=["growt"])
            self.xattn_kv()
            for sg in range(NSEG):
                self.norm_T(l, sg, 8)
                self.xattn_seg(sg)
                self.out_proj(sg, self.hT, "hT", "XO0", "XO1")
            if l == 0:
                self.dump("x2", self.X[:], [128, 16, D], "X")

    def norm_T(self, l, sg, gcol):
        P = self.P
        for t in range(4):
            tt = 4 * sg + t
            P.act(lambda e, tt=tt: e.activation(out=self.t32a[:], in_=self.X[:, tt, 0:512], func=AF.Square,
                                                accum_out=self.col1[:, 0:1]), r=[("X", tt)], w=["t32a", "col1"])
            P.act(lambda e, tt=tt: e.activation(out=self.t32a[:], in_=self.X[:, tt, 512:1024], func=AF.Square,
                                                accum_out=self.col1[:, 1:2]), r=[("X", tt)], w=["t32a", "col1"])
            P.dve(lambda e: e.tensor_tensor(out=self.col1[:, 2:3], in0=self.col1[:, 0:1], in1=self.col1[:, 1:2], op=ALU.add),
                  r=["col1"], w=["col1"])
            self.rstd_col(self.col1[:, 3:4], self.col1[:, 2:3], D, 1e-6, ["col1"], "col1")
            P.dve(lambda e, tt=tt: e.tensor_scalar(out=self.xn[:], in0=self.X[:, tt, :], scalar1=self.col1[:, 3:4],
                                                   scalar2=None, op0=ALU.mult), r=[("X", tt), "col1"], w=["xn"])
            for i in range(8):
                P.pe(lambda e, i=i: e.transpose(self.tp[:, 128 * i:128 * i + 128], self.xn[:, 128 * i:128 * i + 128],
                                                self.identb), r=["xn", "CB"], w=["tp"])
            gc = self.colp[:, gcol:gcol + 8]
            P.dve(lambda e, t=t, gc=gc: e.tensor_tensor(
                out=self.hT[:, :, 128 * t:128 * t + 128], in0=self.tp[:, :].rearrange("p (a b) -> p a b", b=128),
                in1=gc.unsqueeze(2).to_broadcast([128, 8, 128]), op=ALU.mult), r=["tp", "colp"], w=["hT"])

    def out_proj(self, sg, srcT, skey, tag0, tag1):
        P = self.P
        w0, k0 = self.wnext(tag0)
        w1, k1 = self.wnext(tag1)
        for t in range(4):
            tt = 4 * sg + t
            banks = (self.pj, "pj") if t % 2 == 0 else (self.sc, "sc")
            c0 = 4 * (t % 2)
            ck = ("col1", t % 2)
            for half, (w, k) in enumerate(((w0, k0), (w1, k1))):
                pj, pk = banks[0][half], (banks[1], half)
                for dc in range(8):
                    P.pe(lambda e, dc=dc, w=w, pj=pj, t=t: e.matmul(pj[:], lhsT=srcT[:, dc, 128 * t:128 * t + 128],
                                                                    rhs=w[:, dc, :], start=(dc == 0), stop=(dc == 7)),
                         r=[skey, k], w=[pk])
                P.act(lambda e, pj=pj, half=half, c0=c0: e.activation(out=self.t32a[:], in_=pj[:], func=AF.Square,
                                                                      accum_out=self.col1[:, c0 + half:c0 + half + 1]),
                      r=[pk], w=["t32a", ck])
            P.dve(lambda e, c0=c0: e.tensor_tensor(out=self.col1[:, c0 + 2:c0 + 3], in0=self.col1[:, c0:c0 + 1],
                                                   in1=self.col1[:, c0 + 1:c0 + 2], op=ALU.add), r=[ck], w=[ck])
            self.rstd_col(self.col1[:, c0 + 3:c0 + 4], self.col1[:, c0 + 2:c0 + 3], D, 1e-6, [ck], ck)
            for half in range(2):
                pj, pk = banks[0][half], (banks[1], half)
                tmp = self.t32b if half == 0 else self.t32c
                tk = "t32b" if half == 0 else "t32c"
                P.dve(lambda e, pj=pj, tmp=tmp, half=half, c0=c0: e.scalar_tensor_tensor(
                    out=tmp[:], in0=pj[:], scalar=self.col1[:, c0 + 3:c0 + 4], in1=self.growt[:, 512 * half:512 * half + 512],
                    op0=ALU.mult, op1=ALU.mult), r=[pk, ck, "growt"], w=[tk])
                P.dve(lambda e, tmp=tmp, half=half, tt=tt: e.tensor_tensor(
                    out=self.X[:, tt, 512 * half:512 * half + 512], in0=self.X[:, tt, 512 * half:512 * half + 512],
                    in1=tmp[:], op=ALU.add), r=[tk, ("X", tt)], w=[("X", tt)])
        self.wrel(2)

    def proj_fm(self, w, wk, c0, out_fn, bias_col, n=SEG, func=AF.Identity):
        P = self.P
        pj, pk = self.nextpj()
        for dc in range(8):
            P.pe(lambda e, dc=dc: e.matmul(pj[:, 0:n], lhsT=w[:, dc, c0:c0 + 128], rhs=self.hT[:, dc, 0:n],
                                           start=(dc == 0), stop=(dc == 7)), r=["hT", wk], w=[pk])
        out_ap, out_key = out_fn
        P.act(lambda e: e.activation(out=out_ap, in_=pj[:, 0:n], func=func, bias=bias_col), r=[pk, "colp"], w=[out_key])

    def proj_tm(self, w, wk, ncols, t, b0):
        P = self.P
        pj, pk = self.nextpj()
        for dc in range(8):
            P.pe(lambda e, dc=dc: e.matmul(pj[:, 0:ncols], lhsT=self.hT[:, dc, 128 * t:128 * t + 128],
                                           rhs=w[:, dc, 0:ncols], start=(dc == 0), stop=False), r=["hT", wk], w=[pk])
        P.pe(lambda e: e.matmul(pj[:, 0:ncols], lhsT=self.onesb[0:1, :], rhs=self.brow[0:1, b0:b0 + ncols],
                                start=False, stop=True), r=["CB", "brow"], w=[pk])
        return pj, pk

    def silu_gate(self, z_ap, zkey, out32, okey):
        P = self.P
        P.act(lambda e: e.activation(out=out32, in_=z_ap, func=AF.Sigmoid), r=[zkey], w=[okey])
        P.dve(lambda e: e.tensor_tensor(out=out32, in0=out32, in1=z_ap, op=ALU.mult), r=[zkey, okey], w=[okey])

    def y_to_yT(self, ybf, ykey, t, c0):
        self.transpose_to(lambda i: ybf[:, 128 * i:128 * i + 128], ykey,
                          self.yT[:, c0:c0 + 2, 128 * t:128 * t + 128], "yT", 2, evac="act")

    def gate_rows(self, w, wk, c0, bias_col, out_ap, okey):
        self.proj_fm(w, wk, c0, (out_ap, okey), bias_col)

    def softplus_neg(self, buf, key):
        P = self.P
        P.act(lambda e: e.activation(out=buf, in_=buf, func=AF.Exp, scale=-1.0), r=[key], w=[key])
        P.act(lambda e: e.activation(out=buf, in_=buf, func=AF.Ln, bias=1.0), r=[key], w=[key])

    def alloc_A(self):
        if hasattr(self, "A_halo"):
            return
        sb = self.sb
        self.A_halo = sb("A_halo", [128, 4, 16], BF16)
        self.A_M = sb("A_M", [128, 8])
        self.A_neg30 = sb("A_neg30", [128, 4])
        self.A_Fc = sb("A_Fc", [128, 1])
        self.A_G = sb("A_G", [128, 2, 65])
        self.A_Gb = sb("A_Gb", [128, 4, 65], BF16)
        P = self.P
        P.pool(lambda e: e.memset(self.A_neg30[:], -1e30), w=["A_neg30"])
        P.pool(lambda e: e.memset(self.A_Gb[:], 0.0), w=["A_Gb"])

    def scratch_A(self, l):
        self.scr_reset()
        scr = self.scr
        self.A_raw = scr("A_raw", [128, 4, 16 + SEG], BF16)
        self.A_qk = scr("A_qk", [128, 4, SEG], BF16)
        self.A_qm = scr("A_qm", [128, 4, SEG], BF16)
        self.A_v = scr("A_v", [128, 4, 4, 65], BF16)
        self.A_vs2 = [scr("A_vs%d" % i, [128, 4, 65], BF16) for i in range(2)]
        self.A_z = scr("A_z", [128, 4, 256], BF16)
        self.A_ktm = scr("A_ktm", [128, 4, 256], BF16)
        self.A_PT2 = [scr("A_PT%d" % i, [128, 4, 128], BF16) for i in range(2)]
        self.A_y = scr("A_y", [128, 256], BF16)
        self.A_negF = scr("A_negF", [128, SEG])
        self.A_u = scr("A_u", [128, SEG])
        self.A_er = scr("A_er", [128, SEG])
        self.A_cr = scr("A_cr", [128, SEG])
        self.A_h = scr("A_h", [128, 4, 64])
        self.A_sq = scr("A_sq", [128, 4, 64])
        self.A_gz = scr("A_gz", [128, 256])
        self.A_cm = scr("A_cm", [128, 4])
        self.A_ecol = scr("A_ecol", [128, 4, 4])
        self.A_ccol = scr("A_ccol", [128, 4, 4])
        self.A_drow = scr("A_drow", [128, 4])
        self.A_dec = scr("A_dec", [128, 2, 4])
        self.A_dn = scr("A_dn", [128, 8])
        self.rowA = scr("rowA", [128, 256])
        P = self.P
        P.dma(lambda e: e.dma_start(out=self.rowA, in_=self.rowp_d[l][:, 0:256].partition_broadcast(128)), w=["rowA"])
        P.pool(lambda e: e.memset(self.A_v, 1.0), w=["A_v"])
        P.pool(lambda e: e.memset(self.A_qm, 0.0), w=["A_qm"])

    def mixer_A(self, l, sg):
        P = self.P
        self.alloc_A()
        self.scratch_A(l)
        cf = self.cf
        wfm, kfm = self.wnext("A_fm")
        if sg == 0:
            P.dve(lambda e: e.memset(self.A_raw[:, :, 0:16], 0.0), w=["A_raw"])
            P.dve(lambda e: e.memset(self.A_M[:, 0:1], -1e30), w=["A_M"])
            P.dve(lambda e: e.memset(self.A_Fc[:], 0.0), w=["A_Fc"])
            P.dve(lambda e: e.memset(self.A_G[:], 0.0), w=["A_G"])
        else:
            P.dve(lambda e: e.tensor_copy(out=self.A_raw[:, :, 0:16], in_=self.A_halo[:]), r=["A_halo"], w=["A_raw"])
        for j in range(4):
            self.proj_fm(wfm, kfm, 128 * j, (self.A_raw[:, j, 16:16 + SEG], "A_raw"), self.colp[:, 16 + j:17 + j])
        P.dve(lambda e: e.tensor_copy(out=self.A_halo[:], in_=self.A_raw[:, :, SEG:SEG + 16]), r=["A_raw"], w=["A_halo"])
        self.wrel()
        for j in range(4):
            cw = lambda k, j=j: self.colp[:, 32 + 4 * j + k:33 + 4 * j + k]
            P.dve(lambda e, j=j, cw=cw: e.tensor_scalar(out=self.t32a[:], in0=self.A_raw[:, j, 13:13 + SEG], scalar1=cw(0),
                                                        scalar2=None, op0=ALU.mult), r=["A_raw", "colp"], w=["t32a"])
            for k in range(1, 4):
                P.dve(lambda e, j=j, k=k, cw=cw: e.scalar_tensor_tensor(
                    out=self.t32a[:], in0=self.A_raw[:, j, 13 + k:13 + k + SEG], scalar=cw(k), in1=self.t32a[:],
                    op0=ALU.mult, op1=ALU.add), r=["A_raw", "colp", "t32a"], w=["t32a"])
            P.act(lambda e: e.activation(out=self.t32b[:], in_=self.t32a[:], func=AF.Sigmoid), r=["t32a"], w=["t32b"])
            sc_ = 0.125 if j < 2 else 1.0
            P.dve(lambda e, j=j, sc_=sc_: e.scalar_tensor_tensor(out=self.A_qk[:, j, :], in0=self.t32a[:], scalar=sc_,
                                                                 in1=self.t32b[:], op0=ALU.mult, op1=ALU.mult),
                  r=["t32a", "t32b"], w=["A_qk"])
        for hl in range(2):
            po = 64 * hl
            P.dve(lambda e, hl=hl, po=po: e.tensor_copy(out=self.A_qm[po:po + 64, hl::2, :], in_=self.A_qk[po:po + 64, 0:2, :]),
                  r=["A_qk"], w=["A_qm"])
        if l == 0 and sg == 0:
            self.dump("A_qk", self.A_qk[:], [128, 4, SEG], "A_qk")
        wv, kv = self.wnext("A_v")
        wz, kz = self.wnext("A_z")
        for t in range(4):
            pj, pk = self.proj_tm(wv, kv, 256, t, 0)
            P.act(lambda e, pj=pj, t=t: e.activation(out=self.A_v[:, t, :, 0:64],
                                                     in_=pj[:, 0:256].rearrange("p (h d) -> p h d", d=64), func=AF.Copy),
                  r=[pk], w=["A_v"])
            pj, pk = self.proj_tm(wz, kz, 256, t, 256)
            P.act(lambda e, pj=pj, t=t: e.activation(out=self.A_z[:, t, :], in_=pj[:, 0:256], func=AF.Copy),
                  r=[pk], w=["A_z"])
        self.wrel(2)
        wg, kg = self.wnext("A_g")
        self.gate_rows(wg, kg, 0, self.colp[:, 29:30], self.A_u[:], "A_u")
        self.gate_rows(wg, kg, 128, self.colp[:, 30:31], self.t32a[:], "t32a")
        self.wrel()
        self.softplus_neg(self.t32a[:], "t32a")
        P.dve(lambda e: e.memset(self.t32b[:], 1.0), w=["t32b"])
        P.dve(lambda e: e.tensor_tensor_scan(out=self.A_negF[:], data0=self.t32b[:], data1=self.t32a[:],
                                             initial=self.A_Fc[:, 0:1], op0=ALU.mult, op1=ALU.add),
              r=["t32a", "t32b", "A_Fc"], w=["A_negF"])
        P.dve(lambda e: e.tensor_copy(out=self.A_Fc[:], in_=self.A_negF[:, SEG - 1:SEG]), r=["A_negF"], w=["A_Fc"])
        P.dve(lambda e: e.tensor_tensor(out=self.A_u[:], in0=self.A_u[:], in1=self.A_negF[:], op=ALU.add),
              r=["A_u", "A_negF"], w=["A_u"])
        P.dve(lambda e: e.tensor_reduce(out=self.A_cm[:], in_=self.A_u[:].rearrange("p (c s) -> p c s", s=128),
                                        axis=AX.X, op=ALU.max), r=["A_u"], w=["A_cm"])
        P.dve(lambda e: e.tensor_tensor_scan(out=self.A_M[:, 1:5], data0=self.A_neg30[:], data1=self.A_cm[:],
                                             initial=self.A_M[:, 0:1], op0=ALU.max, op1=ALU.max),
              r=["A_neg30", "A_cm", "A_M"], w=["A_M"])
        P.dve(lambda e: e.tensor_tensor(out=self.A_drow[:], in0=self.A_M[:, 0:4], in1=self.A_M[:, 1:5], op=ALU.subtract),
              r=["A_M"], w=["A_drow"])
        P.act(lambda e: e.activation(out=self.A_drow[:], in_=self.A_drow[:], func=AF.Exp), r=["A_drow"], w=["A_drow"])
        Mb = self.A_M[:, 1:5].unsqueeze(2).to_broadcast([128, 4, 128])
        P.dve(lambda e: e.tensor_tensor(out=self.A_er[:].rearrange("p (c s) -> p c s", s=128),
                                        in0=self.A_u[:].rearrange("p (c s) -> p c s", s=128), in1=Mb, op=ALU.subtract),
              r=["A_u", "A_M"], w=["A_er"])
        P.act(lambda e: e.activation(out=self.A_er[:], in_=self.A_er[:], func=AF.Exp), r=["A_er"], w=["A_er"])
        P.dve(lambda e: e.tensor_tensor(out=self.A_cr[:].rearrange("p (c s) -> p c s", s=128),
                                        in0=self.A_negF[:].rearrange("p (c s) -> p c s", s=128), in1=Mb, op=ALU.subtract),
              r=["A_negF", "A_M"], w=["A_cr"])
        P.act(lambda e: e.activation(out=self.A_cr[:], in_=self.A_cr[:], func=AF.Exp), r=["A_cr"], w=["A_cr"])
        if l == 0 and sg == 0:
            self.dump("A_u", self.A_u[:], [128, SEG], "A_u")
            self.dump("A_negF", self.A_negF[:], [128, SEG], "A_negF")
            self.dump("A_M", self.A_M[:, 0:5], [128, 5], "A_M")
            self.dump("A_er", self.A_er[:], [128, SEG], "A_er")
            self.dump("A_drow", self.A_drow[:], [128, 4], "A_drow")
        P.dve(lambda e: e.tensor_copy(out=self.A_M[:, 0:1], in_=self.A_M[:, 4:5]), r=["A_M"], w=["A_M"])
        for t in range(4):
            P.pe(lambda e, t=t: e.transpose(self.tpf[:, 0:128], self.A_er[:, 128 * t:128 * t + 128], cf["ident"]),
                 r=["A_er", "CF"], w=["tpf"])
            P.pe(lambda e, t=t: e.transpose(self.tpf[:, 128:256], self.A_cr[:, 128 * t:128 * t + 128], cf["ident"]),
                 r=["A_cr", "CF"], w=["tpf"])
            P.dve(lambda e, t=t: e.tensor_copy(out=self.A_ecol[:, t, :], in_=self.tpf[:, 0:128:32]), r=["tpf"], w=["A_ecol"])
            P.dve(lambda e, t=t: e.tensor_copy(out=self.A_ccol[:, t, :], in_=self.tpf[:, 128:256:32]), r=["tpf"], w=["A_ccol"])
        for h in range(4):
            hp, hl = h // 2, h % 2
            P.pe(lambda e, h=h, hp=hp, hl=hl: e.matmul(self.tpf[64 * hl:64 * hl + 64, 256 + 4 * hp:260 + 4 * hp],
                                                       lhsT=cf["selb"][:, 64 * h:64 * h + 64],
                                                       rhs=self.A_drow[:, :], start=True, stop=True),
                 r=["CF", "A_drow"], w=["tpf"])
        P.dve(lambda e: e.tensor_copy(out=self.A_dec[:], in_=self.tpf[:, 256:264].rearrange("p (a c) -> p a c", c=4)),
              r=["tpf"], w=["A_dec"])
        if l == 0 and sg == 0:
            self.dump("A_ecol", self.A_ecol[:], [128, 4, 4], "A_ecol")
            self.dump("A_ccol", self.A_ccol[:], [128, 4, 4], "A_ccol")
            self.dump("A_dec", self.A_dec[:], [128, 2, 4], "A_dec")
        for t in range(4):
            self.transpose_to(lambda i, t=t: self.A_qk[:, 2 + i, 128 * t:128 * t + 128], "A_qk",
                              self.A_ktm[:, t, :], "A_ktm", 2, evac="act")
        for t in range(4):
            A_vs, kvs = self.A_vs2[t % 2], "A_vs%d" % (t % 2)
            A_PT, kpt = self.A_PT2[t % 2], "A_PT%d" % (t % 2)
            sc, sk = self.sc[t % 2], ("sc", t % 2)
            P.dve(lambda e, t=t, A_vs=A_vs, A_PT=A_PT, sc=sc: e.tensor_tensor(out=self.A_G[:], in0=self.A_G[:],
                                                 in1=self.A_dec[:, :, t:t + 1].to_broadcast([128, 2, 65]), op=ALU.mult),
                  r=["A_G", "A_dec"], w=["A_G"])
            for hl in range(2):
                po = 64 * hl
                P.act(lambda e, hl=hl, po=po, A_vs=A_vs, A_PT=A_PT, sc=sc: e.activation(out=self.A_Gb[po:po + 64, hl::2, :], in_=self.A_G[po:po + 64, :, :],
                                                           func=AF.Copy), r=["A_G"], w=["A_Gb"])
            P.dve(lambda e, t=t, A_vs=A_vs, A_PT=A_PT, sc=sc: e.tensor_tensor(out=A_vs, in0=self.A_v[:, t, :, :],
                                                 in1=self.A_ecol[:, t, :].unsqueeze(2).to_broadcast([128, 4, 65]),
                                                 op=ALU.mult), r=["A_v", "A_ecol"], w=[kvs])
            sc, sk = self.sc[t % 2], ("sc", t % 2)
            for h in range(4):
                hp, hl = h // 2, h % 2
                po = 64 * hl
                P.pe(lambda e, h=h, hp=hp, po=po, t=t, A_vs=A_vs, A_PT=A_PT, sc=sc: e.matmul(
                    sc[:, 128 * h:128 * h + 128], lhsT=self.A_qk[:, 2 + hp, 128 * t:128 * t + 128],
                    rhs=self.A_qm[:, h, 128 * t:128 * t + 128], start=True, stop=True), r=["A_qk", "A_qm"], w=[sk])
            P.dve(lambda e, A_vs=A_vs, A_PT=A_PT, sc=sc: e.tensor_tensor(out=A_PT, in0=sc[:, :].rearrange("p (h s) -> p h s", s=128),
                                            in1=cf["maskT"].unsqueeze(1).to_broadcast([128, 4, 128]), op=ALU.mult),
                  r=[sk, "CF"], w=[kpt])
            for h in range(4):
                hp, hl = h // 2, h % 2
                po = 64 * hl
                P.pe(lambda e, h=h, A_vs=A_vs, A_PT=A_PT, sc=sc: e.matmul(self.acc[:, 128 * h:128 * h + 65], lhsT=A_PT[:, h, :],
                                             rhs=A_vs[:, h, :], start=True, stop=False),
                     r=[kpt, kvs], w=["acc"])
                P.pe(lambda e, h=h, hp=hp, po=po, t=t, A_vs=A_vs, A_PT=A_PT, sc=sc: e.matmul(
                    self.acc[:, 128 * h:128 * h + 65], lhsT=self.A_qk[:, hp, 128 * t:128 * t + 128],
                    rhs=self.A_Gb[:, h, :], start=False, stop=True), r=["A_qk", "A_Gb"], w=["acc"])
                P.pe(lambda e, h=h, hp=hp, po=po, t=t, A_vs=A_vs, A_PT=A_PT, sc=sc: e.matmul(
                    self.stp[po:po + 64, 128 * hp:128 * hp + 65], lhsT=self.A_ktm[:, t, 64 * h:64 * h + 64],
                    rhs=A_vs[:, h, :], start=True, stop=True), r=["A_ktm", kvs], w=["stp"])
            P.dve(lambda e, A_vs=A_vs, A_PT=A_PT, sc=sc: e.tensor_tensor(out=self.A_G[:], in0=self.A_G[:],
                                            in1=self.stp[:, 0:256].rearrange("p (a c) -> p a c", c=128)[:, :, 0:65],
                                            op=ALU.add), r=["A_G", "stp"], w=["A_G"])
            accv = self.acc[:, :].rearrange("p (h c) -> p h c", c=128)
            P.dve(lambda e, A_vs=A_vs, A_PT=A_PT, sc=sc: e.tensor_copy(out=self.A_dn[:, 4:8], in_=accv[:, :, 64]), r=["acc"], w=["A_dn"])
            P.dve(lambda e, t=t, A_vs=A_vs, A_PT=A_PT, sc=sc: e.scalar_tensor_tensor(out=self.A_dn[:, 0:4], in0=self.A_dn[:, 4:8], scalar=-1.0,
                                                        in1=self.A_dn[:, 4:8], op0=ALU.mult, op1=ALU.max),
                  r=["A_dn"], w=["A_dn"])
            P.dve(lambda e, t=t, A_vs=A_vs, A_PT=A_PT, sc=sc: e.tensor_tensor(out=self.A_dn[:, 0:4], in0=self.A_dn[:, 0:4], in1=self.A_ccol[:, t, :],
                                                 op=ALU.max), r=["A_dn", "A_ccol"], w=["A_dn"])
            P.dve(lambda e, A_vs=A_vs, A_PT=A_PT, sc=sc: e.reciprocal(out=self.A_dn[:, 4:8], in_=self.A_dn[:, 0:4]), r=["A_dn"], w=["A_dn"])
            P.dve(lambda e, A_vs=A_vs, A_PT=A_PT, sc=sc: e.tensor_tensor(out=self.A_h[:], in0=accv[:, :, 0:64],
                                            in1=self.A_dn[:, 4:8].unsqueeze(2).to_broadcast([128, 4, 64]), op=ALU.mult),
                  r=["acc", "A_dn"], w=["A_h"])
            if l == 0 and sg == 0 and t == 1:
                self.dump("A_h", self.A_h[:], [128, 4, 64], "A_h")
                self.dump("A_G", self.A_G[:], [128, 2, 65], "A_G")
                self.dump(kpt, A_PT, [128, 4, 128], kpt)
                self.dump(kvs, A_vs, [128, 4, 65], kvs)
            P.dve(lambda e, A_vs=A_vs, A_PT=A_PT, sc=sc: e.tensor_tensor(out=self.A_sq[:], in0=self.A_h[:], in1=self.A_h[:], op=ALU.mult),
                  r=["A_h"], w=["A_sq"])
            P.dve(lambda e, A_vs=A_vs, A_PT=A_PT, sc=sc: e.tensor_reduce(out=self.A_dn[:, 0:4], in_=self.A_sq[:], axis=AX.X, op=ALU.add),
                  r=["A_sq"], w=["A_dn"])
            self.rstd_col(self.A_dn[:, 4:8], self.A_dn[:, 0:4], 64, 1e-6, ["A_dn"], "A_dn")
            self.silu_gate(self.A_z[:, t, :], "A_z", self.A_gz[:], "A_gz")
            P.dve(lambda e, A_vs=A_vs, A_PT=A_PT, sc=sc: e.tensor_tensor(out=self.A_gz[:], in0=self.A_gz[:], in1=self.rowA, op=ALU.mult),
                  r=["A_gz", "rowA"], w=["A_gz"])
            P.dve(lambda e, A_vs=A_vs, A_PT=A_PT, sc=sc: e.tensor_tensor(out=self.A_h[:], in0=self.A_h[:],
                                            in1=self.A_dn[:, 4:8].unsqueeze(2).to_broadcast([128, 4, 64]), op=ALU.mult),
                  r=["A_h", "A_dn"], w=["A_h"])
            P.dve(lambda e, A_vs=A_vs, A_PT=A_PT, sc=sc: e.tensor_tensor(out=self.A_y[:], in0=self.A_h[:].rearrange("p h d -> p (h d)"),
                                            in1=self.A_gz[:], op=ALU.mult), r=["A_h", "A_gz"], w=["A_y"])
            self.y_to_yT(self.A_y, "A_y", t, 0)

    def alloc_B(self):
        if hasattr(self, "B_halo"):
            return
        self.B_halo = self.sb("B_halo", [128, 2, 16])

    def scratch_B(self, l):
        self.scr_reset()
        scr = self.scr
        self.B_x = scr("B_x", [128, 2, 16 + SEG])
        self.B_s = scr("B_s0", [128, 16 + SEG])
        self.B_s2 = scr("B_s1", [128, 16 + SEG])
        self.B_p = scr("B_p", [128, 2, SEG], BF16)
        self.B_z = scr("B_z", [128, 256])
        self.B_y = scr("B_y", [128, 256], BF16)
        self.rowB = scr("rowB", [128, 256])
        self.P.dma(lambda e: e.dma_start(out=self.rowB, in_=self.rowp_d[l][:, 256:512].partition_broadcast(128)), w=["rowB"])

    def mixer_B(self, l, sg):
        P = self.P
        self.alloc_B()
        self.scratch_B(l)
        cf = self.cf
        w, wk = self.wnext("B")
        if sg == 0:
            P.dve(lambda e: e.memset(self.B_x[:, :, 0:16], 0.0), w=["B_x"])
        else:
            P.dve(lambda e: e.tensor_copy(out=self.B_x[:, :, 0:16], in_=self.B_halo[:]), r=["B_halo"], w=["B_x"])
        for j in range(2):
            self.proj_fm(w, wk, 128 * j, (self.B_x[:, j, 16:16 + SEG], "B_x"), self.colp[:, 20 + j:21 + j])
        P.dve(lambda e: e.tensor_copy(out=self.B_halo[:], in_=self.B_x[:, :, SEG:SEG + 16]), r=["B_x"], w=["B_halo"])
        wins = (2, 4, 8, 16)
        for j in range(2):
            x = self.B_x[:, j, :]
            bufs = [self.B_s, self.B_s2]
            cur = x
            lvl = {}
            step = 1
            for i in range(4):
                dst = bufs[i % 2]
                n = SEG + 16 - step if i == 0 else SEG + 16 - step
                P.dve(lambda e, dst=dst, cur=cur, step=step: e.memset(dst[:, 0:step], 0.0), w=["B_s%d" % (i % 2)])
                P.dve(lambda e, dst=dst, cur=cur, step=step: e.tensor_tensor(
                    out=dst[:, step:SEG + 16], in0=cur[:, step:SEG + 16], in1=cur[:, 0:SEG + 16 - step], op=ALU.add),
                    r=["B_x", "B_s0", "B_s1"], w=["B_s%d" % (i % 2)])
                for gl in range(2):
                    g = 2 * j + gl
                    if wins[g] == 2 * step:
                        win = wins[g]
                        po = 64 * gl
                        P.dve(lambda e, dst=dst, po=po, win=win, j=j: e.scalar_tensor_tensor(
                            out=self.B_p[po:po + 64, j, :], in0=dst[po:po + 64, 16:16 + SEG], scalar=1.0 / win,
                            in1=self.B_x[po:po + 64, j, 16:16 + SEG], op0=ALU.mult, op1=ALU.subtract),
                            r=["B_s%d" % (i % 2), "B_x"], w=["B_p"])
                        if sg == 0:
                            P.dve(lambda e, dst=dst, po=po, win=win: e.tensor_tensor(
                                out=self.t32a[po:po + 64, 0:win - 1], in0=dst[po:po + 64, 16:16 + win - 1],
                                in1=cf["invc"][po:po + 64, 0:win - 1], op=ALU.mult), r=["B_s%d" % (i % 2), "CF"], w=["t32a"])
                            P.dve(lambda e, po=po, win=win, j=j: e.tensor_tensor(
                                out=self.B_p[po:po + 64, j, 0:win - 1], in0=self.t32a[po:po + 64, 0:win - 1],
                                in1=self.B_x[po:po + 64, j, 16:16 + win - 1], op=ALU.subtract),
                                r=["t32a", "B_x"], w=["B_p"])
                cur = dst
                step *= 2
        wz, kz = w, wk
        for t in range(4):
            pj, pk = self.nextpj()
            for j in range(2):
                P.pe(lambda e, j=j, t=t, pj=pj: e.matmul(
                    pj[:, 128 * j:128 * j + 128], lhsT=self.B_p[:, j, 128 * t:128 * t + 128],
                    rhs=self.wsm[:, 128 * j:128 * j + 128], start=True, stop=True), r=["B_p", "wsm"], w=[pk])
            pz, pzk = self.nextpj()
            for dc in range(8):
                P.pe(lambda e, dc=dc, pz=pz, t=t: e.matmul(pz[:, 0:256], lhsT=self.hT[:, dc, 128 * t:128 * t + 128],
                                                           rhs=w[:, dc, 256:512], start=(dc == 0), stop=False),
                     r=["hT", wk], w=[pzk])
            P.pe(lambda e, pz=pz: e.matmul(pz[:, 0:256], lhsT=self.onesb[0:1, :], rhs=self.brow[0:1, 512:768],
                                           start=False, stop=True), r=["CB", "brow"], w=[pzk])
            P.act(lambda e, pz=pz: e.activation(out=self.B_z[:], in_=pz[:, 0:256], func=AF.Sigmoid), r=[pzk], w=["B_z"])
            P.dve(lambda e, pz=pz: e.tensor_tensor(out=self.B_z[:], in0=self.B_z[:], in1=pz[:, 0:256], op=ALU.mult),
                  r=[pzk, "B_z"], w=["B_z"])
            P.dve(lambda e: e.tensor_tensor(out=self.B_z[:], in0=self.B_z[:], in1=self.rowB, op=ALU.mult),
                  r=["B_z", "rowB"], w=["B_z"])
            P.dve(lambda e, pj=pj: e.tensor_tensor(out=self.B_y[:], in0=pj[:, 0:256], in1=self.B_z[:], op=ALU.mult),
                  r=[pk, "B_z"], w=["B_y"])
            self.y_to_yT(self.B_y, "B_y", t, 2)
        self.wrel()

    def alloc_C(self):
        if hasattr(self, "C_halo"):
            return
        self.C_halo = self.sb("C_halo", [128, 7, 2], BF16)
        self.C_S = self.sb("C_S", [128, 2, 3, 64], BF16)
        self.C_slot = [0, 0]

    def mixer_C(self, l, sg):
        P = self.P
        cf = self.cf
        self.alloc_C()
        self.scr_reset()
        scr = self.scr
        craw = scr("C_raw", [128, 514], BF16)
        tlw = scr("C_tlw", [128, 512], BF16)
        tla = scr("C_tla", [128, 512], BF16)
        arTm = scr("C_arTm", [128, 2, 2, 512], BF16)
        aTf = scr("C_aTf", [128, 512], BF16)
        bT = scr("C_bT", [128, 512], BF16)
        kT = scr("C_kT", [128, 512], BF16)
        tmpA = scr("C_tmpA", [128, 512], BF16)
        tmpB = scr("C_tmpB", [128, 512], BF16)
        a_tm = scr("C_atm", [128, 4, 128], BF16)
        v_tm = scr("C_vtm", [128, 4, 128], BF16)
        bhm = scr("C_bhm", [128, 4, 2, 128], BF16)
        khm = scr("C_khm", [128, 4, 2, 128], BF16)
        AT4 = scr("C_AT4", [128, 4, 128], BF16)
        X0 = scr("C_X0", [128, 128], BF16)
        XX = scr("C_XX", [128, 2, 256], BF16)
        Bb = scr("C_Bb", [128, 2, 128], BF16)
        R1T = scr("C_R1T", [128, 2, 128], BF16)
        Phi = scr("C_Phi", [128, 2, 2, 64], BF16)
        cz = scr("C_z", [128, 4, 256], BF16)
        C_y = scr("C_y", [128, 128], BF16)
        T = [scr("C_T%d" % i, [128, 512]) for i in range(8)]
        TK = ["C_T%d" % i for i in range(8)]
        T1, T2, T3, T4, T5, T6, T7, T8 = T
        K1, K2, K3, K4, K5, K6, K7, K8 = TK
        WT = scr("C_WT", [128, 8])
        Z64 = scr("C_Z64", [128, 64])
        bon = scr("C_bon", [128, 4, 2])
        gst = scr("C_gst", [128, 16])
        gy = scr("C_gy", [128, 2, 64])
        gq = scr("C_gq", [128, 2, 64])
        rowC = scr("C_row", [128, 512])
        P.dma(lambda e: e.dma_start(out=rowC, in_=self.rowp_d[l][:, 768:1280].partition_broadcast(128)), w=["C_row"])
        for ap_, k_ in ((tlw, "C_tlw"), (tla, "C_tla"), (arTm, "C_arTm"), (bhm, "C_bhm"), (khm, "C_khm"),
                        (R1T, "C_R1T"), (Phi, "C_Phi")):
            P.pool(lambda e, ap_=ap_: e.memset(ap_, 0.0), w=[k_])
        P.dve(lambda e: e.memset(Z64, 0.0), w=["C_Z64"])
        if sg == 0:
            P.dve(lambda e: e.memset(self.C_S[:], 0.0), w=["C_S"])
            self.C_slot = [0, 0]
        wz, kz = self.wnext("C_z")
        for t in range(4):
            pj, pk = self.proj_tm(wz, kz, 256, t, 768)
            P.act(lambda e, pj=pj, t=t: e.activation(out=cz[:, t, :], in_=pj[:, 0:256], func=AF.Copy), r=[pk], w=["C_z"])
        self.wrel()
        wrk, krk = self.wnext("C_rk")
        wvw, kvw = self.wnext("C_vw")

        def shifted(j, out32, okey):
            w, wk, c0 = (wrk, krk, 128 * j) if j < 4 else (wvw, kvw, 128 * (j - 4))
            if sg == 0:
                P.dve(lambda e: e.memset(craw[:, 0:1], 0.0), w=["C_raw"])
            else:
                P.dve(lambda e: e.tensor_copy(out=craw[:, 0:1], in_=self.C_halo[:, j, 0:1]), r=["C_halo"], w=["C_raw"])
            self.proj_fm(w, wk, c0, (craw[:, 1:513], "C_raw"), self.colp[:, 22 + j:23 + j])
            P.dve(lambda e: e.tensor_copy(out=self.C_halo[:, j, 0:1], in_=craw[:, 512:513]), r=["C_raw"], w=["C_halo"])
            P.dve(lambda e: e.tensor_tensor(out=out32, in0=craw[:, 0:512], in1=craw[:, 1:513], op=ALU.subtract),
                  r=["C_raw"], w=[okey])
            P.dve(lambda e: e.scalar_tensor_tensor(out=out32, in0=out32, scalar=self.colp[:, 48 + j:49 + j],
                                                   in1=craw[:, 1:513], op0=ALU.mult, op1=ALU.add),
                  r=["C_raw", "colp", okey], w=[okey])

        shifted(6, T1, K1)
        P.act(lambda e: e.activation(out=tlw[0:64, :], in_=T1[0:64, :], func=AF.Tanh), r=[K1], w=["C_tlw"])
        P.act(lambda e: e.activation(out=tla[64:128, :], in_=T1[64:128, :], func=AF.Copy), r=[K1], w=["C_tla"])
        for ct in range(2):
            W2 = self.wsm[:, 256 + 128 * ct:256 + 128 * ct + 128]
            pj, pk = self.nextpj()
            P.pe(lambda e, pj=pj, W2=W2: e.matmul(pj[:], lhsT=W2, rhs=tlw, start=True, stop=True), r=["wsm", "C_tlw"], w=[pk])
            P.act(lambda e, pj=pj, ct=ct: e.activation(out=T1, in_=pj[:], func=AF.Sigmoid, bias=self.colp[:, 55 + ct:56 + ct]),
                  r=[pk, "colp"], w=[K1])
            P.dve(lambda e: e.tensor_scalar(out=T1, in0=T1, scalar1=-0.6065306597126334, scalar2=None, op0=ALU.mult),
                  r=[K1], w=[K1])
            for c8 in range(8):
                P.dve(lambda e, c8=c8: e.tensor_tensor_scan(out=T2[:, 64 * c8:64 * c8 + 64], data0=Z64,
                                                            data1=T1[:, 64 * c8:64 * c8 + 64], initial=0.0,
                                                            op0=ALU.add, op1=ALU.add), r=[K1, "C_Z64"], w=[K2])
            P.dve(lambda e: e.tensor_tensor(out=T3, in0=T2, in1=T1, op=ALU.subtract), r=[K1, K2], w=[K3])
            P.act(lambda e: e.activation(out=T3, in_=T3, func=AF.Exp), r=[K3], w=[K3])
            P.act(lambda e: e.activation(out=WT, in_=T2[:, 63:512:64], func=AF.Exp), r=[K2], w=["C_WT"])
            P.act(lambda e: e.activation(out=T1, in_=T2, func=AF.Exp), r=[K2], w=[K1])
            P.act(lambda e: e.activation(out=T4, in_=T2, func=AF.Exp, scale=-1.0), r=[K2], w=[K4])
            P.dve(lambda e: e.tensor_tensor(out=T2.rearrange("p (c s) -> p c s", s=64),
                                            in0=T4.rearrange("p (c s) -> p c s", s=64),
                                            in1=WT.unsqueeze(2).to_broadcast([128, 8, 64]), op=ALU.mult),
                  r=[K4, "C_WT"], w=[K2])
            pj, pk = self.nextpj()
            P.pe(lambda e, pj=pj, W2=W2: e.matmul(pj[:], lhsT=W2, rhs=tla, start=True, stop=True), r=["wsm", "C_tla"], w=[pk])
            P.act(lambda e, pj=pj, ct=ct: e.activation(out=T5, in_=pj[:], func=AF.Sigmoid, bias=self.colp[:, 57 + ct:58 + ct]),
                  r=[pk, "colp"], w=[K5])
            shifted(2 + ct, T6, K6)
            P.dve(lambda e, ct=ct: e.tensor_scalar(out=T7, in0=T6, scalar1=self.colp[:, 59 + ct:60 + ct], scalar2=None,
                                                   op0=ALU.mult), r=[K6, "colp"], w=[K7])
            P.act(lambda e: e.activation(out=tmpA, in_=T7, func=AF.Square), r=[K7], w=["C_tmpA"])
            pj, pk = self.nextpj()
            P.pe(lambda e, pj=pj: e.matmul(pj[:], lhsT=self.blk64b, rhs=tmpA, start=True, stop=True), r=["CB", "C_tmpA"], w=[pk])
            P.act(lambda e, pj=pj: e.activation(out=T8, in_=pj[:], func=AF.Sqrt), r=[pk], w=[K8])
            P.dve(lambda e: e.tensor_scalar(out=T8, in0=T8, scalar1=1e-12, scalar2=None, op0=ALU.max), r=[K8], w=[K8])
            P.dve(lambda e: e.reciprocal(out=T8, in_=T8), r=[K8], w=[K8])
            P.dve(lambda e: e.tensor_tensor(out=T7, in0=T7, in1=T8, op=ALU.mult), r=[K7, K8], w=[K7])
            P.dve(lambda e: e.scalar_tensor_tensor(out=aTf, in0=T7, scalar=-1.0, in1=T3, op0=ALU.mult, op1=ALU.mult),
                  r=[K7, K3], w=["C_aTf"])
            for hl in range(2):
                po = 64 * hl
                P.act(lambda e, hl=hl, po=po: e.activation(out=arTm[po:po + 64, hl, 0, :], in_=aTf[po:po + 64, :], func=AF.Copy),
                      r=["C_aTf"], w=["C_arTm"])
            P.dve(lambda e: e.tensor_tensor(out=T8, in0=T7, in1=T5, op=ALU.mult), r=[K7, K5], w=[K8])
            P.dve(lambda e: e.tensor_tensor(out=bT, in0=T8, in1=T4, op=ALU.mult), r=[K8, K4], w=["C_bT"])
            P.dve(lambda e: e.tensor_tensor(out=tmpA, in0=T8, in1=T2, op=ALU.mult), r=[K8, K2], w=["C_tmpA"])

            def to_masked_tm(srcT, skey, dst, dkey):
                for t in range(4):
                    P.pe(lambda e, t=t: e.transpose(self.tp[:, 128 * t:128 * t + 128], srcT[:, 128 * t:128 * t + 128], self.identb),
                         r=[skey, "CB"], w=["tp"])
                for c in range(2):
                    P.act(lambda e, c=c: e.activation(out=dst[64 * c:64 * c + 64, :, c, :],
                                                      in_=self.tp[64 * c:64 * c + 64, 0:512].rearrange("p (t n) -> p t n", n=128),
                                                      func=AF.Copy), r=["tp"], w=[dkey])

            def to_tm(srcT, skey, dst, dkey):
                for t in range(4):
                    P.pe(lambda e, t=t: e.transpose(self.tp[:, 128 * t:128 * t + 128], srcT[:, 128 * t:128 * t + 128], self.identb),
                         r=[skey, "CB"], w=["tp"])
                P.act(lambda e: e.activation(out=dst, in_=self.tp[:, 0:512].rearrange("p (t n) -> p t n", n=128), func=AF.Copy),
                      r=["tp"], w=[dkey])

            to_masked_tm(tmpA, "C_tmpA", bhm, "C_bhm")
            P.dve(lambda e, ct=ct: e.tensor_scalar(out=T5, in0=T5, scalar1=1.0, scalar2=self.colp[:, 61 + ct:62 + ct],
                                                   op0=ALU.subtract, op1=ALU.mult), r=[K5, "colp"], w=[K5])
            P.dve(lambda e: e.scalar_tensor_tensor(out=T5, in0=T5, scalar=1.0, in1=T6, op0=ALU.add, op1=ALU.mult),
                  r=[K5, K6], w=[K5])
            P.dve(lambda e: e.tensor_tensor(out=kT, in0=T5, in1=T4, op=ALU.mult), r=[K5, K4], w=["C_kT"])
            P.dve(lambda e: e.tensor_tensor(out=tmpB, in0=T5, in1=T2, op=ALU.mult), r=[K5, K2], w=["C_tmpB"])
            to_masked_tm(tmpB, "C_tmpB", khm, "C_khm")
            to_tm(aTf, "C_aTf", a_tm, "C_atm")
            shifted(ct, T6, K6)
            for hl in range(2):
                po = 64 * hl
                P.dve(lambda e, hl=hl, po=po: e.tensor_tensor(out=arTm[po:po + 64, hl, 1, :], in0=T6[po:po + 64, :],
                                                              in1=T1[po:po + 64, :], op=ALU.mult), r=[K6, K1], w=["C_arTm"])
            P.dve(lambda e, ct=ct: e.scalar_tensor_tensor(out=tmpA, in0=T6, scalar=self.colx[:, ct:ct + 1], in1=T5,
                                                          op0=ALU.mult, op1=ALU.mult), r=[K6, K5, "colx"], w=["C_tmpA"])
            pj, pk = self.nextpj()
            for t in range(4):
                P.pe(lambda e, t=t, pj=pj: e.matmul(pj[:, 2 * t:2 * t + 2], lhsT=tmpA[:, 128 * t:128 * t + 128], rhs=self.hselb,
                                                    start=True, stop=True), r=["C_tmpA", "CB"], w=[pk])
            P.dve(lambda e, pj=pj: e.tensor_copy(out=bon, in_=pj[:, 0:8].rearrange("p (t h) -> p t h", h=2)), r=[pk], w=["C_bon"])
            shifted(4 + ct, T6, K6)
            P.act(lambda e: e.activation(out=tmpB, in_=T6, func=AF.Copy), r=[K6], w=["C_tmpB"])
            to_tm(tmpB, "C_tmpB", v_tm, "C_vtm")
            if l == 0 and sg == 0 and ct == 0:
                self.dump("C_aTf", aTf, [128, 512], "C_aTf")
                self.dump("C_bT", bT, [128, 512], "C_bT")
                self.dump("C_kT", kT, [128, 512], "C_kT")
                self.dump("C_arTm", arTm, [128, 2, 2, 512], "C_arTm")
                self.dump("C_bhm", bhm, [128, 4, 2, 128], "C_bhm")
                self.dump("C_khm", khm, [128, 4, 2, 128], "C_khm")
                self.dump("C_vtm", v_tm, [128, 4, 128], "C_vtm")
                self.dump("C_atm", a_tm, [128, 4, 128], "C_atm")
                self.dump("C_WT", WT, [128, 8], "C_WT")
                self.dump("C_bon", bon, [128, 4, 2], "C_bon")
            for t in range(4):
                ts_ = slice(128 * t, 128 * t + 128)
                s0 = self.C_slot[ct]
                Sin = [self.C_S[:, ct, (s0 + i) % 3, :] for i in range(3)]
                for hl in range(2):
                    po = 64 * hl
                    h = 2 * ct + hl
                    sc0, sk0 = self.sc[0], ("sc", 0)
                    sc1, sk1 = self.sc[1], ("sc", 1)
                    rhs_ar = arTm[:, hl, :, ts_]
                    P.pe(lambda e, rhs_ar=rhs_ar, ts_=ts_: e.matmul(sc0[:, 0:256], lhsT=bT[:, ts_], rhs=rhs_ar, start=True, stop=True),
                         r=["C_bT", "C_arTm"], w=[sk0])
                    P.pe(lambda e, rhs_ar=rhs_ar, ts_=ts_: e.matmul(sc0[:, 256:512], lhsT=kT[:, ts_], rhs=rhs_ar, start=True, stop=True),
                         r=["C_kT", "C_arTm"], w=[sk0])
                    P.pe(lambda e, hl=hl, ts_=ts_: e.matmul(sc1[:, 0:128], lhsT=arTm[:, hl, 0, ts_], rhs=bT[:, ts_], start=True, stop=True),
                         r=["C_bT", "C_arTm"], w=[sk1])
                    sc0v = sc0[:, :].rearrange("p (b n) -> p b n", n=128)
                    P.dve(lambda e, sc0v=sc0v: e.tensor_tensor(out=AT4[:, 0:4:2, :], in0=sc0v[:, 0:4:2, :],
                                                               in1=cf["mS"].unsqueeze(1).to_broadcast([128, 2, 128]), op=ALU.mult),
                          r=[sk0, "CF"], w=["C_AT4"])
                    P.dve(lambda e, sc0v=sc0v: e.tensor_tensor(out=AT4[:, 1:4:2, :], in0=sc0v[:, 1:4:2, :],
                                                               in1=cf["mI"].unsqueeze(1).to_broadcast([128, 2, 128]), op=ALU.mult),
                          r=[sk0, "CF"], w=["C_AT4"])
                    P.dve(lambda e: e.tensor_tensor(out=X0, in0=sc1[:, 0:128], in1=cf["mSt"], op=ALU.mult), r=[sk1, "CF"], w=["C_X0"])
                    P.pe(lambda e, t=t, po=po: e.matmul(self.stp[:, 0:64], lhsT=AT4[:, 2, :], rhs=v_tm[:, t, po:po + 64],
                                                        start=True, stop=True), r=["C_AT4", "C_vtm"], w=["stp"])
                    P.act(lambda e, t=t, po=po: e.activation(out=Bb[:, 0, 0:64], in_=a_tm[:, t, po:po + 64], func=AF.Copy),
                          r=["C_atm"], w=["C_Bb"])
                    P.act(lambda e: e.activation(out=Bb[:, 0, 64:128], in_=self.stp[:, 0:64], func=AF.Copy), r=["stp"], w=["C_Bb"])
                    Xc, XTc = X0, AT4[:, 0, :]
                    xk, xtk = "C_X0", "C_AT4"
                    for i in range(6):
                        bi, bo = i % 2, (i + 1) % 2
                        P.pe(lambda e, XTc=XTc, bi=bi: e.matmul(self.stp[:, 128:256], lhsT=XTc, rhs=Bb[:, bi, :], start=True, stop=True),
                             r=[xtk, "C_Bb"], w=["stp"])
                        P.dve(lambda e, bi=bi, bo=bo: e.tensor_tensor(out=Bb[:, bo, :], in0=self.stp[:, 128:256], in1=Bb[:, bi, :],
                                                                      op=ALU.add), r=["stp", "C_Bb"], w=["C_Bb"])
                        if i < 5:
                            P.pe(lambda e, XTc=XTc, Xc=Xc: e.matmul(sc1[:, 128:256], lhsT=XTc, rhs=Xc, start=True, stop=True),
                                 r=[xk, xtk], w=[sk1])
                            P.pe(lambda e, XTc=XTc, Xc=Xc: e.matmul(sc1[:, 256:384], lhsT=Xc, rhs=XTc, start=True, stop=True),
                                 r=[xk, xtk], w=[sk1])
                            xi = i % 2
                            P.act(lambda e, xi=xi: e.activation(out=XX[:, xi, :], in_=sc1[:, 128:384], func=AF.Copy),
                                  r=[sk1], w=[("C_XX", xi)])
                            Xc, XTc = XX[:, xi, 0:128], XX[:, xi, 128:256]
                            xk = xtk = ("C_XX", xi)
                    Pf = Bb[:, 0, :]
                    if l == 0 and sg == 0 and ct == 0 and t == 0 and hl == 0:
                        self.dump("C_AT4", AT4, [128, 4, 128], "C_AT4")
                        self.dump("C_X0", X0, [128, 128], "C_X0")
                        self.dump("C_Pf", Pf, [128, 128], "C_Bb")
                    P.pe(lambda e, po=po: e.matmul(self.stp[po:po + 64, 256:384], lhsT=Pf[:, 0:64], rhs=AT4[:, 1, :],
                                                   start=True, stop=True), r=["C_Bb", "C_AT4"], w=["stp"])
                    P.dve(lambda e, po=po, hl=hl, ts_=ts_: e.tensor_tensor(out=R1T[po:po + 64, hl, :], in0=self.stp[po:po + 64, 256:384],
                                                                           in1=arTm[po:po + 64, hl, 1, ts_], op=ALU.add),
                          r=["stp", "C_arTm"], w=["C_R1T"])
                    P.pe(lambda e, hl=hl: e.matmul(self.acc[:, 64 * hl:64 * hl + 64], lhsT=AT4[:, 1, :], rhs=Pf[:, 64:128],
                                                   start=True, stop=False, skip_group_check=True), r=["C_AT4", "C_Bb"], w=["acc"])
                    P.pe(lambda e, hl=hl, t=t, po=po: e.matmul(self.acc[:, 64 * hl:64 * hl + 64], lhsT=AT4[:, 3, :],
                                                               rhs=v_tm[:, t, po:po + 64], start=False, stop=False,
                                                               skip_group_check=True), r=["C_AT4", "C_vtm"], w=["acc"])
                    for c in range(2):
                        P.pe(lambda e, po=po, c=c, t=t: e.matmul(self.stp[po:po + 64, 384 + 64 * c:448 + 64 * c], lhsT=Pf[:, 0:64],
                                                                 rhs=bhm[:, t, c, po:po + 64], start=True, stop=True),
                             r=["C_Bb", "C_bhm"], w=["stp"])
                        P.dve(lambda e, po=po, c=c, hl=hl, t=t: e.scalar_tensor_tensor(
                            out=Phi[po:po + 64, hl, c, :], in0=cf["ident"][po:po + 64, po:po + 64],
                            scalar=WT[po:po + 64, 2 * t + c:2 * t + c + 1], in1=self.stp[po:po + 64, 384 + 64 * c:448 + 64 * c],
                            op0=ALU.mult, op1=ALU.add), r=["CF", "C_WT", "stp"], w=["C_Phi"])
                        P.pe(lambda e, hl=hl, c=c, Sin=Sin: e.matmul(self.acc[64 * c:64 * c + 64, 64 * hl:64 * hl + 64],
                                                            lhsT=R1T[:, hl, 64 * c:64 * c + 64], rhs=Sin[c], start=False,
                                                            stop=(c == 1), skip_group_check=True),
                             r=["C_R1T", ("C_S", ct)], w=["acc"])
                        P.pe(lambda e, po=po, hl=hl, c=c, Sin=Sin: e.matmul(self.tpf[po:po + 64, 64 * c:64 * c + 64], lhsT=Phi[:, hl, c, :],
                                                                   rhs=Sin[c], start=True, stop=False, skip_group_check=True),
                             r=["C_Phi", ("C_S", ct)], w=["tpf"])
                        P.pe(lambda e, po=po, c=c, t=t: e.matmul(self.tpf[po:po + 64, 64 * c:64 * c + 64], lhsT=bhm[:, t, c, po:po + 64],
                                                                 rhs=Pf[:, 64:128], start=False, stop=False, skip_group_check=True),
                             r=["C_bhm", "C_Bb"], w=["tpf"])
                        P.pe(lambda e, po=po, c=c, t=t: e.matmul(self.tpf[po:po + 64, 64 * c:64 * c + 64], lhsT=khm[:, t, c, po:po + 64],
                                                                 rhs=v_tm[:, t, po:po + 64], start=False, stop=True,
                                                                 skip_group_check=True), r=["C_khm", "C_vtm"], w=["tpf"])
                        P.act(lambda e, po=po, c=c, Sin=Sin: e.activation(out=Sin[c + 1][po:po + 64, :], in_=self.tpf[po:po + 64, 64 * c:64 * c + 64],
                                                                 func=AF.Copy), r=["tpf"], w=[("C_S", ct)])
                        if l == 0 and sg == 0 and ct == 0 and t == 0 and hl == 0 and c == 0:
                            P.act(lambda e: e.activation(out=gy.rearrange("p h d -> p (h d)")[0:64, 0:64], in_=self.tpf[0:64, 0:64], func=AF.Copy),
                                  r=["tpf"], w=["C_gy"])
                            self.dump("C_tpf", gy.rearrange("p h d -> p (h d)")[0:64, 0:64], [64, 64], "C_gy")
                            self.dump("C_S1", Sin[1][0:64, :], [64, 64], ("C_S", 0))
                if l == 0 and sg == 0 and ct == 0 and t == 0:
                    self.dump("C_Phi", Phi, [128, 2, 2, 64], "C_Phi")
                    self.dump("C_R1T", R1T, [128, 2, 128], "C_R1T")
                    self.dump("C_S", self.C_S[:, 0, :, :], [128, 3, 64], ("C_S", 0))
                    self.dump("C_Y", gy, [128, 2, 64], "C_gy") if False else None
                self.C_slot[ct] = (s0 + 2) % 3
                accv = self.acc[:, 0:128].rearrange("p (h d) -> p h d", d=64)
                P.dve(lambda e: e.tensor_reduce(out=gst[:, 0:2], in_=accv, axis=AX.X, op=ALU.add), r=["acc"], w=["C_gst"])
                P.act(lambda e: e.activation(out=gq, in_=accv, func=AF.Square), r=["acc"], w=["C_gq"])
                P.dve(lambda e: e.tensor_reduce(out=gst[:, 2:4], in_=gq, axis=AX.X, op=ALU.add), r=["C_gq"], w=["C_gst"])
                P.dve(lambda e: e.tensor_scalar(out=gst[:, 4:6], in0=gst[:, 0:2], scalar1=1.0 / 64, scalar2=None, op0=ALU.mult),
                      r=["C_gst"], w=["C_gst"])
                P.dve(lambda e: e.tensor_tensor(out=gst[:, 6:8], in0=gst[:, 4:6], in1=gst[:, 4:6], op=ALU.mult),
                      r=["C_gst"], w=["C_gst"])
                P.dve(lambda e: e.scalar_tensor_tensor(out=gst[:, 8:10], in0=gst[:, 2:4], scalar=1.0 / 64, in1=gst[:, 6:8],
                                                       op0=ALU.mult, op1=ALU.subtract), r=["C_gst"], w=["C_gst"])
                P.act(lambda e: e.activation(out=gst[:, 10:12], in_=gst[:, 8:10], func=AF.Sqrt, bias=self.epsc[:, 1:2]),
                      r=["C_gst", "epsc"], w=["C_gst"])
                P.dve(lambda e: e.reciprocal(out=gst[:, 10:12], in_=gst[:, 10:12]), r=["C_gst"], w=["C_gst"])
                P.dve(lambda e: e.tensor_tensor(out=gy, in0=accv, in1=gst[:, 4:6].unsqueeze(2).to_broadcast([128, 2, 64]),
                                                op=ALU.subtract), r=["acc", "C_gst"], w=["C_gy"])
                P.dve(lambda e: e.tensor_tensor(out=gy, in0=gy, in1=gst[:, 10:12].unsqueeze(2).to_broadcast([128, 2, 64]),
                                                op=ALU.mult), r=["C_gy", "C_gst"], w=["C_gy"])
                gy2 = gy.rearrange("p h d -> p (h d)")
                P.dve(lambda e, ct=ct: e.tensor_tensor(out=gy2, in0=gy2, in1=rowC[:, 128 * ct:128 * ct + 128], op=ALU.mult),
                      r=["C_gy", "C_row"], w=["C_gy"])
                P.dve(lambda e, ct=ct: e.tensor_tensor(out=gy2, in0=gy2, in1=rowC[:, 256 + 128 * ct:256 + 128 * ct + 128], op=ALU.add),
                      r=["C_gy", "C_row"], w=["C_gy"])
                P.dve(lambda e, t=t: e.tensor_tensor(out=gq, in0=v_tm[:, t, :].rearrange("p (h d) -> p h d", d=64),
                                                     in1=bon[:, t, :].unsqueeze(2).to_broadcast([128, 2, 64]), op=ALU.mult),
                      r=["C_vtm", "C_bon"], w=["C_gq"])
                P.dve(lambda e: e.tensor_tensor(out=gy, in0=gy, in1=gq, op=ALU.add), r=["C_gy", "C_gq"], w=["C_gy"])
                gq2 = gq.rearrange("p h d -> p (h d)")
                self.silu_gate(cz[:, t, 128 * ct:128 * ct + 128], "C_z", gq2, "C_gq")
                P.dve(lambda e: e.tensor_tensor(out=C_y, in0=gy2, in1=gq2, op=ALU.mult), r=["C_gy", "C_gq"], w=["C_y"])
                self.transpose_to(lambda i: C_y, "C_y", self.yT[:, 4 + ct:5 + ct, 128 * t:128 * t + 128], "yT", 1, evac="act")
        self.wrel(2)

    def alloc_D(self):
        if hasattr(self, "D_K"):
            return
        sb = self.sb
        self.D_K = sb("D_K", [65, 4, S], BF16)
        self.D_V = sb("D_V", [128, 16, 4, 65], BF16)
        self.D_Fc = sb("D_Fc", [128, 1])
        self.D_fcol = sb("D_fcol", [128, 16, 4])
        P = self.P
        P.dve(lambda e: e.memset(self.D_K[64:65, :, :], 1.0), w=["D_K"])
        P.dve(lambda e: e.memset(self.D_V[:], 1.0), w=["D_V"])

    def scratch_D(self, l):
        self.scr_reset()
        scr = self.scr
        self.D_Q = scr("D_Q", [65, 4, SEG], BF16)
        self.D_z = scr("D_z", [128, 4, 256], BF16)
        self.D_qn = scr("D_qn", [128, 512], BF16)
        self.D_y = scr("D_y", [128, 256], BF16)
        self.PT = scr("PT", [128, 2, 512], BF16)
        self.D_negF = scr("D_negF", [128, SEG])
        self.D_ss = scr("D_ss", [128, 16])
        self.D_o = scr("D_o", [128, 4, 4, 64])
        self.D_rd = scr("D_rd", [128, 4])
        self.rowD = scr("rowD", [128, 512])
        self.P.dma(lambda e: e.dma_start(out=self.rowD, in_=self.rowp_d[l][:, 1280:1792].partition_broadcast(128)), w=["rowD"])

    def mixer_D(self, l, sg):
        P = self.P
        self.alloc_D()
        self.scratch_D(l)
        cf = self.cf
        wqk, kqk = self.wnext("D_qk")
        if sg == 0:
            P.dve(lambda e: e.memset(self.D_Fc[:], 0.0), w=["D_Fc"])
        for t in range(4):
            tt = 4 * sg + t
            pj, pk = self.proj_tm(wqk, kqk, 512, t, 1024)
            P.act(lambda e, pj=pj: e.activation(out=self.t32a[:], in_=pj[:], func=AF.Copy), r=[pk], w=["t32a"])
            if KD <= 1:
                continue
            P.dve(lambda e: e.tensor_tensor(out=self.t32b[:], in0=self.t32a[:], in1=self.t32a[:], op=ALU.mult),
                  r=["t32a"], w=["t32b"])
            P.dve(lambda e: e.tensor_reduce(out=self.D_ss[:, 0:8], in_=self.t32b[:].rearrange("p (h d) -> p h d", d=64),
                                            axis=AX.X, op=ALU.add), r=["t32b"], w=["D_ss"])
            self.rstd_col(self.D_ss[:, 8:16], self.D_ss[:, 0:8], 64, 1e-6, ["D_ss"], "D_ss")
            P.dve(lambda e: e.tensor_scalar(out=self.D_ss[:, 8:12], in0=self.D_ss[:, 8:12], scalar1=0.125, scalar2=None,
                                            op0=ALU.mult), r=["D_ss"], w=["D_ss"])
            P.dve(lambda e: e.tensor_tensor(out=self.t32a[:].rearrange("p (h d) -> p h d", d=64),
                                            in0=self.t32a[:].rearrange("p (h d) -> p h d", d=64),
                                            in1=self.D_ss[:, 8:16].unsqueeze(2).to_broadcast([128, 8, 64]), op=ALU.mult),
                  r=["t32a", "D_ss"], w=["t32a"])
            P.dve(lambda e: e.tensor_tensor(out=self.D_qn[:], in0=self.t32a[:], in1=self.rowD, op=ALU.mult),
                  r=["t32a", "rowD"], w=["D_qn"])
            if KD <= 2:
                continue
            for i in range(8):
                P.pe(lambda e, i=i: e.transpose(self.tp[0:64, 128 * i:128 * i + 128], self.D_qn[:, 64 * i:64 * i + 64],
                                                self.identb), r=["D_qn", "CB"], w=["tp"])
            if KD <= 3:
                continue
            P.act(lambda e, t=t: e.activation(out=self.D_Q[0:64, :, 128 * t:128 * t + 128],
                                              in_=self.tp[0:64, 0:512].rearrange("p (h s) -> p h s", s=128), func=AF.Copy),
                  r=["tp"], w=["D_Q"])
            if os.environ.get("KE") == "1":
                continue
            P.act(lambda e, tt=tt: e.activation(out=self.D_K[0:64, :, 128 * tt:128 * tt + 128],
                                                in_=self.tp[0:64, 512:1024].rearrange("p (h s) -> p h s", s=128),
                                                func=AF.Copy), r=["tp"], w=[("D_K", tt)])
        self.wrel()
        wv, kv = self.wnext("D_v")
        for t in range(4 if KD > 4 else 0):
            tt = 4 * sg + t
            pj, pk = self.proj_tm(wv, kv, 256, t, 1536)
            P.act(lambda e, pj=pj, tt=tt: e.activation(out=self.D_V[:, tt, :, 0:64],
                                                       in_=pj[:, 0:256].rearrange("p (h d) -> p h d", d=64), func=AF.Copy),
                  r=[pk], w=[("D_V", tt)])
        self.wrel()
        wz, kz = self.wnext("D_z")
        for t in range(4 if KD > 5 else 0):
            tt = 4 * sg + t
            pj, pk = self.proj_tm(wz, kz, 256, t, 1792)
            P.act(lambda e, pj=pj, t=t: e.activation(out=self.D_z[:, t, :], in_=pj[:, 0:256], func=AF.Copy),
                  r=[pk], w=["D_z"])
        self.wrel()
        wg, kg = self.wnext("D_g")
        if KD <= 6:
            self.wrel()
            return
        self.gate_rows(wg, kg, 0, self.colp[:, 31:32], self.t32a[:], "t32a")
        self.wrel()
        self.softplus_neg(self.t32a[:], "t32a")
        P.dve(lambda e: e.memset(self.t32b[:], 1.0), w=["t32b"])
        P.dve(lambda e: e.tensor_tensor_scan(out=self.D_negF[:], data0=self.t32b[:], data1=self.t32a[:],
                                             initial=self.D_Fc[:, 0:1], op0=ALU.mult, op1=ALU.add),
              r=["t32a", "t32b", "D_Fc"], w=["D_negF"])
        P.dve(lambda e: e.tensor_copy(out=self.D_Fc[:], in_=self.D_negF[:, SEG - 1:SEG]), r=["D_negF"], w=["D_Fc"])
        if KSTOP <= 1:
            return
        for h in range(4):
            pj, pk = self.nextpj()
            P.pe(lambda e, h=h, pj=pj: e.matmul(pj[0:65, :], lhsT=cf["sel"][:, 128 * h:128 * h + 65], rhs=self.D_negF[:],
                                                start=True, stop=True), r=["CF", "D_negF"], w=[pk])
            P.act(lambda e, h=h, pj=pj: e.activation(out=self.D_Q[64:65, h, :], in_=pj[64:65, :], func=AF.Copy, scale=-1.0),
                  r=[pk], w=["D_Q"])
        if KSTOP <= 2:
            return
        for t in range(4):
            tt = 4 * sg + t
            P.pe(lambda e, t=t: e.transpose(self.tpf[:, 0:128], self.D_negF[:, 128 * t:128 * t + 128], cf["ident"]),
                 r=["D_negF", "CF"], w=["tpf"])
            P.dve(lambda e, tt=tt: e.tensor_copy(out=self.D_fcol[:, tt, :], in_=self.tpf[:, 0:128:32]), r=["tpf"],
                  w=["D_fcol"])
        if KSTOP <= 3:
            return
        nq = 4
        for h in range(4 if KSTOP > 4 else 1):
            for kb in range(4 * sg + 4):
                q0 = max(kb - 4 * sg, 0)
                ncol = 128 * (nq - q0)
                sc, sk = self.sc[kb % 2], ("sc", kb % 2)
                diag = kb >= 4 * sg
                P.pe(lambda e, h=h, kb=kb, q0=q0, ncol=ncol, sc=sc, diag=diag: e.matmul(
                    sc[:, 0:ncol], lhsT=self.D_K[0:65, h, 128 * kb:128 * kb + 128], rhs=self.D_Q[0:65, h, 128 * q0:SEG],
                    start=True, stop=(not diag)), r=[("D_K", kb), "D_Q"], w=[sk])
                if diag:
                    P.pe(lambda e, sc=sc: e.matmul(sc[:, 0:128], lhsT=self.identb, rhs=self.masknegb,
                                                   start=False, stop=True), r=["CB", "CB"], w=[sk])
                pt, ptk = self.PT[:, kb % 2, :], ("PT", kb % 2)
                P.act(lambda e, h=h, kb=kb, ncol=ncol, sc=sc, pt=pt: e.activation(
                    out=pt[:, 0:ncol], in_=sc[:, 0:ncol], func=AF.Exp, bias=self.D_fcol[:, kb, h:h + 1]),
                    r=[sk, "D_fcol"], w=[ptk])
                for ql in range(q0, nq):
                    qb = 4 * sg + ql
                    P.pe(lambda e, h=h, kb=kb, ql=ql, q0=q0, qb=qb, pt=pt: e.matmul(
                        self.acc[:, 128 * ql:128 * ql + 65], lhsT=pt[:, 128 * (ql - q0):128 * (ql - q0) + 128],
                        rhs=self.D_V[:, kb, h, :], start=(kb == 0 and ql == 0), stop=(kb == qb),
                        skip_group_check=True), r=[ptk, ("D_V", kb)], w=["acc"])
            accv = self.acc[:, :].rearrange("p (q c) -> p q c", c=128)
            P.dve(lambda e, accv=accv: e.reciprocal(out=self.D_rd[:], in_=accv[:, :, 64]), r=["acc"], w=["D_rd"])
            P.dve(lambda e, accv=accv, h=h: e.tensor_tensor(out=self.D_o[:, :, h, :], in0=accv[:, :, 0:64],
                                                            in1=self.D_rd[:].unsqueeze(2).to_broadcast([128, 4, 64]),
                                                            op=ALU.mult), r=["acc", "D_rd"], w=["D_o"])
        for t in range(4):
            self.silu_gate(self.D_z[:, t, :], "D_z", self.t32a[:, 0:256], "t32a")
            P.dve(lambda e, t=t: e.tensor_tensor(out=self.D_y[:], in0=self.D_o[:, t, :, :].rearrange("p h d -> p (h d)"),
                                                 in1=self.t32a[:, 0:256], op=ALU.mult), r=["D_o", "t32a"], w=["D_y"])
            self.y_to_yT(self.D_y, "D_y", t, 6)

    def xattn_kv(self):
        P = self.P
        self.scr_reset()
        self.kmT = self.scr("kmT", [128, 8, 256], BF16)
        self.vm = self.scr("vm", [128, 2, D], BF16)
        self.PT = self.scr("PT", [128, 2, 512], BF16)
        self.t32d = self.scr("t32d", [128, 512])
        for i in range(2):
            w, wk = self.wnext("KV%d" % i)
            for c in range(4):
                fc = 4 * i + c
                pj, pk = self.nextpj()
                for dc in range(8):
                    P.pe(lambda e, dc=dc, c=c, pj=pj, w=w: e.matmul(pj[:, 0:256], lhsT=w[:, dc, 128 * c:128 * c + 128],
                                                                    rhs=self.memT[:, dc, :], start=(dc == 0), stop=(dc == 7)),
                         r=[wk, "memT"], w=[pk])
                P.act(lambda e, fc=fc, pj=pj: e.activation(out=self.kmT[:, fc, :], in_=pj[:, 0:256], func=AF.Copy),
                      r=[pk], w=["kmT"])
            self.wrel()
        for i in range(2):
            w, wk = self.wnext("KV%d" % (2 + i))
            for mt in range(2):
                pj, pk = self.nextpj()
                for dc in range(8):
                    P.pe(lambda e, dc=dc, mt=mt, pj=pj, w=w: e.matmul(pj[:], lhsT=self.memT[:, dc, 128 * mt:128 * mt + 128],
                                                                      rhs=w[:, dc, :], start=(dc == 0), stop=(dc == 7)),
                         r=[wk, "memT"], w=[pk])
                P.act(lambda e, i=i, mt=mt, pj=pj: e.activation(out=self.vm[:, mt, 512 * i:512 * i + 512], in_=pj[:],
                                                                func=AF.Copy), r=[pk], w=["vm"])
            self.wrel()

    def xattn_seg(self, sg):
        P = self.P
        qT = self.yT
        for i in range(2):
            w, wk = self.wnext("Q%d" % i)
            for c in range(4):
                fc = 4 * i + c
                pj, pk = self.nextpj()
                for dc in range(8):
                    P.pe(lambda e, dc=dc, c=c, pj=pj, w=w: e.matmul(pj[:], lhsT=w[:, dc, 128 * c:128 * c + 128],
                                                                    rhs=self.hT[:, dc, :], start=(dc == 0), stop=(dc == 7)),
                         r=[wk, "hT"], w=[pk])
                P.act(lambda e, fc=fc, pj=pj: e.activation(out=qT[:, fc, :], in_=pj[:], func=AF.Copy), r=[pk], w=["yT"])
            self.wrel()
        oT = self.hT
        for h in range(4):
            for mt in range(2):
                sc, sk = self.sc[mt], ("sc", mt)
                for j in range(2):
                    P.pe(lambda e, h=h, mt=mt, j=j, sc=sc: e.matmul(
                        sc[:], lhsT=self.kmT[:, 2 * h + j, 128 * mt:128 * mt + 128], rhs=qT[:, 2 * h + j, :],
                        start=(j == 0), stop=(j == 1)), r=["kmT", "yT"], w=[sk])
                P.act(lambda e, mt=mt, sc=sc: e.activation(out=self.PT[:, mt, :], in_=sc[:], func=AF.Exp, scale=1.0 / 16.0),
                      r=[sk], w=[("PT", mt)])
            for mt in range(2):
                P.pe(lambda e, mt=mt: e.matmul(self.stp[:], lhsT=self.onesb, rhs=self.PT[:, mt, :], start=(mt == 0),
                                               stop=(mt == 1)), r=["CB", ("PT", mt)], w=["stp"])
            P.dve(lambda e: e.reciprocal(out=self.t32d[:], in_=self.stp[:]), r=["stp"], w=["t32d"])
            for j in range(2):
                fc = 2 * h + j
                for mt in range(2):
                    P.pe(lambda e, fc=fc, mt=mt: e.matmul(self.acc[:], lhsT=self.vm[:, mt, 128 * fc:128 * fc + 128],
                                                          rhs=self.PT[:, mt, :], start=(mt == 0), stop=(mt == 1)),
                         r=["vm", ("PT", mt)], w=["acc"])
                P.dve(lambda e, fc=fc: e.tensor_tensor(out=oT[:, fc, :], in0=self.acc[:], in1=self.t32d[:], op=ALU.mult),
                      r=["acc", "t32d"], w=["hT"])


def build_program(nl=NL, dbg=(), mixers="ABCD", xattn=True):
    nc = bass.Bass("TRN2", target_bir_lowering=False)
    b = Builder(nc, nl, dbg, mixers, xattn)
    with b.es:
        b.build()
    return nc, b


_CACHE = {}


def kernel(**inputs):
    inp = {k: np.asarray(v) for k, v in inputs.items()}
    lay = host_layout(inp)
    if "nc" not in _CACHE:
        _CACHE["nc"] = build_program()[0]
    nc = _CACHE["nc"]
    shared = dict(w_in=inp["w_in"], w_out=inp["w_out"], xattn_wq=inp["xattn_wq"], xattn_wkv=inp["xattn_wkv"],
                  xattn_wo=inp["xattn_wo"], post_norm_g=inp["post_norm_g"], xattn_post_g=inp["xattn_post_g"],
                  mem_norm_g=inp["mem_norm_g"].reshape(1, D), **lay)
    shared = {k: np.ascontiguousarray(v, dtype=np.float32) for k, v in shared.items()}
    in_maps = []
    for b in range(8):
        m = dict(shared)
        m["x"] = np.ascontiguousarray(inp["x"][b], dtype=np.float32)
        m["mem"] = np.ascontiguousarray(inp["mem"][b], dtype=np.float32)
        in_maps.append(m)
    res = run_bass_kernel_spmd(nc, in_maps, core_ids=list(range(8)))
    return np.stack([res.results[b]["out"] for b in range(8)]).astype(np.float32)
```

```python
import contextlib
import os
import numpy as np
import concourse.bass as bass
import concourse.mybir as mybir
from concourse.bass_utils import run_bass_kernel_spmd

F32 = mybir.dt.float32
BF16 = mybir.dt.bfloat16
AF = mybir.ActivationFunctionType
ALU = mybir.AluOpType
AX = mybir.AxisListType

S = 2048
D = 1024
NL = 4
SEG = 512
NSEG = S // SEG
N_IN = 3724
OFF = dict(a_q=0, a_k=256, a_v=512, a_i=768, a_f=772, a_z=776, b_x=1032, b_z=1288, c_r=1544, c_k=1800,
           c_v=2056, c_w=2312, c_a=2376, c_z=2440, d_q=2696, d_k=2952, d_v=3208, d_f=3464, d_z=3468)
NEG = -30000.0
KSTOP = int(os.environ.get('KSTOP', '99'))
KD = int(os.environ.get('KD', '99'))


class Prog:
    NDMA = 12

    def __init__(self, nc):
        self.nc = nc
        self.ops = []
        self.state = {}
        self.children = {}
        self.clock = {e: {} for e in ("pe", "act", "dve", "pool", "sp")}
        self.evclock = {}
        self.count = {}
        self.dma_rr = {"sp": 0, "pool": 0, "act": 0}
        self.alias = set()

    def _conf(self, key):
        out = []
        for i in range(1, len(key) + 1):
            k = key[:i]
            if k in self.state:
                out.append(k)
        for k in self.children.get(key, ()):
            if k != key:
                out.append(k)
        return out

    def _reg(self, key):
        if key not in self.state:
            self.state[key] = [None, {}]
            for i in range(1, len(key) + 1):
                self.children.setdefault(key[:i], set()).add(key)

    def _k(self, key):
        key = key if isinstance(key, tuple) else (key,)
        if key[0] in self.alias:
            key = ("SCR",) + key
        return key

    def add(self, eng, fn, r=(), w=(), dma=False):
        r = [self._k(x) for x in r]
        w = [self._k(x) for x in w]
        deps = {}

        def need(ev):
            if ev is None:
                return
            s, v = ev
            if deps.get(s, 0) < v:
                deps[s] = v

        for k in r:
            self._reg(k)
            for c in self._conf(k):
                need(self.state[c][0])
        for k in w:
            self._reg(k)
            for c in self._conf(k):
                st = self.state[c]
                need(st[0])
                for s, v in st[1].items():
                    need((s, v))
        if dma:
            i = self.dma_rr[eng]
            self.dma_rr[eng] = (i + 1) % self.NDMA
            sem = "dma_%s_%d" % (eng, i)
            inc = 16
            if self.count.get(sem, 0) > 0:
                need((sem, self.count[sem]))
        else:
            sem = eng
            inc = 1
        val = self.count.get(sem, 0) + inc
        self.count[sem] = val
        ev = (sem, val)
        clk = self.clock[eng]
        waits = []
        for s, v in deps.items():
            if eng == "pe" and s == "pe":
                continue
            if clk.get(s, 0) >= v:
                continue
            waits.append((s, v))
        for s, v in waits:
            for s2, v2 in self.evclock[(s, v)].items():
                if clk.get(s2, 0) < v2:
                    clk[s2] = v2
        snap = dict(clk)
        snap[sem] = val
        self.evclock[ev] = snap
        for k in r:
            rd = self.state[k][1]
            if rd.get(sem, 0) < val:
                rd[sem] = val
        for k in w:
            self.state[k] = [ev, {}]
        self.ops.append((eng, fn, waits, sem, inc))
        return ev

    def pe(self, fn, r=(), w=()):
        return self.add("pe", fn, r, w)

    def act(self, fn, r=(), w=()):
        return self.add("act", fn, r, w)

    def dve(self, fn, r=(), w=()):
        return self.add("dve", fn, r, w)

    def pool(self, fn, r=(), w=()):
        return self.add("pool", fn, r, w)

    def dma(self, fn, r=(), w=(), q="sp"):
        return self.add(q, fn, r, w, dma=True)

    def finish(self, keys):
        self.add("sp", None, r=keys, w=())

    def emit(self, es):
        nc = self.nc
        names = sorted(self.count.keys())
        sems = {n: es.enter_context(nc.semaphore("s_" + n)) for n in names}
        block = es.enter_context(nc.Block())
        per = {e: [o for o in self.ops if o[0] == e] for e in self.clock}

        def run(engobj, lst):
            for (_, fn, waits, sem, inc) in lst:
                for s, v in waits:
                    engobj.wait_ge(sems[s], v)
                if fn is None:
                    continue
                ins = fn(engobj)
                ins.then_inc(sems[sem], inc)

        @block.tensor
        def _(e):
            run(e, per["pe"])

        @block.scalar
        def _(e):
            run(e, per["act"])

        @block.vector
        def _(e):
            run(e, per["dve"])

        @block.gpsimd
        def _(e):
            run(e, per["pool"])

        @block.sync
        def _(e):
            run(e, per["sp"])


NCOLP = 64
FM_OFFS = [0, 128, 256, 384, 1032, 1160, 1544, 1672, 1800, 1928, 2056, 2184, 2312]


def host_layout(inp):
    f = np.float32
    colp = np.zeros((NL, 128, NCOLP), f)
    rows = []
    wg = np.zeros((NL, D, 384), f)
    wsm = np.zeros((NL, 128, 256 + 256), f)
    brow = np.zeros((NL, 1, 2048), f)
    for l in range(NL):
        colp[l, :, 0:8] = inp["pre_norm_g"][l].reshape(8, 128).T
        colp[l, :, 8:16] = inp["xattn_pre_g"][l].reshape(8, 128).T
        b = inp["b_in"][l]
        for j, o in enumerate(FM_OFFS):
            colp[l, :, 16 + j] = b[o:o + 128]
        for h in range(4):
            colp[l, 32 * h, 29] = b[OFF["a_i"] + h]
            colp[l, 32 * h, 30] = b[OFF["a_f"] + h]
            colp[l, 32 * h, 31] = b[OFF["d_f"] + h]
        for j in range(4):
            for k in range(4):
                colp[l, :, 32 + 4 * j + k] = inp["conv_a"][l, k, 128 * j:128 * j + 128]
        colp[l, :, 48:55] = inp["shift_mu_c"][l].reshape(7, 128).T
        colp[l, :, 55:57] = inp["decay_w0"][l].reshape(2, 128).T
        colp[l, :, 57:59] = inp["iclr_a0"][l].reshape(2, 128).T
        colp[l, :, 59:61] = inp["key_k"][l].reshape(2, 128).T
        colp[l, :, 61:63] = inp["key_a"][l].reshape(2, 128).T
        colp[l, :, 63] = 0.0
        qk = inp["qk_norm_d"][l]
        rows.append(np.concatenate([
            inp["norm_a_g"][l], inp["pool_scale"][l], inp["bonus_u"][l], inp["gn_c_g"][l], inp["gn_c_b"][l],
            np.tile(qk[0], 4), np.tile(qk[1], 4)]).astype(f))
        w = inp["w_in"][l]
        for h in range(4):
            wg[l, :, 32 * h] = w[:, OFF["a_i"] + h]
            wg[l, :, 128 + 32 * h] = w[:, OFF["a_f"] + h]
            wg[l, :, 256 + 32 * h] = w[:, OFF["d_f"] + h]
        pw = inp["pool_w"][l]
        for j in range(2):
            for gl in range(2):
                wsm[l, 64 * gl:64 * gl + 64, 128 * j + 64 * gl:128 * j + 64 * gl + 64] = pw[2 * j + gl]
        wsm[l, 0:64, 256:512] = inp["decay_w2"][l]
        wsm[l, 64:128, 256:512] = inp["iclr_a2"][l]
        brow[l, 0, :] = np.concatenate([b[OFF["a_v"]:OFF["a_v"] + 256], b[OFF["a_z"]:OFF["a_z"] + 256],
                                        b[OFF["b_z"]:OFF["b_z"] + 256], b[OFF["c_z"]:OFF["c_z"] + 256],
                                        b[OFF["d_q"]:OFF["d_q"] + 512], b[OFF["d_v"]:OFF["d_v"] + 256],
                                        b[OFF["d_z"]:OFF["d_z"] + 256]])
    rowp = np.stack(rows)[:, None, :]
    colx = np.zeros((NL, 128, 2), f)
    for l in range(NL):
        colx[l] = inp["bonus_u"][l].reshape(2, 128).T
    c = {}
    c["ident"] = np.eye(128, dtype=f)
    s_i = np.arange(128)[:, None]
    t_i = np.arange(128)[None, :]
    c["maskT"] = (s_i <= t_i).astype(f)
    c["maskneg"] = np.where(s_i <= t_i, 0.0, NEG).astype(f)
    same = (s_i // 64) == (t_i // 64)
    c["mS"] = ((s_i < t_i) & same).astype(f)
    c["mI"] = ((s_i <= t_i) & same).astype(f)
    c["mSt"] = c["mS"].T.copy()
    c["ones"] = np.ones((128, 128), f)
    c["blk64"] = same.astype(f)
    hs = np.zeros((128, 128), f)
    hs[0:64, 0] = 1.0
    hs[64:128, 1] = 1.0
    c["hsel"] = hs
    sel = np.zeros((128, 4 * 128), f)
    for h in range(4):
        sel[32 * h, 128 * h + 64] = 1.0
    c["sel"] = sel
    selb = np.zeros((128, 256), f)
    for h in range(4):
        selb[32 * h, 64 * h:64 * h + 64] = 1.0
    c["selb"] = selb
    rs = np.ones((128, 512), f)
    rs[:, 0::64] = 0.0
    c["rs64"] = rs
    cnt = np.ones((128, 16), f)
    for t in range(16):
        cnt[:, t] = 1.0 / (t + 1.0)
    c["invc"] = cnt
    consts = np.concatenate([c[k] for k in CONST_KEYS], axis=1)
    constsb = np.concatenate([c[k] for k in CONSTB_KEYS], axis=1)
    return dict(colp=colp, rowp=rowp, wg=wg, wsm=wsm, brow=brow, colx=colx, consts=consts, constsb=constsb)


CONST_KEYS = ["ident", "maskT", "mS", "mI", "mSt", "sel", "selb", "invc"]
CONST_W = dict(ident=128, maskT=128, mS=128, mI=128, mSt=128, sel=512, selb=256, invc=16)
NCONST = sum(CONST_W[k] for k in CONST_KEYS)
CONSTB_KEYS = ["ident", "ones", "maskneg", "blk64", "hsel"]
NCONSTB = 128 * len(CONSTB_KEYS)


class Builder:
    def __init__(self, nc, nl=NL, dbg=(), mixers="ABCD", xattn=True):
        self.nc = nc
        self.nl = nl
        self.dbg = set(dbg)
        self.mixers = mixers
        self.xattn = xattn
        self.es = contextlib.ExitStack()
        self.P = Prog(nc)
        self.outs = []
        self.wq = []
        self.wissued = 0
        self.wused = 0
        self.wreleased = 0

    def sb(self, name, shape, dt=F32):
        nb = int(np.prod(shape[1:])) * (2 if dt == BF16 else 4)
        self.sbsizes = getattr(self, "sbsizes", {})
        self.sbsizes[name] = nb
        try:
            return self.es.enter_context(self.nc.sbuf_tensor("sb_" + name, shape, dt))
        except AssertionError:
            tot = 0
            for k, v in sorted(self.sbsizes.items(), key=lambda kv: -kv[1]):
                tot += v
                print("SBUF", k, v)
            print("SBUF total", tot)
            raise

    def ps(self, name, shape, dt=F32):
        return self.es.enter_context(self.nc.psum_tensor("ps_" + name, shape, dt))

    def scr_reset(self):
        self.o16 = 0
        self.o32 = 0
        self.P.dve(lambda e: e.memset(self.bar[:], 0.0), w=[("SCR",)])

    def scr(self, name, shape, dt=F32):
        n = int(np.prod(shape[1:]))
        if dt == BF16:
            ar, off = self.ar16, self.o16
            self.o16 += n + (n % 2)
            assert self.o16 <= self.N16, ("ar16 overflow", name, self.o16)
        else:
            ar, off = self.ar32, self.o32
            self.o32 += n
            assert self.o32 <= self.N32, ("ar32 overflow", name, self.o32)
        v = ar[0:shape[0], off:off + n]
        if len(shape) == 3:
            v = v.rearrange("p (a b) -> p a b", b=shape[2])
        elif len(shape) == 4:
            v = v.rearrange("p (a b c) -> p a b c", b=shape[2], c=shape[3])
        self.P.alias.add(name)
        return v

    def din(self, name, shape):
        return self.nc.dram_tensor(name, shape, F32, kind="ExternalInput").ap()

    def dump(self, name, ap, shape, key):
        if name not in self.dbg:
            return
        o = self.nc.dram_tensor("dbg_" + name, shape, ap.dtype, kind="ExternalOutput").ap()
        self.P.dma(lambda e: e.dma_start(out=o, in_=ap), r=[key], w=["dbg_" + name])
        self.outs.append("dbg_" + name)

    def wplan(self, tag, src, ncols):
        self.wq.append((tag, src, ncols))

    def _wissue(self):
        while self.wissued < len(self.wq) and self.wissued < self.wreleased + 3:
            i = self.wissued
            tag, src, n = self.wq[i]
            buf = self.WB[i % 3]
            srcv = src.rearrange("(dc p) n -> p dc n", p=128)
            self.P.dma(lambda e, buf=buf, srcv=srcv, n=n: e.dma_start(out=buf[:, :, 0:n], in_=srcv),
                       w=[("WB", i % 3)], q="pool")
            self.wissued += 1

    def wnext(self, tag):
        i = self.wused
        assert self.wq[i][0] == tag, (self.wq[i][0], tag)
        self._wissue()
        assert i < self.wissued, "weight group not issued (too many groups held)"
        self.wused += 1
        return self.WB[i % 3], ("WB", i % 3)

    def wrel(self, n=1):
        self.wreleased += n
        assert self.wreleased <= self.wused
        self._wissue()

    def build(self):
        nc, P = self.nc, self.P
        self.x_d = self.din("x", [S, D])
        self.mem_d = self.din("mem", [256, D])
        self.w_in = self.din("w_in", [self.nl, D, N_IN])
        self.w_out = self.din("w_out", [self.nl, D, D])
        self.wq_d = self.din("xattn_wq", [self.nl, D, D])
        self.wkv_d = self.din("xattn_wkv", [self.nl, D, 2 * D])
        self.wo_d = self.din("xattn_wo", [self.nl, D, D])
        self.post_g = self.din("post_norm_g", [self.nl, D])
        self.xpost_g = self.din("xattn_post_g", [self.nl, D])
        self.memg = self.din("mem_norm_g", [1, D])
        self.colp_d = self.din("colp", [self.nl, 128, NCOLP])
        self.colx_d = self.din("colx", [self.nl, 128, 2])
        self.rowp_d = self.din("rowp", [self.nl, 1, 1792])
        self.wg_d = self.din("wg", [self.nl, D, 384])
        self.wsm_d = self.din("wsm", [self.nl, 128, 512])
        self.brow_d = self.din("brow", [self.nl, 1, 2048])
        self.consts_d = self.din("consts", [128, NCONST])
        self.constsb_d = self.din("constsb", [128, NCONSTB])
        self.out_d = nc.dram_tensor("out", [S, D], F32, kind="ExternalOutput").ap()

        sb, ps = self.sb, self.ps
        self.X = sb("X", [128, 16, D])
        self.hT = sb("hT", [128, 8, SEG], BF16)
        self.yT = sb("yT", [128, 8, SEG], BF16)
        self.WB = [sb("WB%d" % i, [128, 8, 512], BF16) for i in range(3)]
        self.CF = sb("CF", [128, NCONST])
        self.cf = {}
        o = 0
        for k in CONST_KEYS:
            self.cf[k] = self.CF[:, o:o + CONST_W[k]]
            o += CONST_W[k]
        self.CB = sb("CB", [128, NCONSTB], BF16)
        self.identb = self.CB[:, 0:128]
        self.onesb = self.CB[:, 128:256]
        self.masknegb = self.CB[:, 256:384]
        self.blk64b = self.CB[:, 384:512]
        self.hselb = self.CB[:, 512:514]
        self.colp = sb("colp", [128, NCOLP])
        self.colx = sb("colx", [128, 2])
        self.growt = sb("growt", [128, D])
        self.brow = sb("brow", [1, 2048], BF16)
        self.wsm = sb("wsm", [128, 512], BF16)
        self.memT = sb("memT", [128, 8, 256], BF16)
        self.N16, self.N32 = 12416, 5440
        self.ar16 = sb("ar16", [128, self.N16], BF16)
        self.ar32 = sb("ar32", [128, self.N32])
        self.bar = sb("bar", [128, 1])
        self.o16 = self.o32 = 0
        self.xn = sb("xn", [128, D], BF16)
        self.col1 = sb("col1", [128, 8])
        self.epsc = sb("epsc", [128, 2])
        self.t32a = sb("t32a", [128, 512])
        self.t32b = sb("t32b", [128, 512])
        self.t32c = sb("t32c", [128, 512])
        self.pj = [ps("pj0", [128, 512]), ps("pj1", [128, 512])]
        self.tp = ps("tp", [128, 1024], BF16)
        self.tpf = ps("tpf", [128, 512])
        self.sc = [ps("sc0", [128, 512]), ps("sc1", [128, 512])]
        self.acc = ps("acc", [128, 512])
        self.stp = ps("stp", [128, 512])
        self.pji = 0

        self.plan_weights()
        self.setup()
        for l in range(self.nl):
            self.layer(l)
        for tt in range(16):
            P.dma(lambda e, tt=tt: e.dma_start(out=self.out_d[128 * tt:128 * tt + 128, :], in_=self.X[:, tt, :]),
                  r=[("X", tt)], w=[("out", tt)], q="sp")
        P.finish(["out"] + self.outs)
        P.emit(self.es)
        assert self.wused == len(self.wq) == self.wreleased, (self.wused, len(self.wq), self.wreleased)

    def plan_weights(self):
        for l in range(self.nl):
            wi = self.w_in[l]
            for sg in range(NSEG):
                if "A" in self.mixers:
                    self.wplan("A_fm", wi[:, 0:512], 512)
                    self.wplan("A_v", wi[:, OFF["a_v"]:OFF["a_v"] + 256], 256)
                    self.wplan("A_z", wi[:, OFF["a_z"]:OFF["a_z"] + 256], 256)
                    self.wplan("A_g", self.wg_d[l][:, 0:256], 256)
                if "B" in self.mixers:
                    self.wplan("B", wi[:, OFF["b_x"]:OFF["b_x"] + 512], 512)
                if "C" in self.mixers:
                    self.wplan("C_z", wi[:, OFF["c_z"]:OFF["c_z"] + 256], 256)
                    self.wplan("C_rk", wi[:, OFF["c_r"]:OFF["c_r"] + 512], 512)
                    self.wplan("C_vw", wi[:, OFF["c_v"]:OFF["c_v"] + 384], 384)
                if "D" in self.mixers:
                    self.wplan("D_qk", wi[:, OFF["d_q"]:OFF["d_q"] + 512], 512)
                    self.wplan("D_v", wi[:, OFF["d_v"]:OFF["d_v"] + 256], 256)
                    self.wplan("D_z", wi[:, OFF["d_z"]:OFF["d_z"] + 256], 256)
                    self.wplan("D_g", self.wg_d[l][:, 256:384], 128)
                self.wplan("O0", self.w_out[l][:, 0:512], 512)
                self.wplan("O1", self.w_out[l][:, 512:1024], 512)
            if self.xattn:
                for i in range(4):
                    self.wplan("KV%d" % i, self.wkv_d[l][:, 512 * i:512 * i + 512], 512)
                for sg in range(NSEG):
                    self.wplan("Q0", self.wq_d[l][:, 0:512], 512)
                    self.wplan("Q1", self.wq_d[l][:, 512:1024], 512)
                    self.wplan("XO0", self.wo_d[l][:, 0:512], 512)
                    self.wplan("XO1", self.wo_d[l][:, 512:1024], 512)

    def nextpj(self):
        i = self.pji
        self.pji ^= 1
        return self.pj[i], ("pj", i)

    def rstd_col(self, out, ss, n, eps, rkeys, wkey):
        P = self.P
        P.act(lambda e: e.activation(out=out, in_=ss, func=AF.Sqrt, scale=1.0 / n, bias=self.epsc[:, 0:1] if eps == 1e-6 else self.epsc[:, 1:2]),
              r=rkeys + ["epsc"], w=[wkey])
        P.dve(lambda e: e.reciprocal(out=out, in_=out), r=[wkey], w=[wkey])

    def transpose_to(self, src_ap, src_key, dst_ap, dst_key, n, evac="dve"):
        P = self.P
        for i in range(n):
            P.pe(lambda e, i=i: e.transpose(self.tp[:, 128 * i:128 * i + 128], src_ap(i), self.identb),
                 r=[src_key, "CB"], w=["tp"])
        if evac == "dve":
            P.dve(lambda e: e.tensor_copy(out=dst_ap, in_=self.tp[:, 0:128 * n]), r=["tp"], w=[dst_key])
        else:
            P.act(lambda e: e.activation(out=dst_ap, in_=self.tp[:, 0:128 * n], func=AF.Copy), r=["tp"], w=[dst_key])

    def setup(self):
        P = self.P
        P.dma(lambda e: e.dma_start(out=self.CF[:], in_=self.consts_d[:, :]), w=["CF"])
        for tt in range(16):
            P.dma(lambda e, tt=tt: e.dma_start(out=self.X[:, tt, :], in_=self.x_d[128 * tt:128 * tt + 128, :]),
                  w=[("X", tt)], q=("sp" if tt % 2 == 0 else "act"))
        c = self.cf
        P.dve(lambda e: e.memset(self.epsc[:, 0:1], 1e-6), w=["epsc"])
        P.dve(lambda e: e.memset(self.epsc[:, 1:2], 64e-5), w=["epsc"])
        P.dma(lambda e: e.dma_start(out=self.CB[:], in_=self.constsb_d[:, :]), w=["CB"], q="pool")
        P.dma(lambda e: e.dma_start(out=self.growt[:], in_=self.memg[0:1, :].partition_broadcast(128)), w=["growt"])
        for mt in range(2):
            xin = self.t32a
            P.dma(lambda e, mt=mt: e.dma_start(out=self.t32a[:], in_=self.mem_d[128 * mt:128 * mt + 128, 0:512]), w=["t32a"])
            P.dma(lambda e, mt=mt: e.dma_start(out=self.t32b[:], in_=self.mem_d[128 * mt:128 * mt + 128, 512:1024]), w=["t32b"])
            P.act(lambda e: e.activation(out=self.t32c[:], in_=self.t32a[:], func=AF.Square, accum_out=self.col1[:, 0:1]),
                  r=["t32a"], w=["t32c", "col1"])
            P.act(lambda e: e.activation(out=self.t32c[:], in_=self.t32b[:], func=AF.Square, accum_out=self.col1[:, 1:2]),
                  r=["t32b"], w=["t32c", "col1"])
            P.dve(lambda e: e.tensor_tensor(out=self.col1[:, 2:3], in0=self.col1[:, 0:1], in1=self.col1[:, 1:2], op=ALU.add),
                  r=["col1"], w=["col1"])
            self.rstd_col(self.col1[:, 3:4], self.col1[:, 2:3], D, 1e-6, ["col1"], "col1")
            P.dve(lambda e: e.scalar_tensor_tensor(out=self.xn[:, 0:512], in0=self.t32a[:], scalar=self.col1[:, 3:4],
                                                   in1=self.growt[:, 0:512], op0=ALU.mult, op1=ALU.mult),
                  r=["t32a", "col1", "growt"], w=["xn"])
            P.dve(lambda e: e.scalar_tensor_tensor(out=self.xn[:, 512:1024], in0=self.t32b[:], scalar=self.col1[:, 3:4],
                                                   in1=self.growt[:, 512:1024], op0=ALU.mult, op1=ALU.mult),
                  r=["t32b", "col1", "growt"], w=["xn"])
            self.transpose_to(lambda i: self.xn[:, 128 * i:128 * i + 128], "xn",
                              self.memT[:, :, 128 * mt:128 * mt + 128],
                              "memT", 8)
        self.dump("memT", self.memT[:], [128, 8, 256], "memT")
        if self.mixers != "ABCD":
            P.dve(lambda e: e.memset(self.yT[:], 0.0), w=["yT"])

    def layer(self, l):
        P = self.P
        P.dma(lambda e: e.dma_start(out=self.colp[:], in_=self.colp_d[l]), w=["colp"])
        P.dma(lambda e: e.dma_start(out=self.colx[:], in_=self.colx_d[l]), w=["colx"])
        P.dma(lambda e: e.dma_start(out=self.brow[:], in_=self.brow_d[l]), w=["brow"], q="pool")
        P.dma(lambda e: e.dma_start(out=self.wsm[:], in_=self.wsm_d[l]), w=["wsm"], q="pool")
        P.dma(lambda e: e.dma_start(out=self.growt[:], in_=self.post_g[l:l + 1, :].partition_broadcast(128)), w=["growt"])
        for sg in range(NSEG):
            self.norm_T(l, sg, 0)
            if l == 0 and sg == 0:
                self.dump("hT0", self.hT[:], [128, 8, SEG], "hT")
            if "A" in self.mixers:
                self.mixer_A(l, sg)
            if "B" in self.mixers:
                self.mixer_B(l, sg)
            if "C" in self.mixers:
                self.mixer_C(l, sg)
            if "D" in self.mixers:
                self.mixer_D(l, sg)
            if l == 0:
                self.dump("yT%d" % sg, self.yT[:], [128, 8, SEG], "yT")
            self.out_proj(sg, self.yT, "yT", "O0", "O1")
        if l == 0:
            self.dump("x1", self.X[:], [128, 16, D], "X")
        if self.xattn:
            P.dma(lambda e: e.dma_start(out=self.growt[:], in_=self.xpost_g[l:l + 1, :].partition_broadcast(128)), w=["growt"])
            self.xattn_kv()
            for sg in range(NSEG):
                self.norm_T(l, sg, 8)
                self.xattn_seg(sg)
                self.out_proj(sg, self.hT, "hT", "XO0", "XO1")
            if l == 0:
                self.dump("x2", self.X[:], [128, 16, D], "X")

    def norm_T(self, l, sg, gcol):
        P = self.P
        for t in range(4):
            tt = 4 * sg + t
            c0 = 4 * (t % 2)
            ck = ("col1", t % 2)
            P.act(lambda e, tt=tt, c0=c0: e.activation(out=self.t32a[:], in_=self.X[:, tt, 0:512], func=AF.Square,
                                                       accum_out=self.col1[:, c0:c0 + 1]), r=[("X", tt)], w=["t32a", ck])
            P.act(lambda e, tt=tt, c0=c0: e.activation(out=self.t32a[:], in_=self.X[:, tt, 512:1024], func=AF.Square,
                                                       accum_out=self.col1[:, c0 + 1:c0 + 2]), r=[("X", tt)], w=["t32a", ck])
            P.dve(lambda e, c0=c0: e.tensor_tensor(out=self.col1[:, c0 + 2:c0 + 3], in0=self.col1[:, c0:c0 + 1],
                                                   in1=self.col1[:, c0 + 1:c0 + 2], op=ALU.add), r=[ck], w=[ck])
            self.rstd_col(self.col1[:, c0 + 3:c0 + 4], self.col1[:, c0 + 2:c0 + 3], D, 1e-6, [ck], ck)
            P.dve(lambda e, tt=tt, c0=c0: e.tensor_scalar(out=self.xn[:], in0=self.X[:, tt, :], scalar1=self.col1[:, c0 + 3:c0 + 4],
                                                          scalar2=None, op0=ALU.mult), r=[("X", tt), ck], w=["xn"])
            for i in range(8):
                P.pe(lambda e, i=i: e.transpose(self.tp[:, 128 * i:128 * i + 128], self.xn[:, 128 * i:128 * i + 128],
                                                self.identb), r=["xn", "CB"], w=["tp"])
            gc = self.colp[:, gcol:gcol + 8]
            P.dve(lambda e, t=t, gc=gc: e.tensor_tensor(
                out=self.hT[:, :, 128 * t:128 * t + 128], in0=self.tp[:, :].rearrange("p (a b) -> p a b", b=128),
                in1=gc.unsqueeze(2).to_broadcast([128, 8, 128]), op=ALU.mult), r=["tp", "colp"], w=["hT"])

    def out_proj(self, sg, srcT, skey, tag0, tag1):
        P = self.P
        w0, k0 = self.wnext(tag0)
        w1, k1 = self.wnext(tag1)
        for t in range(4):
            tt = 4 * sg + t
            banks = (self.pj, "pj") if t % 2 == 0 else (self.sc, "sc")
            c0 = 4 * (t % 2)
            ck = ("col1", t % 2)
            for half, (w, k) in enumerate(((w0, k0), (w1, k1))):
                pj, pk = banks[0][half], (banks[1], half)
                for dc in range(8):
                    P.pe(lambda e, dc=dc, w=w, pj=pj, t=t: e.matmul(pj[:], lhsT=srcT[:, dc, 128 * t:128 * t + 128],
                                                                    rhs=w[:, dc, :], start=(dc == 0), stop=(dc == 7)),
                         r=[skey, k], w=[pk])
                P.act(lambda e, pj=pj, half=half, c0=c0: e.activation(out=self.t32a[:], in_=pj[:], func=AF.Square,
                                                                      accum_out=self.col1[:, c0 + half:c0 + half + 1]),
                      r=[pk], w=["t32a", ck])
            P.dve(lambda e, c0=c0: e.tensor_tensor(out=self.col1[:, c0 + 2:c0 + 3], in0=self.col1[:, c0:c0 + 1],
                                                   in1=self.col1[:, c0 + 1:c0 + 2], op=ALU.add), r=[ck], w=[ck])
            self.rstd_col(self.col1[:, c0 + 3:c0 + 4], self.col1[:, c0 + 2:c0 + 3], D, 1e-6, [ck], ck)
            for half in range(2):
                pj, pk = banks[0][half], (banks[1], half)
                tmp = self.t32b if half == 0 else self.t32c
                tk = "t32b" if half == 0 else "t32c"
                P.dve(lambda e, pj=pj, tmp=tmp, half=half, c0=c0: e.scalar_tensor_tensor(
                    out=tmp[:], in0=pj[:], scalar=self.col1[:, c0 + 3:c0 + 4], in1=self.growt[:, 512 * half:512 * half + 512],
                    op0=ALU.mult, op1=ALU.mult), r=[pk, ck, "growt"], w=[tk])
                P.dve(lambda e, tmp=tmp, half=half, tt=tt: e.tensor_tensor(
                    out=self.X[:, tt, 512 * half:512 * half + 512], in0=self.X[:, tt, 512 * half:512 * half + 512],
                    in1=tmp[:], op=ALU.add), r=[tk, ("X", tt)], w=[("X", tt)])
        self.wrel(2)

    def proj_fm(self, w, wk, c0, out_fn, bias_col, n=SEG, func=AF.Identity):
        P = self.P
        pj, pk = self.nextpj()
        for dc in range(8):
            P.pe(lambda e, dc=dc: e.matmul(pj[:, 0:n], lhsT=w[:, dc, c0:c0 + 128], rhs=self.hT[:, dc, 0:n],
                                           start=(dc == 0), stop=(dc == 7)), r=["hT", wk], w=[pk])
        out_ap, out_key = out_fn
        P.act(lambda e: e.activation(out=out_ap, in_=pj[:, 0:n], func=func, bias=bias_col), r=[pk, "colp"], w=[out_key])

    def proj_tm(self, w, wk, ncols, t, b0):
        P = self.P
        pj, pk = self.nextpj()
        for dc in range(8):
            P.pe(lambda e, dc=dc: e.matmul(pj[:, 0:ncols], lhsT=self.hT[:, dc, 128 * t:128 * t + 128],
                                           rhs=w[:, dc, 0:ncols], start=(dc == 0), stop=False), r=["hT", wk], w=[pk])
        P.pe(lambda e: e.matmul(pj[:, 0:ncols], lhsT=self.onesb[0:1, :], rhs=self.brow[0:1, b0:b0 + ncols],
                                start=False, stop=True), r=["CB", "brow"], w=[pk])
        return pj, pk

    def silu_gate(self, z_ap, zkey, out32, okey):
        P = self.P
        P.act(lambda e: e.activation(out=out32, in_=z_ap, func=AF.Sigmoid), r=[zkey], w=[okey])
        P.dve(lambda e: e.tensor_tensor(out=out32, in0=out32, in1=z_ap, op=ALU.mult), r=[zkey, okey], w=[okey])

    def y_to_yT(self, ybf, ykey, t, c0):
        self.transpose_to(lambda i: ybf[:, 128 * i:128 * i + 128], ykey,
                          self.yT[:, c0:c0 + 2, 128 * t:128 * t + 128], "yT", 2, evac="act")

    def gate_rows(self, w, wk, c0, bias_col, out_ap, okey):
        self.proj_fm(w, wk, c0, (out_ap, okey), bias_col)

    def softplus_neg(self, buf, key):
        P = self.P
        P.act(lambda e: e.activation(out=buf, in_=buf, func=AF.Exp, scale=-1.0), r=[key], w=[key])
        P.act(lambda e: e.activation(out=buf, in_=buf, func=AF.Ln, bias=1.0), r=[key], w=[key])

    def alloc_A(self):
        if hasattr(self, "A_halo"):
            return
        sb = self.sb
        self.A_halo = sb("A_halo", [128, 4, 16], BF16)
        self.A_M = sb("A_M", [128, 8])
        self.A_neg30 = sb("A_neg30", [128, 4])
        self.A_Fc = sb("A_Fc", [128, 1])
        self.A_G = sb("A_G", [128, 2, 65])
        self.A_Gb = sb("A_Gb", [128, 4, 65], BF16)
        P = self.P
        P.pool(lambda e: e.memset(self.A_neg30[:], -1e30), w=["A_neg30"])
        P.pool(lambda e: e.memset(self.A_Gb[:], 0.0), w=["A_Gb"])

    def scratch_A(self, l):
        self.scr_reset()
        scr = self.scr
        self.A_raw = scr("A_raw", [128, 4, 16 + SEG], BF16)
        self.A_qk = scr("A_qk", [128, 4, SEG], BF16)
        self.A_qm = scr("A_qm", [128, 4, SEG], BF16)
        self.A_v = scr("A_v", [128, 4, 4, 65], BF16)
        self.A_vs = scr("A_vs", [128, 4, 65], BF16)
        self.A_z = scr("A_z", [128, 4, 256], BF16)
        self.A_ktm = scr("A_ktm", [128, 4, 256], BF16)
        self.A_PT = scr("A_PT", [128, 4, 128], BF16)
        self.A_y = scr("A_y", [128, 256], BF16)
        self.A_negF = scr("A_negF", [128, SEG])
        self.A_u = scr("A_u", [128, SEG])
        self.A_er = scr("A_er", [128, SEG])
        self.A_cr = scr("A_cr", [128, SEG])
        self.A_h = scr("A_h", [128, 4, 64])
        self.A_sq = scr("A_sq", [128, 4, 64])
        self.A_gz = scr("A_gz", [128, 256])
        self.A_cm = scr("A_cm", [128, 4])
        self.A_ecol = scr("A_ecol", [128, 4, 4])
        self.A_ccol = scr("A_ccol", [128, 4, 4])
        self.A_drow = scr("A_drow", [128, 4])
        self.A_dec = scr("A_dec", [128, 2, 4])
        self.A_dn = scr("A_dn", [128, 8])
        self.rowA = scr("rowA", [128, 256])
        P = self.P
        P.dma(lambda e: e.dma_start(out=self.rowA, in_=self.rowp_d[l][:, 0:256].partition_broadcast(128)), w=["rowA"])
        P.pool(lambda e: e.memset(self.A_v, 1.0), w=["A_v"])
        P.pool(lambda e: e.memset(self.A_qm, 0.0), w=["A_qm"])

    def mixer_A(self, l, sg):
        P = self.P
        self.alloc_A()
        self.scratch_A(l)
        cf = self.cf
        wfm, kfm = self.wnext("A_fm")
        if sg == 0:
            P.dve(lambda e: e.memset(self.A_raw[:, :, 0:16], 0.0), w=["A_raw"])
            P.dve(lambda e: e.memset(self.A_M[:, 0:1], -1e30), w=["A_M"])
            P.dve(lambda e: e.memset(self.A_Fc[:], 0.0), w=["A_Fc"])
            P.dve(lambda e: e.memset(self.A_G[:], 0.0), w=["A_G"])
        else:
            P.dve(lambda e: e.tensor_copy(out=self.A_raw[:, :, 0:16], in_=self.A_halo[:]), r=["A_halo"], w=["A_raw"])
        for j in range(4):
            self.proj_fm(wfm, kfm, 128 * j, (self.A_raw[:, j, 16:16 + SEG], "A_raw"), self.colp[:, 16 + j:17 + j])
        P.dve(lambda e: e.tensor_copy(out=self.A_halo[:], in_=self.A_raw[:, :, SEG:SEG + 16]), r=["A_raw"], w=["A_halo"])
        self.wrel()
        for j in range(4):
            cw = lambda k, j=j: self.colp[:, 32 + 4 * j + k:33 + 4 * j + k]
            P.dve(lambda e, j=j, cw=cw: e.tensor_scalar(out=self.t32a[:], in0=self.A_raw[:, j, 13:13 + SEG], scalar1=cw(0),
                                                        scalar2=None, op0=ALU.mult), r=["A_raw", "colp"], w=["t32a"])
            for k in range(1, 4):
                P.dve(lambda e, j=j, k=k, cw=cw: e.scalar_tensor_tensor(
                    out=self.t32a[:], in0=self.A_raw[:, j, 13 + k:13 + k + SEG], scalar=cw(k), in1=self.t32a[:],
                    op0=ALU.mult, op1=ALU.add), r=["A_raw", "colp", "t32a"], w=["t32a"])
            P.act(lambda e: e.activation(out=self.t32b[:], in_=self.t32a[:], func=AF.Sigmoid), r=["t32a"], w=["t32b"])
            sc_ = 0.125 if j < 2 else 1.0
            P.dve(lambda e, j=j, sc_=sc_: e.scalar_tensor_tensor(out=self.A_qk[:, j, :], in0=self.t32a[:], scalar=sc_,
                                                                 in1=self.t32b[:], op0=ALU.mult, op1=ALU.mult),
                  r=["t32a", "t32b"], w=["A_qk"])
        for hl in range(2):
            po = 64 * hl
            P.dve(lambda e, hl=hl, po=po: e.tensor_copy(out=self.A_qm[po:po + 64, hl::2, :], in_=self.A_qk[po:po + 64, 0:2, :]),
                  r=["A_qk"], w=["A_qm"])
        if l == 0 and sg == 0:
            self.dump("A_qk", self.A_qk[:], [128, 4, SEG], "A_qk")
        wv, kv = self.wnext("A_v")
        wz, kz = self.wnext("A_z")
        for t in range(4):
            pj, pk = self.proj_tm(wv, kv, 256, t, 0)
            P.act(lambda e, pj=pj, t=t: e.activation(out=self.A_v[:, t, :, 0:64],
                                                     in_=pj[:, 0:256].rearrange("p (h d) -> p h d", d=64), func=AF.Copy),
                  r=[pk], w=["A_v"])
            pj, pk = self.proj_tm(wz, kz, 256, t, 256)
            P.act(lambda e, pj=pj, t=t: e.activation(out=self.A_z[:, t, :], in_=pj[:, 0:256], func=AF.Copy),
                  r=[pk], w=["A_z"])
        self.wrel(2)
        wg, kg = self.wnext("A_g")
        self.gate_rows(wg, kg, 0, self.colp[:, 29:30], self.A_u[:], "A_u")
        self.gate_rows(wg, kg, 128, self.colp[:, 30:31], self.t32a[:], "t32a")
        self.wrel()
        self.softplus_neg(self.t32a[:], "t32a")
        P.dve(lambda e: e.memset(self.t32b[:], 1.0), w=["t32b"])
        P.dve(lambda e: e.tensor_tensor_scan(out=self.A_negF[:], data0=self.t32b[:], data1=self.t32a[:],
                                             initial=self.A_Fc[:, 0:1], op0=ALU.mult, op1=ALU.add),
              r=["t32a", "t32b", "A_Fc"], w=["A_negF"])
        P.dve(lambda e: e.tensor_copy(out=self.A_Fc[:], in_=self.A_negF[:, SEG - 1:SEG]), r=["A_negF"], w=["A_Fc"])
        P.dve(lambda e: e.tensor_tensor(out=self.A_u[:], in0=self.A_u[:], in1=self.A_negF[:], op=ALU.add),
              r=["A_u", "A_negF"], w=["A_u"])
        P.dve(lambda e: e.tensor_reduce(out=self.A_cm[:], in_=self.A_u[:].rearrange("p (c s) -> p c s", s=128),
                                        axis=AX.X, op=ALU.max), r=["A_u"], w=["A_cm"])
        P.dve(lambda e: e.tensor_tensor_scan(out=self.A_M[:, 1:5], data0=self.A_neg30[:], data1=self.A_cm[:],
                                             initial=self.A_M[:, 0:1], op0=ALU.max, op1=ALU.max),
              r=["A_neg30", "A_cm", "A_M"], w=["A_M"])
        P.dve(lambda e: e.tensor_tensor(out=self.A_drow[:], in0=self.A_M[:, 0:4], in1=self.A_M[:, 1:5], op=ALU.subtract),
              r=["A_M"], w=["A_drow"])
        P.act(lambda e: e.activation(out=self.A_drow[:], in_=self.A_drow[:], func=AF.Exp), r=["A_drow"], w=["A_drow"])
        Mb = self.A_M[:, 1:5].unsqueeze(2).to_broadcast([128, 4, 128])
        P.dve(lambda e: e.tensor_tensor(out=self.A_er[:].rearrange("p (c s) -> p c s", s=128),
                                        in0=self.A_u[:].rearrange("p (c s) -> p c s", s=128), in1=Mb, op=ALU.subtract),
              r=["A_u", "A_M"], w=["A_er"])
        P.act(lambda e: e.activation(out=self.A_er[:], in_=self.A_er[:], func=AF.Exp), r=["A_er"], w=["A_er"])
        P.dve(lambda e: e.tensor_tensor(out=self.A_cr[:].rearrange("p (c s) -> p c s", s=128),
                                        in0=self.A_negF[:].rearrange("p (c s) -> p c s", s=128), in1=Mb, op=ALU.subtract),
              r=["A_negF", "A_M"], w=["A_cr"])
        P.act(lambda e: e.activation(out=self.A_cr[:], in_=self.A_cr[:], func=AF.Exp), r=["A_cr"], w=["A_cr"])
        if l == 0 and sg == 0:
            self.dump("A_u", self.A_u[:], [128, SEG], "A_u")
            self.dump("A_negF", self.A_negF[:], [128, SEG], "A_negF")
            self.dump("A_M", self.A_M[:, 0:5], [128, 5], "A_M")
            self.dump("A_er", self.A_er[:], [128, SEG], "A_er")
            self.dump("A_drow", self.A_drow[:], [128, 4], "A_drow")
        P.dve(lambda e: e.tensor_copy(out=self.A_M[:, 0:1], in_=self.A_M[:, 4:5]), r=["A_M"], w=["A_M"])
        for t in range(4):
            P.pe(lambda e, t=t: e.transpose(self.tpf[:, 0:128], self.A_er[:, 128 * t:128 * t + 128], cf["ident"]),
                 r=["A_er", "CF"], w=["tpf"])
            P.pe(lambda e, t=t: e.transpose(self.tpf[:, 128:256], self.A_cr[:, 128 * t:128 * t + 128], cf["ident"]),
                 r=["A_cr", "CF"], w=["tpf"])
            P.dve(lambda e, t=t: e.tensor_copy(out=self.A_ecol[:, t, :], in_=self.tpf[:, 0:128:32]), r=["tpf"], w=["A_ecol"])
            P.dve(lambda e, t=t: e.tensor_copy(out=self.A_ccol[:, t, :], in_=self.tpf[:, 128:256:32]), r=["tpf"], w=["A_ccol"])
        for h in range(4):
            hp, hl = h // 2, h % 2
            P.pe(lambda e, h=h, hp=hp, hl=hl: e.matmul(self.tpf[64 * hl:64 * hl + 64, 256 + 4 * hp:260 + 4 * hp],
                                                       lhsT=cf["selb"][:, 64 * h:64 * h + 64],
                                                       rhs=self.A_drow[:, :], start=True, stop=True),
                 r=["CF", "A_drow"], w=["tpf"])
        P.dve(lambda e: e.tensor_copy(out=self.A_dec[:], in_=self.tpf[:, 256:264].rearrange("p (a c) -> p a c", c=4)),
              r=["tpf"], w=["A_dec"])
        if l == 0 and sg == 0:
            self.dump("A_ecol", self.A_ecol[:], [128, 4, 4], "A_ecol")
            self.dump("A_ccol", self.A_ccol[:], [128, 4, 4], "A_ccol")
            self.dump("A_dec", self.A_dec[:], [128, 2, 4], "A_dec")
        for t in range(4):
            self.transpose_to(lambda i, t=t: self.A_qk[:, 2 + i, 128 * t:128 * t + 128], "A_qk",
                              self.A_ktm[:, t, :], "A_ktm", 2, evac="act")
        for t in range(4):
            P.dve(lambda e, t=t: e.tensor_tensor(out=self.A_G[:], in0=self.A_G[:],
                                                 in1=self.A_dec[:, :, t:t + 1].to_broadcast([128, 2, 65]), op=ALU.mult),
                  r=["A_G", "A_dec"], w=["A_G"])
            for hl in range(2):
                po = 64 * hl
                P.act(lambda e, hl=hl, po=po: e.activation(out=self.A_Gb[po:po + 64, hl::2, :], in_=self.A_G[po:po + 64, :, :],
                                                           func=AF.Copy), r=["A_G"], w=["A_Gb"])
            P.dve(lambda e, t=t: e.tensor_tensor(out=self.A_vs[:], in0=self.A_v[:, t, :, :],
                                                 in1=self.A_ecol[:, t, :].unsqueeze(2).to_broadcast([128, 4, 65]),
                                                 op=ALU.mult), r=["A_v", "A_ecol"], w=["A_vs"])
            sc, sk = self.sc[0], ("sc", 0)
            for h in range(4):
                hp, hl = h // 2, h % 2
                po = 64 * hl
                P.pe(lambda e, h=h, hp=hp, po=po, t=t: e.matmul(
                    sc[:, 128 * h:128 * h + 128], lhsT=self.A_qk[:, 2 + hp, 128 * t:128 * t + 128],
                    rhs=self.A_qm[:, h, 128 * t:128 * t + 128], start=True, stop=True), r=["A_qk", "A_qm"], w=[sk])
            P.dve(lambda e: e.tensor_tensor(out=self.A_PT[:], in0=sc[:, :].rearrange("p (h s) -> p h s", s=128),
                                            in1=cf["maskT"].unsqueeze(1).to_broadcast([128, 4, 128]), op=ALU.mult),
                  r=[sk, "CF"], w=["A_PT"])
            for h in range(4):
                hp, hl = h // 2, h % 2
                po = 64 * hl
                P.pe(lambda e, h=h: e.matmul(self.acc[:, 128 * h:128 * h + 65], lhsT=self.A_PT[:, h, :],
                                             rhs=self.A_vs[:, h, :], start=True, stop=False),
                     r=["A_PT", "A_vs"], w=["acc"])
                P.pe(lambda e, h=h, hp=hp, po=po, t=t: e.matmul(
                    self.acc[:, 128 * h:128 * h + 65], lhsT=self.A_qk[:, hp, 128 * t:128 * t + 128],
                    rhs=self.A_Gb[:, h, :], start=False, stop=True), r=["A_qk", "A_Gb"], w=["acc"])
                P.pe(lambda e, h=h, hp=hp, po=po, t=t: e.matmul(
                    self.stp[po:po + 64, 128 * hp:128 * hp + 65], lhsT=self.A_ktm[:, t, 64 * h:64 * h + 64],
                    rhs=self.A_vs[:, h, :], start=True, stop=True), r=["A_ktm", "A_vs"], w=["stp"])
            P.dve(lambda e: e.tensor_tensor(out=self.A_G[:], in0=self.A_G[:],
                                            in1=self.stp[:, 0:256].rearrange("p (a c) -> p a c", c=128)[:, :, 0:65],
                                            op=ALU.add), r=["A_G", "stp"], w=["A_G"])
            accv = self.acc[:, :].rearrange("p (h c) -> p h c", c=128)
            P.dve(lambda e: e.tensor_copy(out=self.A_dn[:, 4:8], in_=accv[:, :, 64]), r=["acc"], w=["A_dn"])
            P.dve(lambda e, t=t: e.scalar_tensor_tensor(out=self.A_dn[:, 0:4], in0=self.A_dn[:, 4:8], scalar=-1.0,
                                                        in1=self.A_dn[:, 4:8], op0=ALU.mult, op1=ALU.max),
                  r=["A_dn"], w=["A_dn"])
            P.dve(lambda e, t=t: e.tensor_tensor(out=self.A_dn[:, 0:4], in0=self.A_dn[:, 0:4], in1=self.A_ccol[:, t, :],
                                                 op=ALU.max), r=["A_dn", "A_ccol"], w=["A_dn"])
            P.dve(lambda e: e.reciprocal(out=self.A_dn[:, 4:8], in_=self.A_dn[:, 0:4]), r=["A_dn"], w=["A_dn"])
            P.dve(lambda e: e.tensor_tensor(out=self.A_h[:], in0=accv[:, :, 0:64],
                                            in1=self.A_dn[:, 4:8].unsqueeze(2).to_broadcast([128, 4, 64]), op=ALU.mult),
                  r=["acc", "A_dn"], w=["A_h"])
            if l == 0 and sg == 0 and t == 1:
                self.dump("A_h", self.A_h[:], [128, 4, 64], "A_h")
                self.dump("A_G", self.A_G[:], [128, 2, 65], "A_G")
                self.dump("A_PT", self.A_PT[:], [128, 4, 128], "A_PT")
                self.dump("A_vs", self.A_vs[:], [128, 4, 65], "A_vs")
            P.dve(lambda e: e.tensor_tensor(out=self.A_sq[:], in0=self.A_h[:], in1=self.A_h[:], op=ALU.mult),
                  r=["A_h"], w=["A_sq"])
            P.dve(lambda e: e.tensor_reduce(out=self.A_dn[:, 0:4], in_=self.A_sq[:], axis=AX.X, op=ALU.add),
                  r=["A_sq"], w=["A_dn"])
            self.rstd_col(self.A_dn[:, 4:8], self.A_dn[:, 0:4], 64, 1e-6, ["A_dn"], "A_dn")
            self.silu_gate(self.A_z[:, t, :], "A_z", self.A_gz[:], "A_gz")
            P.dve(lambda e: e.tensor_tensor(out=self.A_gz[:], in0=self.A_gz[:], in1=self.rowA, op=ALU.mult),
                  r=["A_gz", "rowA"], w=["A_gz"])
            P.dve(lambda e: e.tensor_tensor(out=self.A_h[:], in0=self.A_h[:],
                                            in1=self.A_dn[:, 4:8].unsqueeze(2).to_broadcast([128, 4, 64]), op=ALU.mult),
                  r=["A_h", "A_dn"], w=["A_h"])
            P.dve(lambda e: e.tensor_tensor(out=self.A_y[:], in0=self.A_h[:].rearrange("p h d -> p (h d)"),
                                            in1=self.A_gz[:], op=ALU.mult), r=["A_h", "A_gz"], w=["A_y"])
            self.y_to_yT(self.A_y, "A_y", t, 0)

    def alloc_B(self):
        if hasattr(self, "B_halo"):
            return
        self.B_halo = self.sb("B_halo", [128, 2, 16])

    def scratch_B(self, l):
        self.scr_reset()
        scr = self.scr
        self.B_x = scr("B_x", [128, 2, 16 + SEG])
        self.B_s = scr("B_s0", [128, 16 + SEG])
        self.B_s2 = scr("B_s1", [128, 16 + SEG])
        self.B_p = scr("B_p", [128, 2, SEG], BF16)
        self.B_z = scr("B_z", [128, 256])
        self.B_y = scr("B_y", [128, 256], BF16)
        self.rowB = scr("rowB", [128, 256])
        self.P.dma(lambda e: e.dma_start(out=self.rowB, in_=self.rowp_d[l][:, 256:512].partition_broadcast(128)), w=["rowB"])

    def mixer_B(self, l, sg):
        P = self.P
        self.alloc_B()
        self.scratch_B(l)
        cf = self.cf
        w, wk = self.wnext("B")
        if sg == 0:
            P.dve(lambda e: e.memset(self.B_x[:, :, 0:16], 0.0), w=["B_x"])
        else:
            P.dve(lambda e: e.tensor_copy(out=self.B_x[:, :, 0:16], in_=self.B_halo[:]), r=["B_halo"], w=["B_x"])
        for j in range(2):
            self.proj_fm(w, wk, 128 * j, (self.B_x[:, j, 16:16 + SEG], "B_x"), self.colp[:, 20 + j:21 + j])
        P.dve(lambda e: e.tensor_copy(out=self.B_halo[:], in_=self.B_x[:, :, SEG:SEG + 16]), r=["B_x"], w=["B_halo"])
        wins = (2, 4, 8, 16)
        for j in range(2):
            x = self.B_x[:, j, :]
            bufs = [self.B_s, self.B_s2]
            cur = x
            lvl = {}
            step = 1
            for i in range(4):
                dst = bufs[i % 2]
                n = SEG + 16 - step if i == 0 else SEG + 16 - step
                P.dve(lambda e, dst=dst, cur=cur, step=step: e.memset(dst[:, 0:step], 0.0), w=["B_s%d" % (i % 2)])
                P.dve(lambda e, dst=dst, cur=cur, step=step: e.tensor_tensor(
                    out=dst[:, step:SEG + 16], in0=cur[:, step:SEG + 16], in1=cur[:, 0:SEG + 16 - step], op=ALU.add),
                    r=["B_x", "B_s0", "B_s1"], w=["B_s%d" % (i % 2)])
                for gl in range(2):
                    g = 2 * j + gl
                    if wins[g] == 2 * step:
                        win = wins[g]
                        po = 64 * gl
                        P.dve(lambda e, dst=dst, po=po, win=win, j=j: e.scalar_tensor_tensor(
                            out=self.B_p[po:po + 64, j, :], in0=dst[po:po + 64, 16:16 + SEG], scalar=1.0 / win,
                            in1=self.B_x[po:po + 64, j, 16:16 + SEG], op0=ALU.mult, op1=ALU.subtract),
                            r=["B_s%d" % (i % 2), "B_x"], w=["B_p"])
                        if sg == 0:
                            P.dve(lambda e, dst=dst, po=po, win=win: e.tensor_tensor(
                                out=self.t32a[po:po + 64, 0:win - 1], in0=dst[po:po + 64, 16:16 + win - 1],
                                in1=cf["invc"][po:po + 64, 0:win - 1], op=ALU.mult), r=["B_s%d" % (i % 2), "CF"], w=["t32a"])
                            P.dve(lambda e, po=po, win=win, j=j: e.tensor_tensor(
                                out=self.B_p[po:po + 64, j, 0:win - 1], in0=self.t32a[po:po + 64, 0:win - 1],
                                in1=self.B_x[po:po + 64, j, 16:16 + win - 1], op=ALU.subtract),
                                r=["t32a", "B_x"], w=["B_p"])
                cur = dst
                step *= 2
        wz, kz = w, wk
        for t in range(4):
            pj, pk = self.nextpj()
            for j in range(2):
                P.pe(lambda e, j=j, t=t, pj=pj: e.matmul(
                    pj[:, 128 * j:128 * j + 128], lhsT=self.B_p[:, j, 128 * t:128 * t + 128],
                    rhs=self.wsm[:, 128 * j:128 * j + 128], start=True, stop=True), r=["B_p", "wsm"], w=[pk])
            pz, pzk = self.nextpj()
            for dc in range(8):
                P.pe(lambda e, dc=dc, pz=pz, t=t: e.matmul(pz[:, 0:256], lhsT=self.hT[:, dc, 128 * t:128 * t + 128],
                                                           rhs=w[:, dc, 256:512], start=(dc == 0), stop=False),
                     r=["hT", wk], w=[pzk])
            P.pe(lambda e, pz=pz: e.matmul(pz[:, 0:256], lhsT=self.onesb[0:1, :], rhs=self.brow[0:1, 512:768],
                                           start=False, stop=True), r=["CB", "brow"], w=[pzk])
            P.act(lambda e, pz=pz: e.activation(out=self.B_z[:], in_=pz[:, 0:256], func=AF.Sigmoid), r=[pzk], w=["B_z"])
            P.dve(lambda e, pz=pz: e.tensor_tensor(out=self.B_z[:], in0=self.B_z[:], in1=pz[:, 0:256], op=ALU.mult),
                  r=[pzk, "B_z"], w=["B_z"])
            P.dve(lambda e: e.tensor_tensor(out=self.B_z[:], in0=self.B_z[:], in1=self.rowB, op=ALU.mult),
                  r=["B_z", "rowB"], w=["B_z"])
            P.dve(lambda e, pj=pj: e.tensor_tensor(out=self.B_y[:], in0=pj[:, 0:256], in1=self.B_z[:], op=ALU.mult),
                  r=[pk, "B_z"], w=["B_y"])
            self.y_to_yT(self.B_y, "B_y", t, 2)
        self.wrel()

    def alloc_C(self):
        if hasattr(self, "C_halo"):
            return
        self.C_halo = self.sb("C_halo", [128, 7, 2], BF16)
        self.C_S = self.sb("C_S", [128, 2, 3, 64], BF16)
        self.C_slot = [0, 0]

    def mixer_C(self, l, sg):
        P = self.P
        cf = self.cf
        self.alloc_C()
        self.scr_reset()
        scr = self.scr
        craw = scr("C_raw", [128, 514], BF16)
        tlw = scr("C_tlw", [128, 512], BF16)
        tla = scr("C_tla", [128, 512], BF16)
        arTm = scr("C_arTm", [128, 2, 2, 512], BF16)
        aTf = scr("C_aTf", [128, 512], BF16)
        bT = scr("C_bT", [128, 512], BF16)
        kT = scr("C_kT", [128, 512], BF16)
        tmpA = scr("C_tmpA", [128, 512], BF16)
        tmpB = scr("C_tmpB", [128, 512], BF16)
        a_tm = scr("C_atm", [128, 4, 128], BF16)
        v_tm = scr("C_vtm", [128, 4, 128], BF16)
        bhm = scr("C_bhm", [128, 4, 2, 128], BF16)
        khm = scr("C_khm", [128, 4, 2, 128], BF16)
        AT4 = scr("C_AT4", [128, 4, 128], BF16)
        X0 = scr("C_X0", [128, 128], BF16)
        XX = scr("C_XX", [128, 2, 256], BF16)
        Bb = scr("C_Bb", [128, 2, 128], BF16)
        R1T = scr("C_R1T", [128, 2, 128], BF16)
        Phi = scr("C_Phi", [128, 2, 2, 64], BF16)
        cz = scr("C_z", [128, 4, 256], BF16)
        C_y = scr("C_y", [128, 128], BF16)
        T = [scr("C_T%d" % i, [128, 512]) for i in range(8)]
        TK = ["C_T%d" % i for i in range(8)]
        T1, T2, T3, T4, T5, T6, T7, T8 = T
        K1, K2, K3, K4, K5, K6, K7, K8 = TK
        WT = scr("C_WT", [128, 8])
        Z64 = scr("C_Z64", [128, 64])
        bon = scr("C_bon", [128, 4, 2])
        gst = scr("C_gst", [128, 16])
        gy = scr("C_gy", [128, 2, 64])
        gq = scr("C_gq", [128, 2, 64])
        rowC = scr("C_row", [128, 512])
        P.dma(lambda e: e.dma_start(out=rowC, in_=self.rowp_d[l][:, 768:1280].partition_broadcast(128)), w=["C_row"])
        for ap_, k_ in ((tlw, "C_tlw"), (tla, "C_tla"), (arTm, "C_arTm"), (bhm, "C_bhm"), (khm, "C_khm"),
                        (R1T, "C_R1T"), (Phi, "C_Phi")):
            P.pool(lambda e, ap_=ap_: e.memset(ap_, 0.0), w=[k_])
        P.dve(lambda e: e.memset(Z64, 0.0), w=["C_Z64"])
        if sg == 0:
            P.dve(lambda e: e.memset(self.C_S[:], 0.0), w=["C_S"])
            self.C_slot = [0, 0]
        wz, kz = self.wnext("C_z")
        for t in range(4):
            pj, pk = self.proj_tm(wz, kz, 256, t, 768)
            P.act(lambda e, pj=pj, t=t: e.activation(out=cz[:, t, :], in_=pj[:, 0:256], func=AF.Copy), r=[pk], w=["C_z"])
        self.wrel()
        wrk, krk = self.wnext("C_rk")
        wvw, kvw = self.wnext("C_vw")

        def shifted(j, out32, okey):
            w, wk, c0 = (wrk, krk, 128 * j) if j < 4 else (wvw, kvw, 128 * (j - 4))
            if sg == 0:
                P.dve(lambda e: e.memset(craw[:, 0:1], 0.0), w=["C_raw"])
            else:
                P.dve(lambda e: e.tensor_copy(out=craw[:, 0:1], in_=self.C_halo[:, j, 0:1]), r=["C_halo"], w=["C_raw"])
            self.proj_fm(w, wk, c0, (craw[:, 1:513], "C_raw"), self.colp[:, 22 + j:23 + j])
            P.dve(lambda e: e.tensor_copy(out=self.C_halo[:, j, 0:1], in_=craw[:, 512:513]), r=["C_raw"], w=["C_halo"])
            P.dve(lambda e: e.tensor_tensor(out=out32, in0=craw[:, 0:512], in1=craw[:, 1:513], op=ALU.subtract),
                  r=["C_raw"], w=[okey])
            P.dve(lambda e: e.scalar_tensor_tensor(out=out32, in0=out32, scalar=self.colp[:, 48 + j:49 + j],
                                                   in1=craw[:, 1:513], op0=ALU.mult, op1=ALU.add),
                  r=["C_raw", "colp", okey], w=[okey])

        shifted(6, T1, K1)
        P.act(lambda e: e.activation(out=tlw[0:64, :], in_=T1[0:64, :], func=AF.Tanh), r=[K1], w=["C_tlw"])
        P.act(lambda e: e.activation(out=tla[64:128, :], in_=T1[64:128, :], func=AF.Copy), r=[K1], w=["C_tla"])
        for ct in range(2):
            W2 = self.wsm[:, 256 + 128 * ct:256 + 128 * ct + 128]
            pj, pk = self.nextpj()
            P.pe(lambda e, pj=pj, W2=W2: e.matmul(pj[:], lhsT=W2, rhs=tlw, start=True, stop=True), r=["wsm", "C_tlw"], w=[pk])
            P.act(lambda e, pj=pj, ct=ct: e.activation(out=T1, in_=pj[:], func=AF.Sigmoid, bias=self.colp[:, 55 + ct:56 + ct]),
                  r=[pk, "colp"], w=[K1])
            P.dve(lambda e: e.tensor_scalar(out=T1, in0=T1, scalar1=-0.6065306597126334, scalar2=None, op0=ALU.mult),
                  r=[K1], w=[K1])
            for c8 in range(8):
                P.dve(lambda e, c8=c8: e.tensor_tensor_scan(out=T2[:, 64 * c8:64 * c8 + 64], data0=Z64,
                                                            data1=T1[:, 64 * c8:64 * c8 + 64], initial=0.0,
                                                            op0=ALU.add, op1=ALU.add), r=[K1, "C_Z64"], w=[K2])
            P.dve(lambda e: e.tensor_tensor(out=T3, in0=T2, in1=T1, op=ALU.subtract), r=[K1, K2], w=[K3])
            P.act(lambda e: e.activation(out=T3, in_=T3, func=AF.Exp), r=[K3], w=[K3])
            P.act(lambda e: e.activation(out=WT, in_=T2[:, 63:512:64], func=AF.Exp), r=[K2], w=["C_WT"])
            P.act(lambda e: e.activation(out=T1, in_=T2, func=AF.Exp), r=[K2], w=[K1])
            P.act(lambda e: e.activation(out=T4, in_=T2, func=AF.Exp, scale=-1.0), r=[K2], w=[K4])
            P.dve(lambda e: e.tensor_tensor(out=T2.rearrange("p (c s) -> p c s", s=64),
                                            in0=T4.rearrange("p (c s) -> p c s", s=64),
                                            in1=WT.unsqueeze(2).to_broadcast([128, 8, 64]), op=ALU.mult),
                  r=[K4, "C_WT"], w=[K2])
            pj, pk = self.nextpj()
            P.pe(lambda e, pj=pj, W2=W2: e.matmul(pj[:], lhsT=W2, rhs=tla, start=True, stop=True), r=["wsm", "C_tla"], w=[pk])
            P.act(lambda e, pj=pj, ct=ct: e.activation(out=T5, in_=pj[:], func=AF.Sigmoid, bias=self.colp[:, 57 + ct:58 + ct]),
                  r=[pk, "colp"], w=[K5])
            shifted(2 + ct, T6, K6)
            P.dve(lambda e, ct=ct: e.tensor_scalar(out=T7, in0=T6, scalar1=self.colp[:, 59 + ct:60 + ct], scalar2=None,
                                                   op0=ALU.mult), r=[K6, "colp"], w=[K7])
            P.act(lambda e: e.activation(out=tmpA, in_=T7, func=AF.Square), r=[K7], w=["C_tmpA"])
            pj, pk = self.nextpj()
            P.pe(lambda e, pj=pj: e.matmul(pj[:], lhsT=self.blk64b, rhs=tmpA, start=True, stop=True), r=["CB", "C_tmpA"], w=[pk])
            P.act(lambda e, pj=pj: e.activation(out=T8, in_=pj[:], func=AF.Sqrt), r=[pk], w=[K8])
            P.dve(lambda e: e.tensor_scalar(out=T8, in0=T8, scalar1=1e-12, scalar2=None, op0=ALU.max), r=[K8], w=[K8])
            P.dve(lambda e: e.reciprocal(out=T8, in_=T8), r=[K8], w=[K8])
            P.dve(lambda e: e.tensor_tensor(out=T7, in0=T7, in1=T8, op=ALU.mult), r=[K7, K8], w=[K7])
            P.dve(lambda e: e.scalar_tensor_tensor(out=aTf, in0=T7, scalar=-1.0, in1=T3, op0=ALU.mult, op1=ALU.mult),
                  r=[K7, K3], w=["C_aTf"])
            for hl in range(2):
                po = 64 * hl
                P.act(lambda e, hl=hl, po=po: e.activation(out=arTm[po:po + 64, hl, 0, :], in_=aTf[po:po + 64, :], func=AF.Copy),
                      r=["C_aTf"], w=["C_arTm"])
            P.dve(lambda e: e.tensor_tensor(out=T8, in0=T7, in1=T5, op=ALU.mult), r=[K7, K5], w=[K8])
            P.dve(lambda e: e.tensor_tensor(out=bT, in0=T8, in1=T4, op=ALU.mult), r=[K8, K4], w=["C_bT"])
            P.dve(lambda e: e.tensor_tensor(out=tmpA, in0=T8, in1=T2, op=ALU.mult), r=[K8, K2], w=["C_tmpA"])

            def to_masked_tm(srcT, skey, dst, dkey):
                for t in range(4):
                    P.pe(lambda e, t=t: e.transpose(self.tp[:, 128 * t:128 * t + 128], srcT[:, 128 * t:128 * t + 128], self.identb),
                         r=[skey, "CB"], w=["tp"])
                for c in range(2):
                    P.act(lambda e, c=c: e.activation(out=dst[64 * c:64 * c + 64, :, c, :],
                                                      in_=self.tp[64 * c:64 * c + 64, 0:512].rearrange("p (t n) -> p t n", n=128),
                                                      func=AF.Copy), r=["tp"], w=[dkey])

            def to_tm(srcT, skey, dst, dkey):
                for t in range(4):
                    P.pe(lambda e, t=t: e.transpose(self.tp[:, 128 * t:128 * t + 128], srcT[:, 128 * t:128 * t + 128], self.identb),
                         r=[skey, "CB"], w=["tp"])
                P.act(lambda e: e.activation(out=dst, in_=self.tp[:, 0:512].rearrange("p (t n) -> p t n", n=128), func=AF.Copy),
                      r=["tp"], w=[dkey])

            to_masked_tm(tmpA, "C_tmpA", bhm, "C_bhm")
            P.dve(lambda e, ct=ct: e.tensor_scalar(out=T5, in0=T5, scalar1=1.0, scalar2=self.colp[:, 61 + ct:62 + ct],
                                                   op0=ALU.subtract, op1=ALU.mult), r=[K5, "colp"], w=[K5])
            P.dve(lambda e: e.scalar_tensor_tensor(out=T5, in0=T5, scalar=1.0, in1=T6, op0=ALU.add, op1=ALU.mult),
                  r=[K5, K6], w=[K5])
            P.dve(lambda e: e.tensor_tensor(out=kT, in0=T5, in1=T4, op=ALU.mult), r=[K5, K4], w=["C_kT"])
            P.dve(lambda e: e.tensor_tensor(out=tmpB, in0=T5, in1=T2, op=ALU.mult), r=[K5, K2], w=["C_tmpB"])
            to_masked_tm(tmpB, "C_tmpB", khm, "C_khm")
            to_tm(aTf, "C_aTf", a_tm, "C_atm")
            shifted(ct, T6, K6)
            for hl in range(2):
                po = 64 * hl
                P.dve(lambda e, hl=hl, po=po: e.tensor_tensor(out=arTm[po:po + 64, hl, 1, :], in0=T6[po:po + 64, :],
                                                              in1=T1[po:po + 64, :], op=ALU.mult), r=[K6, K1], w=["C_arTm"])
            P.dve(lambda e, ct=ct: e.scalar_tensor_tensor(out=tmpA, in0=T6, scalar=self.colx[:, ct:ct + 1], in1=T5,
                                                          op0=ALU.mult, op1=ALU.mult), r=[K6, K5, "colx"], w=["C_tmpA"])
            pj, pk = self.nextpj()
            for t in range(4):
                P.pe(lambda e, t=t, pj=pj: e.matmul(pj[:, 2 * t:2 * t + 2], lhsT=tmpA[:, 128 * t:128 * t + 128], rhs=self.hselb,
                                                    start=True, stop=True), r=["C_tmpA", "CB"], w=[pk])
            P.dve(lambda e, pj=pj: e.tensor_copy(out=bon, in_=pj[:, 0:8].rearrange("p (t h) -> p t h", h=2)), r=[pk], w=["C_bon"])
            shifted(4 + ct, T6, K6)
            P.act(lambda e: e.activation(out=tmpB, in_=T6, func=AF.Copy), r=[K6], w=["C_tmpB"])
            to_tm(tmpB, "C_tmpB", v_tm, "C_vtm")
            if l == 0 and sg == 0 and ct == 0:
                self.dump("C_aTf", aTf, [128, 512], "C_aTf")
                self.dump("C_bT", bT, [128, 512], "C_bT")
                self.dump("C_kT", kT, [128, 512], "C_kT")
                self.dump("C_arTm", arTm, [128, 2, 2, 512], "C_arTm")
                self.dump("C_bhm", bhm, [128, 4, 2, 128], "C_bhm")
                self.dump("C_khm", khm, [128, 4, 2, 128], "C_khm")
                self.dump("C_vtm", v_tm, [128, 4, 128], "C_vtm")
                self.dump("C_atm", a_tm, [128, 4, 128], "C_atm")
                self.dump("C_WT", WT, [128, 8], "C_WT")
                self.dump("C_bon", bon, [128, 4, 2], "C_bon")
            for t in range(4):
                ts_ = slice(128 * t, 128 * t + 128)
                s0 = self.C_slot[ct]
                Sin = [self.C_S[:, ct, (s0 + i) % 3, :] for i in range(3)]
                for hl in range(2):
                    po = 64 * hl
                    h = 2 * ct + hl
                    sc0, sk0 = self.sc[0], ("sc", 0)
                    sc1, sk1 = self.sc[1], ("sc", 1)
                    rhs_ar = arTm[:, hl, :, ts_]
                    P.pe(lambda e, rhs_ar=rhs_ar, ts_=ts_: e.matmul(sc0[:, 0:256], lhsT=bT[:, ts_], rhs=rhs_ar, start=True, stop=True),
                         r=["C_bT", "C_arTm"], w=[sk0])
                    P.pe(lambda e, rhs_ar=rhs_ar, ts_=ts_: e.matmul(sc0[:, 256:512], lhsT=kT[:, ts_], rhs=rhs_ar, start=True, stop=True),
                         r=["C_kT", "C_arTm"], w=[sk0])
                    P.pe(lambda e, hl=hl, ts_=ts_: e.matmul(sc1[:, 0:128], lhsT=arTm[:, hl, 0, ts_], rhs=bT[:, ts_], start=True, stop=True),
                         r=["C_bT", "C_arTm"], w=[sk1])
                    sc0v = sc0[:, :].rearrange("p (b n) -> p b n", n=128)
                    P.dve(lambda e, sc0v=sc0v: e.tensor_tensor(out=AT4[:, 0:4:2, :], in0=sc0v[:, 0:4:2, :],
                                                               in1=cf["mS"].unsqueeze(1).to_broadcast([128, 2, 128]), op=ALU.mult),
                          r=[sk0, "CF"], w=["C_AT4"])
                    P.dve(lambda e, sc0v=sc0v: e.tensor_tensor(out=AT4[:, 1:4:2, :], in0=sc0v[:, 1:4:2, :],
                                                               in1=cf["mI"].unsqueeze(1).to_broadcast([128, 2, 128]), op=ALU.mult),
                          r=[sk0, "CF"], w=["C_AT4"])
                    P.dve(lambda e: e.tensor_tensor(out=X0, in0=sc1[:, 0:128], in1=cf["mSt"], op=ALU.mult), r=[sk1, "CF"], w=["C_X0"])
                    P.pe(lambda e, t=t, po=po: e.matmul(self.stp[:, 0:64], lhsT=AT4[:, 2, :], rhs=v_tm[:, t, po:po + 64],
                                                        start=True, stop=True), r=["C_AT4", "C_vtm"], w=["stp"])
                    P.act(lambda e, t=t, po=po: e.activation(out=Bb[:, 0, 0:64], in_=a_tm[:, t, po:po + 64], func=AF.Copy),
                          r=["C_atm"], w=["C_Bb"])
                    P.act(lambda e: e.activation(out=Bb[:, 0, 64:128], in_=self.stp[:, 0:64], func=AF.Copy), r=["stp"], w=["C_Bb"])
                    Xc, XTc = X0, AT4[:, 0, :]
                    xk, xtk = "C_X0", "C_AT4"
                    for i in range(6):
                        bi, bo = i % 2, (i + 1) % 2
                        P.pe(lambda e, XTc=XTc, bi=bi: e.matmul(self.stp[:, 128:256], lhsT=XTc, rhs=Bb[:, bi, :], start=True, stop=True),
                             r=[xtk, "C_Bb"], w=["stp"])
                        P.dve(lambda e, bi=bi, bo=bo: e.tensor_tensor(out=Bb[:, bo, :], in0=self.stp[:, 128:256], in1=Bb[:, bi, :],
                                                                      op=ALU.add), r=["stp", "C_Bb"], w=["C_Bb"])
                        if i < 5:
                            P.pe(lambda e, XTc=XTc, Xc=Xc: e.matmul(sc1[:, 128:256], lhsT=XTc, rhs=Xc, start=True, stop=True),
                                 r=[xk, xtk], w=[sk1])
                            P.pe(lambda e, XTc=XTc, Xc=Xc: e.matmul(sc1[:, 256:384], lhsT=Xc, rhs=XTc, start=True, stop=True),
                                 r=[xk, xtk], w=[sk1])
                            xi = i % 2
                            P.act(lambda e, xi=xi: e.activation(out=XX[:, xi, :], in_=sc1[:, 128:384], func=AF.Copy),
                                  r=[sk1], w=[("C_XX", xi)])
                            Xc, XTc = XX[:, xi, 0:128], XX[:, xi, 128:256]
                            xk = xtk = ("C_XX", xi)
                    Pf = Bb[:, 0, :]
                    if l == 0 and sg == 0 and ct == 0 and t == 0 and hl == 0:
                        self.dump("C_AT4", AT4, [128, 4, 128], "C_AT4")
                        self.dump("C_X0", X0, [128, 128], "C_X0")
                        self.dump("C_Pf", Pf, [128, 128], "C_Bb")
                    P.pe(lambda e, po=po: e.matmul(self.stp[po:po + 64, 256:384], lhsT=Pf[:, 0:64], rhs=AT4[:, 1, :],
                                                   start=True, stop=True), r=["C_Bb", "C_AT4"], w=["stp"])
                    P.dve(lambda e, po=po, hl=hl, ts_=ts_: e.tensor_tensor(out=R1T[po:po + 64, hl, :], in0=self.stp[po:po + 64, 256:384],
                                                                           in1=arTm[po:po + 64, hl, 1, ts_], op=ALU.add),
                          r=["stp", "C_arTm"], w=["C_R1T"])
                    P.pe(lambda e, hl=hl: e.matmul(self.acc[:, 64 * hl:64 * hl + 64], lhsT=AT4[:, 1, :], rhs=Pf[:, 64:128],
                                                   start=True, stop=False, skip_group_check=True), r=["C_AT4", "C_Bb"], w=["acc"])
                    P.pe(lambda e, hl=hl, t=t, po=po: e.matmul(self.acc[:, 64 * hl:64 * hl + 64], lhsT=AT4[:, 3, :],
                                                               rhs=v_tm[:, t, po:po + 64], start=False, stop=False,
                                                               skip_group_check=True), r=["C_AT4", "C_vtm"], w=["acc"])
                    for c in range(2):
                        P.pe(lambda e, po=po, c=c, t=t: e.matmul(self.stp[po:po + 64, 384 + 64 * c:448 + 64 * c], lhsT=Pf[:, 0:64],
                                                                 rhs=bhm[:, t, c, po:po + 64], start=True, stop=True),
                             r=["C_Bb", "C_bhm"], w=["stp"])
                        P.dve(lambda e, po=po, c=c, hl=hl, t=t: e.scalar_tensor_tensor(
                            out=Phi[po:po + 64, hl, c, :], in0=cf["ident"][po:po + 64, po:po + 64],
                            scalar=WT[po:po + 64, 2 * t + c:2 * t + c + 1], in1=self.stp[po:po + 64, 384 + 64 * c:448 + 64 * c],
                            op0=ALU.mult, op1=ALU.add), r=["CF", "C_WT", "stp"], w=["C_Phi"])
                        P.pe(lambda e, hl=hl, c=c, Sin=Sin: e.matmul(self.acc[64 * c:64 * c + 64, 64 * hl:64 * hl + 64],
                                                            lhsT=R1T[:, hl, 64 * c:64 * c + 64], rhs=Sin[c], start=False,
                                                            stop=(c == 1), skip_group_check=True),
                             r=["C_R1T", ("C_S", ct)], w=["acc"])
                        P.pe(lambda e, po=po, hl=hl, c=c, Sin=Sin: e.matmul(self.tpf[po:po + 64, 64 * c:64 * c + 64], lhsT=Phi[:, hl, c, :],
                                                                   rhs=Sin[c], start=True, stop=False, skip_group_check=True),
                             r=["C_Phi", ("C_S", ct)], w=["tpf"])
                        P.pe(lambda e, po=po, c=c, t=t: e.matmul(self.tpf[po:po + 64, 64 * c:64 * c + 64], lhsT=bhm[:, t, c, po:po + 64],
                                                                 rhs=Pf[:, 64:128], start=False, stop=False, skip_group_check=True),
                             r=["C_bhm", "C_Bb"], w=["tpf"])
                        P.pe(lambda e, po=po, c=c, t=t: e.matmul(self.tpf[po:po + 64, 64 * c:64 * c + 64], lhsT=khm[:, t, c, po:po + 64],
                                                                 rhs=v_tm[:, t, po:po + 64], start=False, stop=True,
                                                                 skip_group_check=True), r=["C_khm", "C_vtm"], w=["tpf"])
                        P.act(lambda e, po=po, c=c, Sin=Sin: e.activation(out=Sin[c + 1][po:po + 64, :], in_=self.tpf[po:po + 64, 64 * c:64 * c + 64],
                                                                 func=AF.Copy), r=["tpf"], w=[("C_S", ct)])
                        if l == 0 and sg == 0 and ct == 0 and t == 0 and hl == 0 and c == 0:
                            P.act(lambda e: e.activation(out=gy.rearrange("p h d -> p (h d)")[0:64, 0:64], in_=self.tpf[0:64, 0:64], func=AF.Copy),
                                  r=["tpf"], w=["C_gy"])
                            self.dump("C_tpf", gy.rearrange("p h d -> p (h d)")[0:64, 0:64], [64, 64], "C_gy")
                            self.dump("C_S1", Sin[1][0:64, :], [64, 64], ("C_S", 0))
                if l == 0 and sg == 0 and ct == 0 and t == 0:
                    self.dump("C_Phi", Phi, [128, 2, 2, 64], "C_Phi")
                    self.dump("C_R1T", R1T, [128, 2, 128], "C_R1T")
                    self.dump("C_S", self.C_S[:, 0, :, :], [128, 3, 64], ("C_S", 0))
                    self.dump("C_Y", gy, [128, 2, 64], "C_gy") if False else None
                self.C_slot[ct] = (s0 + 2) % 3
                accv = self.acc[:, 0:128].rearrange("p (h d) -> p h d", d=64)
                P.dve(lambda e: e.tensor_reduce(out=gst[:, 0:2], in_=accv, axis=AX.X, op=ALU.add), r=["acc"], w=["C_gst"])
                P.act(lambda e: e.activation(out=gq, in_=accv, func=AF.Square), r=["acc"], w=["C_gq"])
                P.dve(lambda e: e.tensor_reduce(out=gst[:, 2:4], in_=gq, axis=AX.X, op=ALU.add), r=["C_gq"], w=["C_gst"])
                P.dve(lambda e: e.tensor_scalar(out=gst[:, 4:6], in0=gst[:, 0:2], scalar1=1.0 / 64, scalar2=None, op0=ALU.mult),
                      r=["C_gst"], w=["C_gst"])
                P.dve(lambda e: e.tensor_tensor(out=gst[:, 6:8], in0=gst[:, 4:6], in1=gst[:, 4:6], op=ALU.mult),
                      r=["C_gst"], w=["C_gst"])
                P.dve(lambda e: e.scalar_tensor_tensor(out=gst[:, 8:10], in0=gst[:, 2:4], scalar=1.0 / 64, in1=gst[:, 6:8],
                                                       op0=ALU.mult, op1=ALU.subtract), r=["C_gst"], w=["C_gst"])
                P.act(lambda e: e.activation(out=gst[:, 10:12], in_=gst[:, 8:10], func=AF.Sqrt, bias=self.epsc[:, 1:2]),
                      r=["C_gst", "epsc"], w=["C_gst"])
                P.dve(lambda e: e.reciprocal(out=gst[:, 10:12], in_=gst[:, 10:12]), r=["C_gst"], w=["C_gst"])
                P.dve(lambda e: e.tensor_tensor(out=gy, in0=accv, in1=gst[:, 4:6].unsqueeze(2).to_broadcast([128, 2, 64]),
                                                op=ALU.subtract), r=["acc", "C_gst"], w=["C_gy"])
                P.dve(lambda e: e.tensor_tensor(out=gy, in0=gy, in1=gst[:, 10:12].unsqueeze(2).to_broadcast([128, 2, 64]),
                                                op=ALU.mult), r=["C_gy", "C_gst"], w=["C_gy"])
                gy2 = gy.rearrange("p h d -> p (h d)")
                P.dve(lambda e, ct=ct: e.tensor_tensor(out=gy2, in0=gy2, in1=rowC[:, 128 * ct:128 * ct + 128], op=ALU.mult),
                      r=["C_gy", "C_row"], w=["C_gy"])
                P.dve(lambda e, ct=ct: e.tensor_tensor(out=gy2, in0=gy2, in1=rowC[:, 256 + 128 * ct:256 + 128 * ct + 128], op=ALU.add),
                      r=["C_gy", "C_row"], w=["C_gy"])
                P.dve(lambda e, t=t: e.tensor_tensor(out=gq, in0=v_tm[:, t, :].rearrange("p (h d) -> p h d", d=64),
                                                     in1=bon[:, t, :].unsqueeze(2).to_broadcast([128, 2, 64]), op=ALU.mult),
                      r=["C_vtm", "C_bon"], w=["C_gq"])
                P.dve(lambda e: e.tensor_tensor(out=gy, in0=gy, in1=gq, op=ALU.add), r=["C_gy", "C_gq"], w=["C_gy"])
                gq2 = gq.rearrange("p h d -> p (h d)")
                self.silu_gate(cz[:, t, 128 * ct:128 * ct + 128], "C_z", gq2, "C_gq")
                P.dve(lambda e: e.tensor_tensor(out=C_y, in0=gy2, in1=gq2, op=ALU.mult), r=["C_gy", "C_gq"], w=["C_y"])
                self.transpose_to(lambda i: C_y, "C_y", self.yT[:, 4 + ct:5 + ct, 128 * t:128 * t + 128], "yT", 1, evac="act")
        self.wrel(2)

    def alloc_D(self):
        if hasattr(self, "D_K"):
            return
        sb = self.sb
        self.D_K = sb("D_K", [65, 4, S], BF16)
        self.D_V = sb("D_V", [128, 16, 4, 65], BF16)
        self.D_Fc = sb("D_Fc", [128, 1])
        self.D_fcol = sb("D_fcol", [128, 16, 4])
        P = self.P
        P.dve(lambda e: e.memset(self.D_K[64:65, :, :], 1.0), w=["D_K"])
        P.dve(lambda e: e.memset(self.D_V[:], 1.0), w=["D_V"])

    def scratch_D(self, l):
        self.scr_reset()
        scr = self.scr
        self.D_Q = scr("D_Q", [65, 4, SEG], BF16)
        self.D_z = scr("D_z", [128, 4, 256], BF16)
        self.D_qn = scr("D_qn", [128, 512], BF16)
        self.D_y = scr("D_y", [128, 256], BF16)
        self.PT = scr("PT", [128, 2, 512], BF16)
        self.D_negF = scr("D_negF", [128, SEG])
        self.D_ss = scr("D_ss", [128, 16])
        self.D_o = scr("D_o", [128, 4, 4, 64])
        self.D_rd = scr("D_rd", [128, 4])
        self.rowD = scr("rowD", [128, 512])
        self.P.dma(lambda e: e.dma_start(out=self.rowD, in_=self.rowp_d[l][:, 1280:1792].partition_broadcast(128)), w=["rowD"])

    def mixer_D(self, l, sg):
        P = self.P
        self.alloc_D()
        self.scratch_D(l)
        cf = self.cf
        wqk, kqk = self.wnext("D_qk")
        if sg == 0:
            P.dve(lambda e: e.memset(self.D_Fc[:], 0.0), w=["D_Fc"])
        for t in range(4):
            tt = 4 * sg + t
            pj, pk = self.proj_tm(wqk, kqk, 512, t, 1024)
            P.act(lambda e, pj=pj: e.activation(out=self.t32a[:], in_=pj[:], func=AF.Copy), r=[pk], w=["t32a"])
            if KD <= 1:
                continue
            P.dve(lambda e: e.tensor_tensor(out=self.t32b[:], in0=self.t32a[:], in1=self.t32a[:], op=ALU.mult),
                  r=["t32a"], w=["t32b"])
            P.dve(lambda e: e.tensor_reduce(out=self.D_ss[:, 0:8], in_=self.t32b[:].rearrange("p (h d) -> p h d", d=64),
                                            axis=AX.X, op=ALU.add), r=["t32b"], w=["D_ss"])
            self.rstd_col(self.D_ss[:, 8:16], self.D_ss[:, 0:8], 64, 1e-6, ["D_ss"], "D_ss")
            P.dve(lambda e: e.tensor_scalar(out=self.D_ss[:, 8:12], in0=self.D_ss[:, 8:12], scalar1=0.125, scalar2=None,
                                            op0=ALU.mult), r=["D_ss"], w=["D_ss"])
            P.dve(lambda e: e.tensor_tensor(out=self.t32a[:].rearrange("p (h d) -> p h d", d=64),
                                            in0=self.t32a[:].rearrange("p (h d) -> p h d", d=64),
                                            in1=self.D_ss[:, 8:16].unsqueeze(2).to_broadcast([128, 8, 64]), op=ALU.mult),
                  r=["t32a", "D_ss"], w=["t32a"])
            P.dve(lambda e: e.tensor_tensor(out=self.D_qn[:], in0=self.t32a[:], in1=self.rowD, op=ALU.mult),
                  r=["t32a", "rowD"], w=["D_qn"])
            if KD <= 2:
                continue
            for i in range(8):
                P.pe(lambda e, i=i: e.transpose(self.tp[0:64, 128 * i:128 * i + 128], self.D_qn[:, 64 * i:64 * i + 64],
                                                self.identb), r=["D_qn", "CB"], w=["tp"])
            if KD <= 3:
                continue
            P.act(lambda e, t=t: e.activation(out=self.D_Q[0:64, :, 128 * t:128 * t + 128],
                                              in_=self.tp[0:64, 0:512].rearrange("p (h s) -> p h s", s=128), func=AF.Copy),
                  r=["tp"], w=["D_Q"])
            if os.environ.get("KE") == "1":
                continue
            P.act(lambda e, tt=tt: e.activation(out=self.D_K[0:64, :, 128 * tt:128 * tt + 128],
                                                in_=self.tp[0:64, 512:1024].rearrange("p (h s) -> p h s", s=128),
                                                func=AF.Copy), r=["tp"], w=[("D_K", tt)])
        self.wrel()
        wv, kv = self.wnext("D_v")
        for t in range(4 if KD > 4 else 0):
            tt = 4 * sg + t
            pj, pk = self.proj_tm(wv, kv, 256, t, 1536)
            P.act(lambda e, pj=pj, tt=tt: e.activation(out=self.D_V[:, tt, :, 0:64],
                                                       in_=pj[:, 0:256].rearrange("p (h d) -> p h d", d=64), func=AF.Copy),
                  r=[pk], w=[("D_V", tt)])
        self.wrel()
        wz, kz = self.wnext("D_z")
        for t in range(4 if KD > 5 else 0):
            tt = 4 * sg + t
            pj, pk = self.proj_tm(wz, kz, 256, t, 1792)
            P.act(lambda e, pj=pj, t=t: e.activation(out=self.D_z[:, t, :], in_=pj[:, 0:256], func=AF.Copy),
                  r=[pk], w=["D_z"])
        self.wrel()
        wg, kg = self.wnext("D_g")
        if KD <= 6:
            self.wrel()
            return
        self.gate_rows(wg, kg, 0, self.colp[:, 31:32], self.t32a[:], "t32a")
        self.wrel()
        self.softplus_neg(self.t32a[:], "t32a")
        P.dve(lambda e: e.memset(self.t32b[:], 1.0), w=["t32b"])
        P.dve(lambda e: e.tensor_tensor_scan(out=self.D_negF[:], data0=self.t32b[:], data1=self.t32a[:],
                                             initial=self.D_Fc[:, 0:1], op0=ALU.mult, op1=ALU.add),
              r=["t32a", "t32b", "D_Fc"], w=["D_negF"])
        P.dve(lambda e: e.tensor_copy(out=self.D_Fc[:], in_=self.D_negF[:, SEG - 1:SEG]), r=["D_negF"], w=["D_Fc"])
        if KSTOP <= 1:
            return
        for h in range(4):
            pj, pk = self.nextpj()
            P.pe(lambda e, h=h, pj=pj: e.matmul(pj[0:65, :], lhsT=cf["sel"][:, 128 * h:128 * h + 65], rhs=self.D_negF[:],
                                                start=True, stop=True), r=["CF", "D_negF"], w=[pk])
            P.act(lambda e, h=h, pj=pj: e.activation(out=self.D_Q[64:65, h, :], in_=pj[64:65, :], func=AF.Copy, scale=-1.0),
                  r=[pk], w=["D_Q"])
        if KSTOP <= 2:
            return
        for t in range(4):
            tt = 4 * sg + t
            P.pe(lambda e, t=t: e.transpose(self.tpf[:, 0:128], self.D_negF[:, 128 * t:128 * t + 128], cf["ident"]),
                 r=["D_negF", "CF"], w=["tpf"])
            P.dve(lambda e, tt=tt: e.tensor_copy(out=self.D_fcol[:, tt, :], in_=self.tpf[:, 0:128:32]), r=["tpf"],
                  w=["D_fcol"])
        if KSTOP <= 3:
            return
        nq = 4
        for h in range(4 if KSTOP > 4 else 1):
            for kb in range(4 * sg + 4):
                q0 = max(kb - 4 * sg, 0)
                ncol = 128 * (nq - q0)
                sc, sk = self.sc[kb % 2], ("sc", kb % 2)
                diag = kb >= 4 * sg
                P.pe(lambda e, h=h, kb=kb, q0=q0, ncol=ncol, sc=sc, diag=diag: e.matmul(
                    sc[:, 0:ncol], lhsT=self.D_K[0:65, h, 128 * kb:128 * kb + 128], rhs=self.D_Q[0:65, h, 128 * q0:SEG],
                    start=True, stop=(not diag)), r=[("D_K", kb), "D_Q"], w=[sk])
                if diag:
                    P.pe(lambda e, sc=sc: e.matmul(sc[:, 0:128], lhsT=self.identb, rhs=self.masknegb,
                                                   start=False, stop=True), r=["CB", "CB"], w=[sk])
                pt, ptk = self.PT[:, kb % 2, :], ("PT", kb % 2)
                P.act(lambda e, h=h, kb=kb, ncol=ncol, sc=sc, pt=pt: e.activation(
                    out=pt[:, 0:ncol], in_=sc[:, 0:ncol], func=AF.Exp, bias=self.D_fcol[:, kb, h:h + 1]),
                    r=[sk, "D_fcol"], w=[ptk])
                for ql in range(q0, nq):
                    qb = 4 * sg + ql
                    P.pe(lambda e, h=h, kb=kb, ql=ql, q0=q0, qb=qb, pt=pt: e.matmul(
                        self.acc[:, 128 * ql:128 * ql + 65], lhsT=pt[:, 128 * (ql - q0):128 * (ql - q0) + 128],
                        rhs=self.D_V[:, kb, h, :], start=(kb == 0 and ql == 0), stop=(kb == qb),
                        skip_group_check=True), r=[ptk, ("D_V", kb)], w=["acc"])
            accv = self.acc[:, :].rearrange("p (q c) -> p q c", c=128)
            P.dve(lambda e, accv=accv: e.reciprocal(out=self.D_rd[:], in_=accv[:, :, 64]), r=["acc"], w=["D_rd"])
            P.dve(lambda e, accv=accv, h=h: e.tensor_tensor(out=self.D_o[:, :, h, :], in0=accv[:, :, 0:64],
                                                            in1=self.D_rd[:].unsqueeze(2).to_broadcast([128, 4, 64]),
                                                            op=ALU.mult), r=["acc", "D_rd"], w=["D_o"])
        for t in range(4):
            self.silu_gate(self.D_z[:, t, :], "D_z", self.t32a[:, 0:256], "t32a")
            P.dve(lambda e, t=t: e.tensor_tensor(out=self.D_y[:], in0=self.D_o[:, t, :, :].rearrange("p h d -> p (h d)"),
                                                 in1=self.t32a[:, 0:256], op=ALU.mult), r=["D_o", "t32a"], w=["D_y"])
            self.y_to_yT(self.D_y, "D_y", t, 6)

    def xattn_kv(self):
        P = self.P
        self.scr_reset()
        self.kmT = self.scr("kmT", [128, 8, 256], BF16)
        self.vm = self.scr("vm", [128, 2, D], BF16)
        self.PT = self.scr("PT", [128, 2, 512], BF16)
        self.t32d = self.scr("t32d", [128, 512])
        for i in range(2):
            w, wk = self.wnext("KV%d" % i)
            for c in range(4):
                fc = 4 * i + c
                pj, pk = self.nextpj()
                for dc in range(8):
                    P.pe(lambda e, dc=dc, c=c, pj=pj, w=w: e.matmul(pj[:, 0:256], lhsT=w[:, dc, 128 * c:128 * c + 128],
                                                                    rhs=self.memT[:, dc, :], start=(dc == 0), stop=(dc == 7)),
                         r=[wk, "memT"], w=[pk])
                P.act(lambda e, fc=fc, pj=pj: e.activation(out=self.kmT[:, fc, :], in_=pj[:, 0:256], func=AF.Copy),
                      r=[pk], w=["kmT"])
            self.wrel()
        for i in range(2):
            w, wk = self.wnext("KV%d" % (2 + i))
            for mt in range(2):
                pj, pk = self.nextpj()
                for dc in range(8):
                    P.pe(lambda e, dc=dc, mt=mt, pj=pj, w=w: e.matmul(pj[:], lhsT=self.memT[:, dc, 128 * mt:128 * mt + 128],
                                                                      rhs=w[:, dc, :], start=(dc == 0), stop=(dc == 7)),
                         r=[wk, "memT"], w=[pk])
                P.act(lambda e, i=i, mt=mt, pj=pj: e.activation(out=self.vm[:, mt, 512 * i:512 * i + 512], in_=pj[:],
                                                                func=AF.Copy), r=[pk], w=["vm"])
            self.wrel()

    def xattn_seg(self, sg):
        P = self.P
        qT = self.yT
        for i in range(2):
            w, wk = self.wnext("Q%d" % i)
            for c in range(4):
                fc = 4 * i + c
                pj, pk = self.nextpj()
                for dc in range(8):
                    P.pe(lambda e, dc=dc, c=c, pj=pj, w=w: e.matmul(pj[:], lhsT=w[:, dc, 128 * c:128 * c + 128],
                                                                    rhs=self.hT[:, dc, :], start=(dc == 0), stop=(dc == 7)),
                         r=[wk, "hT"], w=[pk])
                P.act(lambda e, fc=fc, pj=pj: e.activation(out=qT[:, fc, :], in_=pj[:], func=AF.Copy), r=[pk], w=["yT"])
            self.wrel()
        oT = self.hT
        for h in range(4):
            for mt in range(2):
                sc, sk = self.sc[mt], ("sc", mt)
                for j in range(2):
                    P.pe(lambda e, h=h, mt=mt, j=j, sc=sc: e.matmul(
                        sc[:], lhsT=self.kmT[:, 2 * h + j, 128 * mt:128 * mt + 128], rhs=qT[:, 2 * h + j, :],
                        start=(j == 0), stop=(j == 1)), r=["kmT", "yT"], w=[sk])
                P.act(lambda e, mt=mt, sc=sc: e.activation(out=self.PT[:, mt, :], in_=sc[:], func=AF.Exp, scale=1.0 / 16.0),
                      r=[sk], w=[("PT", mt)])
            for mt in range(2):
                P.pe(lambda e, mt=mt: e.matmul(self.stp[:], lhsT=self.onesb, rhs=self.PT[:, mt, :], start=(mt == 0),
                                               stop=(mt == 1)), r=["CB", ("PT", mt)], w=["stp"])
            P.dve(lambda e: e.reciprocal(out=self.t32d[:], in_=self.stp[:]), r=["stp"], w=["t32d"])
            for j in range(2):
                fc = 2 * h + j
                for mt in range(2):
                    P.pe(lambda e, fc=fc, mt=mt: e.matmul(self.acc[:], lhsT=self.vm[:, mt, 128 * fc:128 * fc + 128],
                                                          rhs=self.PT[:, mt, :], start=(mt == 0), stop=(mt == 1)),
                         r=["vm", ("PT", mt)], w=["acc"])
                P.dve(lambda e, fc=fc: e.tensor_tensor(out=oT[:, fc, :], in0=self.acc[:], in1=self.t32d[:], op=ALU.mult),
                      r=["acc", "t32d"], w=["hT"])


def build_program(nl=NL, dbg=(), mixers="ABCD", xattn=True):
    nc = bass.Bass("TRN2", target_bir_lowering=False)
    b = Builder(nc, nl, dbg, mixers, xattn)
    with b.es:
        b.build()
    return nc, b


_CACHE = {}


def kernel(**inputs):
    inp = {k: np.asarray(v) for k, v in inputs.items()}
    lay = host_layout(inp)
    if "nc" not in _CACHE:
        _CACHE["nc"] = build_program()[0]
    nc = _CACHE["nc"]
    shared = dict(w_in=inp["w_in"], w_out=inp["w_out"], xattn_wq=inp["xattn_wq"], xattn_wkv=inp["xattn_wkv"],
                  xattn_wo=inp["xattn_wo"], post_norm_g=inp["post_norm_g"], xattn_post_g=inp["xattn_post_g"],
                  mem_norm_g=inp["mem_norm_g"].reshape(1, D), **lay)
    shared = {k: np.ascontiguousarray(v, dtype=np.float32) for k, v in shared.items()}
    in_maps = []
    for b in range(8):
        m = dict(shared)
        m["x"] = np.ascontiguousarray(inp["x"][b], dtype=np.float32)
        m["mem"] = np.ascontiguousarray(inp["mem"][b], dtype=np.float32)
        in_maps.append(m)
    res = run_bass_kernel_spmd(nc, in_maps, core_ids=list(range(8)))
    return np.stack([res.results[b]["out"] for b in range(8)]).astype(np.float32)
```
